# Optimizing a Trainium2 kernel written in Bass

```python
import jax, jax.numpy as jnp
from jax import lax
import numpy as np


D_MODEL = 1024
BATCH = 4
SEQ = 4096
DEPTH = 1

EPS = 1e-6
D_RNN = D_MODEL
RNN_BLOCKS = 8
RNN_BW = D_RNN // RNN_BLOCKS
CONV_W = 4
LRU_C = 8.0
N_HEADS = 8
HEAD_DIM = 128
D_ATT = N_HEADS * HEAD_DIM
MOBA_BLOCK = 256
MOBA_TOPK = 3
Q_CHUNK = 16
ROPE_DIMS = HEAD_DIM // 4
ROPE_THETA = 500000.0
N_BRANCH = 2
D_BRANCH = D_MODEL
D_IN = 2 * D_RNN + 3 * D_ATT + N_BRANCH * D_MODEL
N_EXPERTS = 32
TOP_K = 4
D_FF = D_MODEL
SWIGLU_LIMIT = 7.0
SWIGLU_ALPHA = 1.702
EXPERT_ROWS = 256

kernel_name = 'hybrid_rglru_moba_moe_adaln'


def rms_norm(x, g):
    x32 = x.astype(jnp.float32)
    y = x32 * lax.rsqrt(jnp.mean(x32 * x32, axis=-1, keepdims=True) + EPS)
    return (y * g.astype(jnp.float32)).astype(x.dtype)


def partial_rope(x, positions):
    half = ROPE_DIMS // 2
    freqs = ROPE_THETA ** (-jnp.arange(half, dtype=jnp.float32) / half)
    ang = positions.astype(jnp.float32)[:, :, None] * freqs
    cos = jnp.cos(ang)[:, :, None, :]
    sin = jnp.sin(ang)[:, :, None, :]
    x1 = x[..., :half].astype(jnp.float32)
    x2 = x[..., half:ROPE_DIMS].astype(jnp.float32)
    rot = jnp.concatenate([x1 * cos - x2 * sin, x2 * cos + x1 * sin], axis=-1).astype(x.dtype)
    return jnp.concatenate([rot, x[..., ROPE_DIMS:]], axis=-1)


def _lin_combine(left, right):
    a_l, b_l = left
    a_r, b_r = right
    return a_l * a_r, a_r * b_l + b_r


def recurrent_branch(xr, gr, conv_w, conv_b, w_rg_a, b_rg_a, w_rg_x, b_rg_x, lru_lambda):
    B, S, _ = xr.shape
    xp = jnp.pad(xr, ((0, 0), (CONV_W - 1, 0), (0, 0)))
    xc = conv_b + conv_w[0] * xp[:, CONV_W - 1:CONV_W - 1 + S]
    for i in range(1, CONV_W):
        xc = xc + conv_w[i] * xp[:, CONV_W - 1 - i:CONV_W - 1 - i + S]
    xb = xc.reshape(B, S, RNN_BLOCKS, RNN_BW)
    r = jax.nn.sigmoid(jnp.einsum('bsnw,nwv->bsnv', xb, w_rg_a).reshape(B, S, D_RNN) + b_rg_a)
    ig = jax.nn.sigmoid(jnp.einsum('bsnw,nwv->bsnv', xb, w_rg_x).reshape(B, S, D_RNN) + b_rg_x)
    log_a = -LRU_C * r.astype(jnp.float32) * jax.nn.softplus(-lru_lambda.astype(jnp.float32))
    a = jnp.exp(log_a)
    mult = jnp.sqrt(-jnp.expm1(2.0 * log_a))
    mult = mult.at[:, 0].set(1.0)
    u = mult * (ig * xc).astype(jnp.float32)
    _, h = lax.associative_scan(_lin_combine, (a, u), axis=1)
    return h.astype(xr.dtype) * jax.nn.gelu(gr)


def moba_attention(q, k, v):
    B, H, S, dh = q.shape
    nb = -(-S // MOBA_BLOCK)
    pad = nb * MOBA_BLOCK - S
    kp = jnp.pad(k, ((0, 0), (0, 0), (0, pad), (0, 0))).reshape(B, H, nb, MOBA_BLOCK, dh)
    vp = jnp.pad(v, ((0, 0), (0, 0), (0, pad), (0, 0))).reshape(B, H, nb, MOBA_BLOCK, dh)
    k_mean = jnp.mean(kp.astype(jnp.float32), axis=3)
    q_blk = jnp.arange(S) // MOBA_BLOCK
    gate = jnp.einsum('bhsd,bhnd->bhsn', q.astype(jnp.float32), k_mean)
    past = jnp.arange(nb)[None, :] < q_blk[:, None]
    gate = jnp.where(past, gate, -jnp.inf)
    topk = min(MOBA_TOPK, nb)
    _, sel = lax.top_k(gate, topk)
    sel_ok = jnp.arange(topk)[None, :] < jnp.minimum(q_blk, MOBA_TOPK)[:, None]
    nc = S // Q_CHUNK

    def to_chunks(t):
        t = t.reshape(t.shape[:2] + (nc, Q_CHUNK) + t.shape[3:])
        return jnp.moveaxis(t, 2, 0)

    b_ix = jnp.arange(B)[:, None, None, None]
    h_ix = jnp.arange(H)[None, :, None, None]
    scale = dh ** -0.5

    def chunk_attn(args):
        ci, qc, sc, okc = args
        start = ci * Q_CHUNK
        blk = start // MOBA_BLOCK
        k_own = lax.dynamic_index_in_dim(kp, blk, axis=2, keepdims=False)
        v_own = lax.dynamic_index_in_dim(vp, blk, axis=2, keepdims=False)
        k_sel = kp[b_ix, h_ix, sc]
        v_sel = vp[b_ix, h_ix, sc]
        q_pos = start + jnp.arange(Q_CHUNK)
        k_pos = blk * MOBA_BLOCK + jnp.arange(MOBA_BLOCK)
        s_own = jnp.einsum('bhqd,bhkd->bhqk', qc, k_own).astype(jnp.float32) * scale
        s_own = jnp.where(k_pos[None, :] <= q_pos[:, None], s_own, -jnp.inf)
        s_sel = jnp.einsum('bhqd,bhqjkd->bhqjk', qc, k_sel).astype(jnp.float32) * scale
        s_sel = jnp.where(okc[:, :, None], s_sel, -jnp.inf).reshape(B, H, Q_CHUNK, topk * MOBA_BLOCK)
        p = jax.nn.softmax(jnp.concatenate([s_own, s_sel], axis=-1), axis=-1).astype(v.dtype)
        p_own = p[..., :MOBA_BLOCK]
        p_sel = p[..., MOBA_BLOCK:].reshape(B, H, Q_CHUNK, topk, MOBA_BLOCK)
        return (jnp.einsum('bhqk,bhkd->bhqd', p_own, v_own)
                + jnp.einsum('bhqjk,bhqjkd->bhqd', p_sel, v_sel))

    out = lax.map(chunk_attn, (jnp.arange(nc), to_chunks(q), to_chunks(sel),
                               sel_ok.reshape(nc, Q_CHUNK, topk)))
    out = jnp.moveaxis(out, 0, 2).reshape(B, H, S, dh)
    return out.transpose(0, 2, 1, 3).reshape(B, S, H * dh)


def moe_ffn(h, w_router, b_router, w_up, b_up, w_down, b_down):
    B, S, D = h.shape
    N = B * S
    xt = h.reshape(N, D)
    logits = (xt @ w_router + b_router).astype(jnp.float32)
    top_val, top_idx = lax.top_k(logits, TOP_K)
    gates = jax.nn.softmax(top_val, axis=-1)
    A = N * TOP_K
    e_flat = top_idx.reshape(A)
    tok_flat = jnp.arange(A, dtype=jnp.int32) // TOP_K
    g_flat = gates.reshape(A)
    order = jnp.argsort(e_flat)
    e_sorted = e_flat[order]
    counts = jnp.zeros((N_EXPERTS,), jnp.int32).at[e_flat].add(1)
    padded = (counts + EXPERT_ROWS - 1) // EXPERT_ROWS * EXPERT_ROWS
    start = jnp.cumsum(counts) - counts
    pad_end = jnp.cumsum(padded)
    pad_start = pad_end - padded
    dest = pad_start[e_sorted] + jnp.arange(A, dtype=jnp.int32) - start[e_sorted]
    n_blocks = -(-A // EXPERT_ROWS) + N_EXPERTS
    R = n_blocks * EXPERT_ROWS
    row_tok = jnp.full((R,), N, jnp.int32).at[dest].set(tok_flat[order])
    row_gate = jnp.zeros((R,), jnp.float32).at[dest].set(g_flat[order])
    block_start = jnp.arange(n_blocks, dtype=jnp.int32) * EXPERT_ROWS
    block_exp = jnp.minimum(jnp.sum(block_start[:, None] >= pad_end[None, :], axis=1), N_EXPERTS - 1)
    x_pad = jnp.concatenate([xt, jnp.zeros((1, D), xt.dtype)], axis=0)
    x_rows = x_pad[row_tok].reshape(n_blocks, EXPERT_ROWS, D)

    def expert_block(args):
        xb, e = args
        hc = xb @ w_up[e] + b_up[e]
        g = jnp.minimum(hc[:, :D_FF], SWIGLU_LIMIT)
        lin = jnp.clip(hc[:, D_FF:], -SWIGLU_LIMIT, SWIGLU_LIMIT)
        act = (lin + 1.0) * g * jax.nn.sigmoid(SWIGLU_ALPHA * g)
        return act @ w_down[e] + b_down[e]

    y_rows = lax.map(expert_block, (x_rows, block_exp)).reshape(R, D)
    y_rows = y_rows * row_gate[:, None].astype(y_rows.dtype)
    y = jax.ops.segment_sum(y_rows, row_tok, num_segments=N + 1)[:N]
    return y.reshape(B, S, D)


def setup_inputs(seed: int = 0) -> dict:
    key = jax.random.key(seed)
    ks = jax.random.split(key, 32)
    f32 = jnp.float32
    L = DEPTH

    def nrm(k, shape, scale):
        return jax.random.normal(k, shape, f32) * scale

    u = jax.random.uniform(ks[13], (L, D_RNN), f32, 0.9, 0.999)
    a0 = u ** (1.0 / LRU_C)
    return {
        'x': nrm(ks[0], (BATCH, SEQ, D_MODEL), 1.0),
        'c': nrm(ks[1], (BATCH, D_MODEL), 1.0),
        'positions': jnp.tile(jnp.arange(SEQ, dtype=jnp.int32)[None, :], (BATCH, 1)),
        'ada_w': nrm(ks[2], (L, D_MODEL, 6 * D_MODEL), 0.5 * D_MODEL ** -0.5),
        'ada_b': nrm(ks[3], (L, 6 * D_MODEL), 0.01),
        'norm1_g': 1.0 + nrm(ks[4], (L, D_MODEL), 0.02),
        'norm2_g': 1.0 + nrm(ks[5], (L, D_MODEL), 0.02),
        'w_in': nrm(ks[6], (L, D_MODEL, D_IN), D_MODEL ** -0.5),
        'conv_w': nrm(ks[7], (L, CONV_W, D_RNN), CONV_W ** -0.5),
        'conv_b': nrm(ks[8], (L, D_RNN), 0.01),
        'w_rg_a': nrm(ks[9], (L, RNN_BLOCKS, RNN_BW, RNN_BW), RNN_BW ** -0.5),
        'b_rg_a': nrm(ks[10], (L, D_RNN), 0.01),
        'w_rg_x': nrm(ks[11], (L, RNN_BLOCKS, RNN_BW, RNN_BW), RNN_BW ** -0.5),
        'b_rg_x': nrm(ks[12], (L, D_RNN), 0.01),
        'lru_lambda': jnp.log(a0) - jnp.log1p(-a0),
        'q_norm_g': 1.0 + nrm(ks[14], (L, HEAD_DIM), 0.02),
        'k_norm_g': 1.0 + nrm(ks[15], (L, HEAD_DIM), 0.02),
        'b_gate': nrm(ks[16], (L, N_BRANCH, D_MODEL), 0.01),
        'w_branch': nrm(ks[17], (L, N_BRANCH, D_BRANCH, D_MODEL), D_BRANCH ** -0.5),
        'w_out': nrm(ks[18], (L, D_MODEL, D_MODEL), D_MODEL ** -0.5),
        'w_router': nrm(ks[19], (L, D_MODEL, N_EXPERTS), D_MODEL ** -0.5),
        'b_router': nrm(ks[20], (L, N_EXPERTS), 0.01),
        'w_up': nrm(ks[21], (L, N_EXPERTS, D_MODEL, 2 * D_FF), D_MODEL ** -0.5),
        'b_up': nrm(ks[22], (L, N_EXPERTS, 2 * D_FF), 0.01),
        'w_down': nrm(ks[23], (L, N_EXPERTS, D_FF, D_MODEL), D_FF ** -0.5),
        'b_down': nrm(ks[24], (L, N_EXPERTS, D_MODEL), 0.01),
    }


def reference(x, c, positions, ada_w, ada_b, norm1_g, norm2_g, w_in, conv_w, conv_b,
              w_rg_a, b_rg_a, w_rg_x, b_rg_x, lru_lambda, q_norm_g, k_norm_g, b_gate,
              w_branch, w_out, w_router, b_router, w_up, b_up, w_down, b_down):
    B, S, D = x.shape
    split_at = [D_RNN, 2 * D_RNN, 2 * D_RNN + D_ATT, 2 * D_RNN + 2 * D_ATT, 2 * D_RNN + 3 * D_ATT]
    for l in range(DEPTH):
        mod = jax.nn.silu(c) @ ada_w[l] + ada_b[l]
        sh1, sc1, g1, sh2, sc2, g2 = jnp.split(mod[:, None, :], 6, axis=-1)
        h = rms_norm(x, norm1_g[l]) * (1.0 + sc1) + sh1
        proj = h @ w_in[l]
        xr, gr, q, k, v, gl = jnp.split(proj, split_at, axis=-1)
        y_rnn = recurrent_branch(xr, gr, conv_w[l], conv_b[l], w_rg_a[l], b_rg_a[l],
                                 w_rg_x[l], b_rg_x[l], lru_lambda[l])
        q = partial_rope(rms_norm(q.reshape(B, S, N_HEADS, HEAD_DIM), q_norm_g[l]), positions)
        k = partial_rope(rms_norm(k.reshape(B, S, N_HEADS, HEAD_DIM), k_norm_g[l]), positions)
        v = v.reshape(B, S, N_HEADS, HEAD_DIM)
        y_att = moba_attention(q.transpose(0, 2, 1, 3), k.transpose(0, 2, 1, 3),
                               v.transpose(0, 2, 1, 3))
        branches = jnp.stack([y_rnn, y_att], axis=2)
        z = jnp.einsum('bsgi,gid->bsgd', branches, w_branch[l])
        gates = jax.nn.sigmoid(gl.reshape(B, S, N_BRANCH, D_MODEL) + b_gate[l])
        mixed = jnp.sum(gates * z, axis=2) @ w_out[l]
        x = x + g1 * mixed
        h2 = rms_norm(x, norm2_g[l]) * (1.0 + sc2) + sh2
        x = x + g2 * moe_ffn(h2, w_router[l], b_router[l], w_up[l], b_up[l], w_down[l], b_down[l])
    return x
```

```python
import numpy as np
import ml_dtypes
from contextlib import ExitStack
import concourse.bass as bass
import concourse.mybir as mybir
from concourse.bass_utils import run_bass_kernel_spmd

F32 = mybir.dt.float32
BF16 = mybir.dt.bfloat16
I32 = mybir.dt.int32
U32 = mybir.dt.uint32
AF = mybir.ActivationFunctionType
ALU = mybir.AluOpType
AX = mybir.AxisListType

D = 1024
T = 2048
NT = 16
LT = 4096
NE = 32
CAP = 512
NH = 4
CH = 256
NTOT = NE * CAP + NH * CH
EPS = 1e-6
NEG = -30000.0
FLAGS = {}


class _Op:
    __slots__ = ("eng", "fn", "deps", "signal", "sidx", "dma", "sem", "semval", "n")


class Prog:
    CE = ("pe", "act", "dve", "pool", "sp")
    EPOCH = 12000

    def __init__(self, nc, stack):
        self.nc = nc
        self.stack = stack
        self.ops = {e: [] for e in ("pe", "act", "dve", "pool", "sp")}
        self.state = {}
        self.subs = {}
        self.ndma = {"sp": 0, "act": 0, "pool": 0}
        self.dma_last = {}
        self.NDS = 8
        self.n = 0

    def sb(self, name, shape, dt):
        return self.stack.enter_context(self.nc.sbuf_tensor(name, list(shape), dt))

    def ps(self, name, shape, dt):
        return self.stack.enter_context(self.nc.psum_tensor(name, list(shape), dt))

    def _keys(self, k):
        if isinstance(k, tuple):
            name = k[0]
            self.subs.setdefault(name, set()).add(k)
            return [k, name]
        else:
            return [k] + list(self.subs.get(k, ()))

    def op(self, eng, fn, r=(), w=(), dma=False):
        o = _Op()
        o.eng, o.fn, o.deps, o.signal, o.dma = eng, fn, set(), False, dma
        o.sem = o.semval = o.sidx = None
        o.n = self.n
        self.n += 1
        for k in r:
            for kk in self._keys(k):
                st = self.state.get(kk)
                if st and st[0] is not None:
                    o.deps.add(st[0])
        for k in w:
            for kk in self._keys(k):
                st = self.state.get(kk)
                if st:
                    if st[0] is not None:
                        o.deps.add(st[0])
                    for x in st[1]:
                        o.deps.add(x)
        for k in r:
            st = self.state.setdefault(k, [None, []])
            st[1].append(o)
        for k in w:
            self.state[k] = [o, []]
            if not isinstance(k, tuple):
                for kk in self.subs.get(k, ()):
                    self.state[kk] = [o, []]
        o.deps.discard(o)
        if dma:
            j = self.ndma[eng] % self.NDS
            self.ndma[eng] += 1
            key = (eng, j)
            prev = self.dma_last.get(key)
            if prev is not None:
                o.deps.add(prev)
                o.semval = prev.semval + 16
            else:
                o.semval = 16
            o.sem = key
            self.dma_last[key] = o
        for x in o.deps:
            if not x.dma and not (x.eng == "pe" and eng == "pe" and not dma):
                x.signal = True
        self.ops[eng].append(o)
        return o

    def emit(self, final_ops):
        nc = self.nc
        st = self.stack
        nsig = {}
        for e in self.CE:
            c = 0
            for o in self.ops[e]:
                if o.signal and not o.dma:
                    o.sidx = c
                    c += 1
            nsig[e] = c
        esem = {}
        for e in self.CE:
            for ep in range(nsig[e] // self.EPOCH + 1):
                esem[(e, ep)] = st.enter_context(nc.semaphore("s_%s_%d" % (e, ep)))
        dsem = {}
        for q in ("sp", "act", "pool"):
            for j in range(min(self.NDS, self.ndma[q])):
                dsem[(q, j)] = st.enter_context(nc.semaphore("d_%s_%d" % (q, j)))
        fin = st.enter_context(nc.semaphore("fin"))
        semname = {id(v): k for k, v in list(esem.items()) + list(dsem.items())}
        block = st.enter_context(nc.Block())
        EP = self.EPOCH

        def run(ename, eng, extra=None):
            waited = {}
            for o in self.ops[ename]:
                need = {}
                for x in o.deps:
                    if x.dma:
                        s, v = dsem[x.sem], x.semval
                    else:
                        if x.eng == "pe" and ename == "pe" and not o.dma:
                            continue
                        s, v = esem[(x.eng, x.sidx // EP)], x.sidx % EP + 1
                    if waited.get(s, 0) >= v:
                        continue
                    if need.get(s, 0) < v:
                        need[s] = v
                for s, v in need.items():
                    eng.wait_ge(s, v)
                    waited[s] = v
                if FLAGS.get("trace"):
                    print(ename, o.n, "waits", [(semname[id(s)], v) for s, v in need.items()],
                          "sig", (o.sidx if o.signal and not o.dma else None), "dma", (o.sem, o.semval) if o.dma else None)
                ins = o.fn(eng)
                if o.dma:
                    ins.then_inc(dsem[o.sem], 16)
                elif o.signal:
                    ins.then_inc(esem[(ename, o.sidx // EP)], 1)
            if extra:
                extra(eng, waited)

        def fin_sp(eng, waited):
            for x in final_ops:
                s, v = dsem[x.sem], x.semval
                if waited.get(s, 0) < v:
                    eng.wait_ge(s, v)
                    waited[s] = v

        @block.sync
        def _(e):
            run("sp", e, fin_sp)

        @block.scalar
        def _(e):
            run("act", e)

        @block.vector
        def _(e):
            run("dve", e)

        @block.gpsimd
        def _(e):
            run("pool", e)

        @block.tensor
        def _(e):
            run("pe", e)


ARENA_F32 = 51600


class Arena:
    def __init__(self, big):
        self.big = big
        self.off = 0
        self.marks = []

    def push(self):
        self.marks.append(self.off)

    def pop(self):
        self.off = self.marks.pop()

    def get(self, dt, shape, parts=128):
        n = 1
        for s in shape:
            n *= s
        esz = 4 if dt in (F32, I32, U32) else 2
        words = (n * esz + 3) // 4
        words = (words + 7) // 8 * 8
        a = self.big[0:parts, self.off:self.off + words]
        self.off += words
        assert self.off <= ARENA_F32, ("arena overflow", self.off)
        if dt != F32:
            a = a.bitcast(dt)
        a = a[:, 0:n]
        if len(shape) == 2:
            a = a.rearrange("p (a b) -> p a b", a=shape[0])
        elif len(shape) == 3:
            a = a.rearrange("p (a b c) -> p a b c", a=shape[0], b=shape[1])
        return a


def build_program(stage=99, dbg=None):
    nc = bass.Bass("TRN2", target_bir_lowering=False)
    dbg = dbg or {}

    def din(name, shape, dt=F32):
        return nc.dram_tensor(name, list(shape), dt, kind="ExternalInput").ap()

    x_own = din("x_own", [T, D]); x_pre = din("x_pre", [T, D])
    pos = din("pos", [1, LT], I32)
    c_col = din("c_col", [128, 8]); hflag_d = din("hflag", [128, 1]); vbias_d = din("vbias", [128, 8 * 16])
    ada_w = din("ada_w", [D, 6 * D]); ada_b_col = din("ada_b_col", [128, 48]); ada_b_row = din("ada_b_row", [1, 4 * D])
    n1g_col = din("n1g_col", [128, 8]); n2g_row = din("n2g_row", [1, D])
    w_in = din("w_in", [D, 7 * D])
    convw_col = din("convw_col", [128, 32]); convb_col = din("convb_col", [128, 8])
    w_rg_a = din("w_rg_a", [8, 128, 128]); brga_col = din("brga_col", [128, 8])
    w_rg_x = din("w_rg_x", [8, 128, 128]); brgx_col = din("brgx_col", [128, 8])
    lam_col = din("lam_col", [128, 8]); qg_col = din("qg_col", [128, 1]); kg_col = din("kg_col", [128, 1])
    bgate_col = din("bgate_col", [128, 16])
    w_branch = din("w_branch", [2, D, D]); w_out = din("w_out", [D, D])
    w_router = din("w_router", [D, NE]); b_router_row = din("b_router_row", [1, NE])
    w_up = din("w_up", [NE, D, 2 * D]); bup_col = din("bup_col", [128, NE * 16])
    w_down = din("w_down", [NE, D, D]); b_down = din("b_down", [NE, D])
    ident_bf_d = din("ident_bf", [128, 128], BF16); ident_f_d = din("ident_f", [128, 128])
    triu_bf_d = din("triu_bf", [128, 128], BF16); tribias_d = din("tribias", [128, 256], BF16)
    rm_f_d = din("rm_f", [128, 32]); freq_d = din("freq_col", [32, 2]); iota32_d = din("iota32", [128, NE]); pcol_d = din("pcol", [128, 2]); k128_d = din("k128", [128, 8]); bq_d = din("bq", [NE * 128, 16])
    out_d = nc.dram_tensor("out", [T, D], F32, kind="ExternalOutput").ap()
    xg_d = nc.dram_tensor("xg_scr", [NTOT + 128, D], BF16, kind="Internal").ap()
    yall_d = nc.dram_tensor("yall_scr", [NTOT + 128, D], F32, kind="Internal").ap()
    x1_d = nc.dram_tensor("x1_scr", [T, D], F32, kind="Internal").ap()
    modb_d = nc.dram_tensor("modb_scr", [128, 4 * D], F32, kind="Internal").ap()
    dbg_out = {}
    for k, shp in dbg.items():
        dbg_out[k] = nc.dram_tensor("dbg_" + k, list(shp), F32, kind="ExternalOutput").ap()

    st = ExitStack()
    with st:
        P = Prog(nc, st)
        big = P.sb("arena", [128, ARENA_F32], F32)
        AR = Arena(big)
        dest_sb = P.sb("dest_sb", [128, 16, 4], I32)
        idxw_sb = P.sb("idxw_sb", [128, NH, 8], I32)
        idxb_sb = P.sb("idxb_sb", [128, NH, 2], I32)
        stage_sb = [P.sb("stage_sb%d" % i, [128, D], BF16) for i in range(2)]
        pb = [P.ps("pb%d" % i, [128, 512], F32) for i in range(6)]
        pbt = [P.ps("pbt%d" % i, [128, 1024], BF16) for i in range(2)]
        finals = []
        cnt = [0]

        def uid(s):
            cnt[0] += 1
            return "%s_%d" % (s, cnt[0])

        def dma(q, out, in_, r=(), w=()):
            return P.op(q, lambda e: e.dma_start(out=out, in_=in_), r=r, w=w, dma=True)

        def barrier():
            lasts = []
            for e in ("pe", "act", "dve", "pool", "sp"):
                if P.ops[e]:
                    lasts.append(P.ops[e][-1])
            lasts += list(P.dma_last.values())
            for e in ("pe", "act", "dve", "pool", "sp"):
                o = P.op(e, lambda en: en.nop())
                for x in lasts:
                    if x is not o:
                        o.deps.add(x)
                        if not x.dma:
                            x.signal = True

        def dump(name, ap, key):
            if name not in dbg_out:
                return
            finals.append(dma("sp", dbg_out[name], ap, r=[key]))

        ident_bf = AR.get(BF16, [128]); ident_f = AR.get(F32, [128])
        ones_bf = AR.get(BF16, [128]); triu_bf = AR.get(BF16, [128]); tribias = AR.get(BF16, [256])
        hflag = AR.get(F32, [1]); vbias = AR.get(F32, [8, 16])
        sh1c = AR.get(F32, [8]); gs1c = AR.get(F32, [8])
        sccol = AR.get(F32, [8])
        dma("sp", ident_bf, ident_bf_d, w=["ident_bf"]); dma("sp", ident_f, ident_f_d, w=["ident_f"])
        dma("sp", triu_bf, triu_bf_d, w=["triu_bf"]); dma("sp", tribias, tribias_d, w=["tribias"])
        dma("sp", hflag, hflag_d, w=["hflag"])
        dma("sp", vbias, vbias_d.rearrange("p (a b) -> p a b", a=8), w=["vbias"])
        P.op("dve", lambda e: e.memset(ones_bf, 1.0), w=["ones_bf"])
        zt = AR.get(BF16, [2, D])
        P.op("dve", lambda e: e.memset(zt, 0.0), w=["zt"])

        AR.push()
        ccol = AR.get(F32, [8])
        abcol = AR.get(F32, [48]); n1g = AR.get(F32, [8])
        wA = [AR.get(F32, [8, 512]), AR.get(F32, [8, 512])]
        dma("sp", ccol, c_col, w=["ccol"]); dma("sp", abcol, ada_b_col, w=["abcol"]); dma("sp", n1g, n1g_col, w=["n1g"])
        P.op("act", lambda e: e.activation(out=sccol, in_=ccol, func=AF.Silu), r=["ccol"], w=["sccol"])
        adw = ada_w.rearrange("(k p) n -> p k n", p=128)
        for g in range(4):
            wb = wA[g % 2]; wk = ("wA", g % 2)
            dma("sp" if g % 2 == 0 else "act", wb, adw[:, :, g * 512:(g + 1) * 512], w=[wk])
            for j in range(4):
                for k in range(8):
                    P.op("pe", lambda e, j=j, k=k, g=g, wb=wb: e.matmul(pb[0][:, g * 4 + j:g * 4 + j + 1],
                         lhsT=wb[:, k, j * 128:(j + 1) * 128], rhs=sccol[:, k:k + 1], start=(k == 0), stop=(k == 7)),
                         r=[wk, "sccol"], w=[("pb0", g * 4 + j)])
        P.op("dve", lambda e: e.tensor_tensor(out=sh1c, in0=pb[0][:, 0:8], in1=abcol[:, 0:8], op=ALU.add),
             r=["pb0", "abcol"], w=["sh1c"])
        P.op("dve", lambda e: e.scalar_tensor_tensor(out=gs1c, in0=pb[0][:, 8:16], scalar=1.0, in1=abcol[:, 8:16],
             op0=ALU.add, op1=ALU.add), r=["pb0", "abcol"], w=["gs1c"])
        P.op("dve", lambda e: e.tensor_tensor(out=gs1c, in0=gs1c, in1=n1g, op=ALU.mult), r=["gs1c", "n1g"], w=["gs1c"])
        dump("sh1c", sh1c, "sh1c"); dump("gs1c", gs1c, "gs1c")
        barrier()
        AR.pop()
        off_base = AR.off
        mixT = AR.get(BF16, [8, T])
        off_mix = AR.off
        hT = AR.get(BF16, [8, LT])
        yT = AR.get(BF16, [8, T])
        win3 = w_in.rearrange("(k p) n -> p k n", p=128)

        def phaseB():
            AR.push()
            xt = [AR.get(F32, [D]), AR.get(F32, [D])]
            junk = AR.get(BF16, [D])
            xnb = [AR.get(BF16, [D]), AR.get(BF16, [D])]
            ss = AR.get(F32, [32]); lnv = AR.get(F32, [32]); rstd = AR.get(F32, [32])
            def tile_steps(i):
                steps = []
                S = lambda *a_, **k_: steps.append((a_, k_))
                b = i % 2
                src = x_pre if i < 16 else x_own
                r0 = (i % 16) * 128
                S("sp" if b == 0 else "act", (lambda e, b=b, r0=r0, src=src: e.dma_start(out=xt[b], in_=src[r0:r0 + 128, :])), w=[("xt", b)], dma=True)
                S("act", lambda e, b=b, i=i: e.activation(out=junk, in_=xt[b], func=AF.Square, accum_out=ss[:, i:i + 1]),
                     r=[("xt", b)], w=["junk", ("ss", i)])
                S("act", lambda e, i=i: e.activation(out=lnv[:, i:i + 1], in_=ss[:, i:i + 1], func=AF.Ln, scale=1.0 / D, bias=EPS),
                     r=[("ss", i)], w=[("lnv", i)])
                S("act", lambda e, i=i: e.activation(out=rstd[:, i:i + 1], in_=lnv[:, i:i + 1], func=AF.Exp, scale=-0.5),
                     r=[("lnv", i)], w=[("rstd", i)])
                S("dve", lambda e, b=b, i=i: e.tensor_scalar(out=xnb[b], in0=xt[b], scalar1=rstd[:, i:i + 1], scalar2=None,
                     op0=ALU.mult), r=[("xt", b), ("rstd", i)], w=[("xnb", b)])
                pbT = pbt[b]
                pk = "pbt%d" % b
                for c in range(8):
                    S("pe", lambda e, b=b, c=c, pbT=pbT: e.transpose(pbT[:, c * 128:(c + 1) * 128],
                         xnb[b][:, c * 128:(c + 1) * 128], ident_bf), r=[("xnb", b), "ident_bf"], w=[(pk, c)])
                for c in range(8):
                    if FLAGS.get("B_noevac") or c >= FLAGS.get("B_nevac", 8):
                        continue
                    dst = hT[:, c, i * 128:(i + 1) * 128]
                    if FLAGS.get("B_dst2"):
                        dst = junk[:, c * 128:(c + 1) * 128]
                    if FLAGS.get("B_evaccopy"):
                        srcp = pb[3][:, 0:128] if FLAGS.get("B_src2") else pbT[:, c * 128:(c + 1) * 128]
                        S("dve", lambda e, c=c, dst=dst, srcp=srcp: e.tensor_copy(out=dst, in_=srcp),
                             r=[pk if FLAGS.get("B_waitall") else (pk, c)], w=[("hT", i)])
                        continue
                    if c % 2 == 0 and not FLAGS.get("B_dveonly"):
                        S("act", lambda e, c=c, dst=dst, pbT=pbT: e.activation(out=dst, in_=pbT[:, c * 128:(c + 1) * 128],
                             func=AF.Identity, scale=gs1c[:, c:c + 1], bias=sh1c[:, c:c + 1]),
                             r=[pk, "gs1c", "sh1c"], w=[("hT", i)])
                    else:
                        S("dve", lambda e, c=c, dst=dst, pbT=pbT: e.tensor_scalar(out=dst, in0=pbT[:, c * 128:(c + 1) * 128],
                             scalar1=gs1c[:, c:c + 1], scalar2=sh1c[:, c:c + 1], op0=ALU.mult, op1=ALU.add),
                             r=[pk, "gs1c", "sh1c"], w=[("hT", i)])
                return steps

            modb = AR.get(F32, [4 * D]); scb = AR.get(F32, [8, 128]); n2gb = AR.get(F32, [D])
            wA = [AR.get(F32, [8, 256]), AR.get(F32, [8, 256])]
            adw = ada_w.rearrange("(k p) n -> p k n", p=128)
            dma("pool", modb, ada_b_row.partition_broadcast(128), w=["modb"])
            dma("pool", n2gb, n2g_row.partition_broadcast(128), w=["n2gb"])
            for k in range(8):
                P.op("dve", lambda e, k=k: e.tensor_copy(out=scb[:, k, :], in_=sccol[:, k:k + 1].to_broadcast([128, 128])),
                     r=["sccol"], w=[("scb", k)])

            def wload(gg):
                return [(("pool", (lambda e, gg=gg: e.dma_start(out=wA[gg % 2], in_=adw[:, :, 2 * D + gg * 256:2 * D + (gg + 1) * 256]))),
                         dict(w=[("wA", gg % 2)], dma=True))]

            def wmm(gg):
                st_ = []
                wb = wA[gg % 2]; wk = ("wA", gg % 2)
                pp = pb[4 + gg % 2]; pk = "pb%d" % (4 + gg % 2)
                for k in range(8):
                    st_.append((("pe", (lambda e, k=k, wb=wb, pp=pp: e.matmul(pp[:, 0:256], lhsT=scb[:, k, :], rhs=wb[:, k, :],
                               start=(k == 0), stop=(k == 7)))), dict(r=[wk, "scb"], w=[pk])))
                st_.append((("dve", (lambda e, gg=gg, pp=pp: e.tensor_tensor(out=modb[:, gg * 256:(gg + 1) * 256],
                           in0=pp[:, 0:256], in1=modb[:, gg * 256:(gg + 1) * 256], op=ALU.add))), dict(r=[pk, "modb"], w=["modb"])))
                return st_

            import itertools
            ch0 = []; ch1 = [None] * 11; ch2 = wload(0) + wload(1) + [None] * 6
            for i2 in range(0, 32, 2):
                ch0 += tile_steps(i2); ch1 += tile_steps(i2 + 1)
                gg = i2 // 2
                grp = wmm(gg) + (wload(gg + 2) if gg + 2 < 16 else [])
                ch2 += grp + [None] * max(0, 22 - len(grp))
            for xs in itertools.zip_longest(ch0, ch1, ch2):
                for x in xs:
                    if x is not None:
                        P.op(*x[0], **x[1])
            gs2b_ = modb[:, 2 * D:3 * D]
            P.op("dve", lambda e: e.scalar_tensor_tensor(out=gs2b_, in0=gs2b_, scalar=1.0, in1=n2gb, op0=ALU.add, op1=ALU.mult),
                 r=["modb", "n2gb"], w=["modb"])
            dma("sp", modb_d, modb, r=["modb"], w=["modb_scr"])
            barrier()
            AR.pop()

        def phaseC():
            AR.push()
            cw = AR.get(F32, [8, 4]); cb = AR.get(F32, [8]); nba = AR.get(F32, [8]); nbx = AR.get(F32, [8])
            lam = AR.get(F32, [8]); s1 = AR.get(F32, [8]); s2 = AR.get(F32, [8])
            wga = AR.get(BF16, [8, 128]); wgx = AR.get(BF16, [8, 128])
            wxr = AR.get(BF16, [8, 512]); wgr = AR.get(BF16, [8, 512])
            BUF = []

            def mkset():
                d_ = dict(
                    xb=AR.get(F32, [515]), hist=AR.get(F32, [3]), xc=AR.get(F32, [512]), xcb=AR.get(BF16, [512]),
                    er=AR.get(F32, [512]), eg=AR.get(F32, [512]), aa=AR.get(F32, [512]), a2=AR.get(F32, [512]),
                    hb=AR.get(F32, [512]), carry=AR.get(F32, [1]), fx=AR.get(F32, [1]))
                d_["uu"] = d_["er"]; d_["gz"] = d_["er"]
                return d_
            save_ = AR.off
            AR.off = off_base
            BUF.append(mkset()); BUF.append(mkset())
            assert AR.off <= off_mix
            AR.off = save_
            BUF.append(mkset()); BUF.append(mkset())
            PB = [(pb[0], "pb0"), (pb[1], "pb1"), (pb[2], "pb2"), (pb[3], "pb3"), (pb[4], "pb4"), (pb[5], "pb5"),
                  (pbt[0][:, :].bitcast(F32), "pbt0"), (pbt[1][:, :].bitcast(F32), "pbt1")]
            dma("sp", cw, convw_col.rearrange("p (a b) -> p a b", a=8), w=["cw"]); dma("sp", cb, convb_col, w=["cb"])
            dma("sp", nba, brga_col, w=["nba"]); dma("sp", nbx, brgx_col, w=["nbx"]); dma("sp", lam, lam_col, w=["lam"])
            dma("pool", wga, w_rg_a.rearrange("n w v -> w n v"), w=["wga"])
            dma("pool", wgx, w_rg_x.rearrange("n w v -> w n v"), w=["wgx"])
            P.op("dve", lambda e: e.tensor_scalar(out=nba, in0=nba, scalar1=-1.0, scalar2=None, op0=ALU.mult), r=["nba"], w=["nba"])
            P.op("dve", lambda e: e.tensor_scalar(out=nbx, in0=nbx, scalar1=-1.0, scalar2=None, op0=ALU.mult), r=["nbx"], w=["nbx"])
            P.op("act", lambda e: e.activation(out=s1, in_=lam, func=AF.Exp, scale=-1.0), r=["lam"], w=["s1"])
            P.op("act", lambda e: e.activation(out=s1, in_=s1, func=AF.Ln, bias=1.0), r=["s1"], w=["s1"])
            P.op("dve", lambda e: e.tensor_scalar(out=s2, in0=s1, scalar1=-16.0, scalar2=None, op0=ALU.mult), r=["s1"], w=["s2"])
            P.op("dve", lambda e: e.tensor_scalar(out=s1, in0=s1, scalar1=-8.0, scalar2=None, op0=ALU.mult), r=["s1", "s2"], w=["s1"])
            GC = 1.5957691216057308

            def piece(c, j, par):
                B = BUF[par]
                steps = []
                S = lambda *a_, **k_: steps.append((a_, k_))
                cc = c % 4
                K = lambda n: (n, par)
                xc, xcb, er, eg, aa, a2, uu, gz, carry, fx = (B[n] for n in ("xc", "xcb", "er", "eg", "aa", "a2", "uu", "gz", "carry", "fx"))
                mm, ig = a2, eg
                xps, xk = PB[2 * par]
                for k in range(8):
                    S("pe", lambda e, k=k, j=j, cc=cc, xps=xps: e.matmul(xps[:, :],
                         lhsT=wxr[:, k, cc * 128:(cc + 1) * 128], rhs=hT[:, k, j * 512:(j + 1) * 512],
                         start=(k == 0), stop=(k == 7)), r=["wxr", "hT"], w=[xk])
                xb = B["xb"]; xbk = ("xb", par); hist = B["hist"]; hk_ = ("hist", par)
                if j == 0:
                    S("dve", lambda e, xb=xb: e.memset(xb[:, 0:3], 0.0), w=[xbk])
                elif j == 4:
                    S("dve", lambda e, xb=xb, hist=hist: e.tensor_scalar(out=xb[:, 0:3], in0=hist,
                         scalar1=hflag[:, 0:1], scalar2=None, op0=ALU.mult), r=[hk_, "hflag"], w=[xbk])
                else:
                    S("dve", lambda e, xb=xb, hist=hist: e.tensor_copy(out=xb[:, 0:3], in_=hist), r=[hk_], w=[xbk])
                S("dve", lambda e, xb=xb, xps=xps: e.tensor_copy(out=xb[:, 3:515], in_=xps[:, :]),
                     r=[xk, xbk], w=[xbk])
                S("dve", lambda e, xb=xb, c=c: e.tensor_scalar(out=xc, in0=xb[:, 3:515], scalar1=cw[:, c, 0:1],
                     scalar2=cb[:, c:c + 1], op0=ALU.mult, op1=ALU.add), r=[xbk, "cw", "cb"], w=[K("xc")])
                for i in (1, 2, 3):
                    S("dve", lambda e, xb=xb, c=c, i=i: e.scalar_tensor_tensor(out=xc, in0=xb[:, 3 - i:515 - i],
                         scalar=cw[:, c, i:i + 1], in1=xc, op0=ALU.mult, op1=ALU.add), r=[xbk, "cw", K("xc")], w=[K("xc")])
                S("dve", lambda e, xb=xb, hist=hist: e.tensor_copy(out=hist, in_=xb[:, 512:515]), r=[xbk], w=[hk_])
                S("pool", lambda e: e.tensor_copy(out=xcb, in_=xc), r=[K("xc")], w=[K("xcb")])
                rps, rk = PB[2 * par + 1]; gps, gk = PB[2 * par]
                S("pe", lambda e, c=c, rps=rps: e.matmul(rps[:, :], lhsT=wga[:, c, :], rhs=xcb, start=True, stop=True),
                     r=["wga", K("xcb")], w=[rk])
                S("pe", lambda e, c=c, gps=gps: e.matmul(gps[:, :], lhsT=wgx[:, c, :], rhs=xcb, start=True, stop=True),
                     r=["wgx", K("xcb")], w=[gk])
                S("act", lambda e, c=c, rps=rps: e.activation(out=er, in_=rps[:, :], func=AF.Exp, scale=-1.0,
                     bias=nba[:, c:c + 1]), r=[rk, "nba"], w=[K("er")])
                S("act", lambda e: e.activation(out=er, in_=er, func=AF.Ln, bias=1.0), r=[K("er")], w=[K("er")])
                S("act", lambda e: e.activation(out=er, in_=er, func=AF.Exp, scale=-1.0), r=[K("er")], w=[K("er")])
                S("act", lambda e, c=c, gps=gps: e.activation(out=eg, in_=gps[:, :], func=AF.Exp, scale=-1.0,
                     bias=nbx[:, c:c + 1]), r=[gk, "nbx"], w=[K("eg")])
                S("act", lambda e: e.activation(out=eg, in_=eg, func=AF.Ln, bias=1.0), r=[K("eg")], w=[K("eg")])
                S("act", lambda e: e.activation(out=eg, in_=eg, func=AF.Exp, scale=-1.0), r=[K("eg")], w=[K("eg")])
                S("act", lambda e, c=c: e.activation(out=aa, in_=er, func=AF.Exp, scale=s1[:, c:c + 1]), r=[K("er"), "s1"], w=[K("aa")])
                S("act", lambda e, c=c: e.activation(out=a2, in_=er, func=AF.Exp, scale=s2[:, c:c + 1]), r=[K("er"), "s2"], w=[K("a2")])
                S("pool", lambda e: e.tensor_scalar(out=mm, in0=a2, scalar1=-1.0, scalar2=1.0, op0=ALU.mult, op1=ALU.add),
                     r=[K("a2")], w=[K("a2")])
                S("act", lambda e: e.activation(out=mm, in_=mm, func=AF.Ln), r=[K("a2")], w=[K("a2")])
                S("act", lambda e: e.activation(out=mm, in_=mm, func=AF.Exp, scale=0.5), r=[K("a2")], w=[K("a2")])
                if j == 0:
                    S("dve", lambda e: e.memset(mm[:, 0:1], 1.0), r=[K("a2")], w=[K("a2")])
                elif j == 4:
                    S("dve", lambda e: e.tensor_scalar(out=fx, in0=mm[:, 0:1], scalar1=-1.0, scalar2=hflag[:, 0:1],
                         op0=ALU.add, op1=ALU.mult), r=[K("a2"), "hflag"], w=[K("fx")])
                    S("dve", lambda e: e.tensor_scalar(out=mm[:, 0:1], in0=fx, scalar1=1.0, scalar2=None, op0=ALU.add),
                         r=[K("fx"), K("a2")], w=[K("a2")])
                S("pool", lambda e: e.tensor_tensor(out=uu, in0=mm, in1=ig, op=ALU.mult), r=[K("a2"), K("eg")], w=[K("er")])
                S("pool", lambda e: e.tensor_tensor(out=uu, in0=uu, in1=xc, op=ALU.mult), r=[K("er"), K("xc")], w=[K("er")])
                hdst = B["hb"]; hk = ("hb", par)
                if j == 0:
                    S("dve", lambda e, hdst=hdst: e.tensor_tensor_scan(out=hdst, data0=aa, data1=uu, initial=0.0,
                         op0=ALU.mult, op1=ALU.add), r=[K("aa"), K("er")], w=[hk])
                else:
                    S("dve", lambda e, hdst=hdst: e.tensor_tensor_scan(out=hdst, data0=aa, data1=uu, initial=carry,
                         op0=ALU.mult, op1=ALU.add), r=[K("aa"), K("er"), K("carry")], w=[hk])
                if j == 3:
                    S("dve", lambda e, hdst=hdst: e.tensor_scalar(out=carry, in0=hdst[:, 511:512], scalar1=hflag[:, 0:1],
                         scalar2=None, op0=ALU.mult), r=[hk, "hflag"], w=[K("carry")])
                elif j < 7:
                    S("dve", lambda e, hdst=hdst: e.tensor_copy(out=carry, in_=hdst[:, 511:512]), r=[hk], w=[K("carry")])
                if j >= 4:
                    jj = j - 4
                    gps2, gk2 = PB[2 * par]
                    for k in range(8):
                        S("pe", lambda e, k=k, jj=jj, cc=cc, gps2=gps2: e.matmul(gps2[:, :],
                             lhsT=wgr[:, k, cc * 128:(cc + 1) * 128], rhs=hT[:, k, T + jj * 512:T + (jj + 1) * 512],
                             start=(k == 0), stop=(k == 7)), r=["wgr", "hT"], w=[gk2])
                    S("act", lambda e, gps2=gps2: e.activation(out=gz, in_=gps2[:, :], func=AF.Square), r=[gk2], w=[K("er")])
                    S("pool", lambda e: e.tensor_scalar(out=gz, in0=gz, scalar1=0.044715, scalar2=1.0, op0=ALU.mult, op1=ALU.add),
                         r=[K("er")], w=[K("er")])
                    S("dve", lambda e, gps2=gps2: e.tensor_tensor(out=gz, in0=gz, in1=gps2[:, :], op=ALU.mult), r=[K("er"), gk2], w=[K("er")])
                    S("act", lambda e: e.activation(out=gz, in_=gz, func=AF.Exp, scale=-GC), r=[K("er")], w=[K("er")])
                    S("act", lambda e: e.activation(out=gz, in_=gz, func=AF.Ln, bias=1.0), r=[K("er")], w=[K("er")])
                    S("act", lambda e: e.activation(out=gz, in_=gz, func=AF.Exp, scale=-1.0), r=[K("er")], w=[K("er")])
                    S("dve", lambda e, gps2=gps2: e.tensor_tensor(out=gz, in0=gz, in1=gps2[:, :], op=ALU.mult), r=[K("er"), gk2], w=[K("er")])
                    S("dve", lambda e, jj=jj, c=c, hdst=hdst: e.tensor_tensor(out=yT[:, c, jj * 512:(jj + 1) * 512],
                         in0=hdst, in1=gz, op=ALU.mult), r=[hk, K("er")], w=[("yT", c)])
                return steps

            import itertools
            xg2 = xg_d[0:NTOT, :].rearrange("(e p r) d -> e p r d", p=128, r=2)
            for e_ in range(NTOT // 256):
                dma("sp", xg2[e_], zt, r=["zt"], w=["Xg"])
            for cg in range(2):
                dma("pool", wxr, win3[:, :, cg * 512:(cg + 1) * 512], w=["wxr"])
                dma("pool", wgr, win3[:, :, D + cg * 512:D + (cg + 1) * 512], w=["wgr"])
                c0 = cg * 4
                sts = []
                for q in range(4):
                    lst = [None] * (q * 12)
                    for j in range(8):
                        lst += piece(c0 + q, j, q)
                    sts.append(lst)
                for xs in itertools.zip_longest(*sts):
                    for x in xs:
                        if x is not None:
                            P.op(*x[0], **x[1])
            barrier()
            AR.pop()

        def merge(g, first):
            AR.push()
            wbr = [AR.get(BF16, [8, 512]), AR.get(BF16, [8, 512])]
            wgl = [AR.get(BF16, [8, 512]), AR.get(BF16, [8, 512])]
            bgc = AR.get(F32, [16]); sg = AR.get(F32, [512]); tmp = AR.get(F32, [512])
            dma("sp", bgc, bgate_col, w=["bgc"])
            wb3 = w_branch[g].rearrange("(k p) n -> p k n", p=128)
            for grp in range(2):
                dma("pool", wbr[grp], wb3[:, :, grp * 512:(grp + 1) * 512], w=[("wbr", grp)])
                c0 = 5 * D + g * D + grp * 512
                dma("pool", wgl[grp], win3[:, :, c0:c0 + 512], w=[("wgl", grp)])
            for m in range(8):
                grp, mm = m // 4, m % 4
                for pc in range(4):
                    zps = pb[pc % 2]; zk = "pb%d" % (pc % 2); gps = pb[2 + pc % 2]; gk = "pb%d" % (2 + pc % 2)
                    for k in range(8):
                        P.op("pe", lambda e, k=k, pc=pc, grp=grp, mm=mm, zps=zps: e.matmul(zps[:, :],
                             lhsT=wbr[grp][:, k, mm * 128:(mm + 1) * 128], rhs=yT[:, k, pc * 512:(pc + 1) * 512],
                             start=(k == 0), stop=(k == 7)), r=[("wbr", grp), "yT"], w=[zk])
                    for k in range(8):
                        P.op("pe", lambda e, k=k, pc=pc, grp=grp, mm=mm, gps=gps: e.matmul(gps[:, :],
                             lhsT=wgl[grp][:, k, mm * 128:(mm + 1) * 128], rhs=hT[:, k, T + pc * 512:T + (pc + 1) * 512],
                             start=(k == 0), stop=(k == 7)), r=[("wgl", grp), "hT"], w=[gk])
                    P.op("act", lambda e, gps=gps, m=m: e.activation(out=sg, in_=gps[:, :], func=AF.Sigmoid,
                         bias=bgc[:, g * 8 + m:g * 8 + m + 1]), r=[gk, "bgc"], w=["sg"])
                    dst = mixT[:, m, pc * 512:(pc + 1) * 512]
                    if first:
                        P.op("dve", lambda e, zps=zps, dst=dst: e.tensor_tensor(out=dst, in0=zps[:, :], in1=sg, op=ALU.mult),
                             r=[zk, "sg"], w=[("mixT", m)])
                    else:
                        P.op("dve", lambda e, zps=zps: e.tensor_tensor(out=tmp, in0=zps[:, :], in1=sg, op=ALU.mult),
                             r=[zk, "sg"], w=["tmp"])
                        P.op("dve", lambda e, dst=dst: e.tensor_tensor(out=dst, in0=dst, in1=tmp, op=ALU.add),
                             r=["tmp", ("mixT", m)], w=[("mixT", m)])
            barrier()
            AR.pop()

        def phaseD():
            merge(0, True)

        def phaseF():
            merge(1, False)

        def phaseE():
            AR.push()
            SCALE = 128.0 ** -0.5
            C32 = AR.get(BF16, [LT], parts=32); S32 = AR.get(BF16, [LT], parts=32)
            qgc = AR.get(F32, [1]); kgc = AR.get(F32, [1]); rmf = AR.get(F32, [32]); fq = AR.get(F32, [2], parts=32)
            dma("sp", qgc, qg_col, w=["qgc"]); dma("sp", kgc, kg_col, w=["kgc"])
            dma("sp", rmf, rm_f_d, w=["rmf"]); dma("sp", fq, freq_d, w=["fq"])
            AR.push()
            posi = AR.get(I32, [1024], parts=32); ang = AR.get(F32, [1024], parts=32); tq = AR.get(F32, [1024], parts=32)
            ki = AR.get(I32, [1024], parts=32); kf = AR.get(F32, [1024], parts=32); rr_ = AR.get(F32, [1024], parts=32)
            ones32 = AR.get(F32, [1], parts=32)
            P.op("dve", lambda e: e.memset(ones32, 1.0), w=["ones32"])
            TWO_PI = 6.283185307179586
            C1 = 6.28125
            C2 = TWO_PI - C1
            for pc in range(4):
                sl = slice(pc * 1024, (pc + 1) * 1024)
                dma("sp", posi, pos[:, sl].partition_broadcast(32), w=["posi"])
                P.op("dve", lambda e: e.tensor_copy(out=ang, in_=posi), r=["posi"], w=["ang"])
                P.op("dve", lambda e: e.tensor_scalar(out=ang, in0=ang, scalar1=fq[:, 0:1], scalar2=None, op0=ALU.mult),
                     r=["ang", "fq"], w=["ang"])
                for which in (0, 1):
                    off = 0.0 if which == 0 else np.pi / 2
                    P.op("dve", lambda e, off=off: e.tensor_scalar(out=tq, in0=ang, scalar1=float(off), scalar2=1.0 / TWO_PI,
                         op0=ALU.add, op1=ALU.mult), r=["ang"], w=["tq"])
                    P.op("dve", lambda e: e.tensor_copy(out=ki, in_=tq), r=["tq"], w=["ki"])
                    P.op("dve", lambda e: e.tensor_copy(out=kf, in_=ki), r=["ki"], w=["kf"])
                    P.op("dve", lambda e, off=off: e.scalar_tensor_tensor(out=rr_, in0=kf, scalar=-C1, in1=ang,
                         op0=ALU.mult, op1=ALU.add), r=["kf", "ang"], w=["rr"])
                    P.op("dve", lambda e, off=off: e.scalar_tensor_tensor(out=rr_, in0=kf, scalar=-C2, in1=rr_,
                         op0=ALU.mult, op1=ALU.add), r=["kf", "rr"], w=["rr"])
                    P.op("dve", lambda e, off=off: e.tensor_scalar(out=rr_, in0=rr_, scalar1=float(off), scalar2=3.14159,
                         op0=ALU.add, op1=ALU.min), r=["rr"], w=["rr"])
                    P.op("dve", lambda e: e.tensor_scalar(out=rr_, in0=rr_, scalar1=-3.14159, scalar2=None, op0=ALU.max),
                         r=["rr"], w=["rr"])
                    if which == 0:
                        P.op("act", lambda e, sl=sl: e.activation(out=S32[:, sl], in_=rr_, func=AF.Sin, scale=fq[:, 1:2]),
                             r=["rr", "fq"], w=[("S32", pc)])
                    else:
                        P.op("act", lambda e, sl=sl: e.activation(out=C32[:, sl], in_=rr_, func=AF.Sin), r=["rr"], w=[("C32", pc)])
            barrier()
            AR.pop()
            kT = AR.get(BF16, [LT]); vb_ = AR.get(BF16, [32, 128]); qT = AR.get(BF16, [T])
            mbT = AR.get(BF16, [T], parts=16); mball = AR.get(BF16, [16, 16])
            wq = AR.get(BF16, [8, 128]); wk_ = AR.get(BF16, [8, 128]); wv = AR.get(BF16, [8, 128])
            kraw2 = [AR.get(F32, [512]), AR.get(F32, [512])]; sq2 = [AR.get(BF16, [512]), AR.get(BF16, [512])]
            rstd = AR.get(F32, [512]); lnr = rstd
            t2 = AR.get(F32, [512], parts=32)
            kmT = AR.get(F32, [16])
            gb = AR.get(F32, [16]); mx = AR.get(F32, [8]); sel = AR.get(F32, [16])
            pT = [AR.get(BF16, [512]), AR.get(BF16, [512]), AR.get(BF16, [512])]
            rl = AR.get(F32, [512])

            pbtF = [pbt[0][:, :].bitcast(F32), pbt[1][:, :].bitcast(F32)]
            OH = AR.get(BF16, [16, 128], parts=16)
            for n_ in range(16):
                P.op("dve", lambda e, n_=n_: e.tensor_copy(out=OH[:, n_, :], in_=ident_bf[0:16, n_:n_ + 1].to_broadcast([16, 128])),
                     r=["ident_bf"], w=[("OH", n_)])

            def piece_info(hd, j):
                if j < 8:
                    return wk_, kgc, "kgc", j * 512, kT[:, j * 512:(j + 1) * 512]
                jj = j - 8
                return wq, qgc, "qgc", T + jj * 512, qT[:, jj * 512:(jj + 1) * 512]

            def s1_proj(hd, j):
                wt, gcol, gkey, tok0, dstT = piece_info(hd, j)
                ps = pb[j % 2]; pk = "pb%d" % (j % 2)
                for k in range(8):
                    P.op("pe", lambda e, k=k, ps=ps, wt=wt, tok0=tok0: e.matmul(ps[:, :], lhsT=wt[:, k, :], rhs=hT[:, k, tok0:tok0 + 512],
                         start=(k == 0), stop=(k == 7)), r=["wqkv", "hT"], w=[pk])

            def s1_evac(hd, j):
                ps = pb[j % 2]; pk = "pb%d" % (j % 2)
                kr = kraw2[j % 2]; krk = ("kraw", j % 2); sq_ = sq2[j % 2]; sqk = ("sq", j % 2)
                P.op("act", lambda e, ps=ps, kr=kr: e.activation(out=kr, in_=ps[:, :], func=AF.Copy), r=[pk], w=[krk])
                P.op("act", lambda e, ps=ps, sq_=sq_: e.activation(out=sq_, in_=ps[:, :], func=AF.Square), r=[pk], w=[sqk])

            def s2a(hd, j):
                wt, gcol, gkey, tok0, dstT = piece_info(hd, j)
                kr = kraw2[j % 2]; krk = ("kraw", j % 2); sq_ = sq2[j % 2]; sqk = ("sq", j % 2)
                sp_ = pb[2 + j % 2]; sk = "pb%d" % (2 + j % 2)
                P.op("pe", lambda e, sp_=sp_, sq_=sq_: e.matmul(sp_[:, :], lhsT=ones_bf, rhs=sq_, start=True, stop=True),
                     r=["ones_bf", sqk], w=[sk])
                P.op("act", lambda e, sp_=sp_: e.activation(out=lnr, in_=sp_[:, :], func=AF.Ln, scale=1.0 / 128, bias=EPS),
                     r=[sk], w=["rstd"])
                P.op("act", lambda e: e.activation(out=rstd, in_=lnr, func=AF.Exp, scale=-0.5), r=["rstd"], w=["rstd"])
                P.op("dve", lambda e, kr=kr, gcol=gcol: e.scalar_tensor_tensor(out=kr, in0=kr, scalar=gcol[:, 0:1], in1=rstd,
                     op0=ALU.mult, op1=ALU.mult), r=[krk, gkey, "rstd"], w=[krk])

            def s2b(hd, j):
                wt, gcol, gkey, tok0, dstT = piece_info(hd, j)
                kr = kraw2[j % 2]; krk = ("kraw", j % 2)
                rp = pb[4 + j % 2]; rk = "pb%d" % (4 + j % 2)
                P.op("pe", lambda e, rp=rp, kr=kr: e.matmul(rp[0:32, :], lhsT=rmf, rhs=kr, start=True, stop=True),
                     r=["rmf", krk], w=[rk])
                P.op("dve", lambda e, rp=rp, tok0=tok0: e.tensor_tensor(out=t2, in0=rp[0:32, :], in1=S32[:, tok0:tok0 + 512], op=ALU.mult),
                     r=[rk, "S32"], w=["t2"])
                P.op("dve", lambda e, kr=kr, tok0=tok0: e.tensor_tensor(out=kr[0:32, :], in0=kr[0:32, :], in1=C32[:, tok0:tok0 + 512],
                     op=ALU.mult), r=[krk, "C32", rk], w=[krk])
                P.op("dve", lambda e, kr=kr: e.tensor_tensor(out=kr[0:32, :], in0=kr[0:32, :], in1=t2, op=ALU.add), r=["t2", krk], w=[krk])
                P.op("act", lambda e, kr=kr, dstT=dstT: e.activation(out=dstT, in_=kr, func=AF.Copy), r=[krk], w=["qkT"])
                if j < 8:
                    P.op("dve", lambda e, j=j, kr=kr: e.tensor_reduce(out=kmT[:, 2 * j:2 * j + 2],
                         in_=kr.rearrange("p (b t) -> p b t", b=2), axis=AX.X, op=ALU.add), r=[krk], w=["kmT"])
                else:
                    jj = j - 8
                    gp = pb[4 + j % 2]; gk = rk
                    for qt in range(4):
                        P.op("pe", lambda e, qt=qt, gp=gp, kr=kr: e.matmul(gp[:, 64 + qt * 16:64 + (qt + 1) * 16],
                             lhsT=kr[:, qt * 128:(qt + 1) * 128], rhs=kmT, start=True, stop=True),
                             r=[krk, "kmT"], w=[gk])
                    for qt in range(4):
                        i = jj * 4 + qt
                        P.op("dve", lambda e, qt=qt, i=i, gp=gp: e.tensor_tensor(out=gb, in0=gp[:, 64 + qt * 16:64 + (qt + 1) * 16],
                             in1=vbias[:, i // 2, :], op=ALU.add), r=[gk, "vbias"], w=["gb"])
                        P.op("dve", lambda e: e.max(out=mx, in_=gb), r=["gb"], w=["mx"])
                        P.op("dve", lambda e: e.tensor_scalar(out=sel, in0=gb, scalar1=mx[:, 2:3], scalar2=-1.0,
                             op0=ALU.is_ge, op1=ALU.add), r=["gb", "mx"], w=["sel"])
                        P.op("dve", lambda e, i=i: e.scalar_tensor_tensor(out=mball[:, i, :], in0=sel, scalar=-NEG,
                             in1=vbias[:, i // 2, :], op0=ALU.mult, op1=ALU.min), r=["sel", "vbias"], w=["mball"])

            def v_group(hd, tg):
                vps = pbtF[tg % 2]; vk = "pbt%d" % (tg % 2)
                for tt in range(4):
                    tile_i = tg * 4 + tt
                    for k in range(8):
                        P.op("pe", lambda e, k=k, tt=tt, tile_i=tile_i, vps=vps: e.matmul(vps[:, tt * 128:(tt + 1) * 128],
                             lhsT=hT[:, k, tile_i * 128:(tile_i + 1) * 128], rhs=wv[:, k, :], start=(k == 0), stop=(k == 7)),
                             r=["wqkv", "hT"], w=[vk])
                P.op("act", lambda e, tg=tg, vps=vps: e.activation(out=vb_[:, tg * 4:(tg + 1) * 4, :],
                     in_=vps.rearrange("p (a b) -> p a b", a=4), func=AF.Copy), r=[vk], w=["vb"])

            for hd in range(8):
                dma("pool", wq, win3[:, :, 2 * D + hd * 128:2 * D + (hd + 1) * 128], w=["wqkv"])
                dma("pool", wk_, win3[:, :, 3 * D + hd * 128:3 * D + (hd + 1) * 128], w=["wqkv"])
                dma("pool", wv, win3[:, :, 4 * D + hd * 128:4 * D + (hd + 1) * 128], w=["wqkv"])
                NP_ = 12
                s1_proj(hd, 0)
                s1_evac(hd, 0)
                for j in range(NP_):
                    if j + 1 < NP_:
                        s1_proj(hd, j + 1)
                    if j < 8:
                        v_group(hd, j)
                    if j >= 1:
                        s2b(hd, j - 1)
                    if j + 1 < NP_:
                        s1_evac(hd, j + 1)
                    s2a(hd, j)
                s2b(hd, NP_ - 1)
                for half in range(2):
                    tp = pbt[half]; tk = "pbt%d" % half
                    for ii in range(8):
                        i = half * 8 + ii
                        P.op("pe", lambda e, i=i, ii=ii, tp=tp: e.transpose(tp[0:16, ii * 128:(ii + 1) * 128], mball[:, i, :], ident_bf),
                             r=["mball", "ident_bf"], w=[tk])
                    P.op("dve", lambda e, half=half, tp=tp: e.tensor_copy(out=mbT[:, half * 1024:(half + 1) * 1024], in_=tp[0:16, :]),
                         r=[tk], w=["mbT"])
                for g4 in range(4):
                    qbA = 8 + 2 * g4
                    q0 = g4 * 512
                    ops_ = pb[2 + g4 % 2]; ok = "pb%d" % (2 + g4 % 2)
                    lps = pb[4 + g4 % 2]; lk = "pb%d" % (4 + g4 % 2)
                    chunks = [("A0", 2 * qbA, 0, 512), ("A1", 2 * qbA + 1, 128, 384),
                              ("B0", 2 * qbA + 2, 256, 256), ("B1", 2 * qbA + 3, 384, 128)]
                    chunks += [("P", c, 0, 512) for c in range(2 * qbA)]

                    def scores_pe(ci):
                        kind, kc, co, nq = chunks[ci]
                        sps = pb[ci % 2]; sk = "pb%d" % (ci % 2)
                        qs = q0 + co
                        P.op("pe", lambda e, kc=kc, qs=qs, nq=nq, sps=sps: e.matmul(sps[:, 0:nq], lhsT=kT[:, kc * 128:(kc + 1) * 128],
                             rhs=qT[:, qs:qs + nq], start=True, stop=False), r=["qkT"], w=[sk])
                        if kind == "P":
                            n = kc // 2
                            P.op("pe", lambda e, n=n, qs=qs, nq=nq, sps=sps: e.matmul(sps[:, 0:nq],
                                 lhsT=OH[:, n, :], rhs=mbT[:, qs:qs + nq],
                                 start=False, stop=True), r=["mbT", "OH"], w=[sk])
                        elif kind in ("A0", "A1"):
                            ntri = 256 if kind == "A0" else 128
                            P.op("pe", lambda e, ntri=ntri, sps=sps: e.matmul(sps[:, 0:ntri], lhsT=ident_bf, rhs=tribias[:, 0:ntri],
                                 start=False, stop=False), r=["tribias", "ident_bf"], w=[sk])
                            P.op("pe", lambda e, ntri=ntri, nq=nq, sps=sps, qbA=qbA, q0=q0: e.matmul(sps[:, ntri:nq],
                                 lhsT=OH[:, qbA, :], rhs=mbT[:, q0 + 256:q0 + 512],
                                 start=False, stop=True), r=["mbT", "OH"], w=[sk])
                        else:
                            P.op("pe", lambda e, nq=nq, sps=sps: e.matmul(sps[:, 0:nq], lhsT=ident_bf, rhs=tribias[:, 0:nq],
                                 start=False, stop=True), r=["tribias", "ident_bf"], w=[sk])

                    nch = len(chunks)
                    scores_pe(0)
                    for ci in range(nch):
                        kind, kc, co, nq = chunks[ci]
                        sps = pb[ci % 2]; sk = "pb%d" % (ci % 2)
                        ptile = pT[ci % 3]; ptk = ("pT", ci % 3)
                        P.op("act", lambda e, nq=nq, sps=sps, ptile=ptile: e.activation(out=ptile[:, 0:nq], in_=sps[:, 0:nq],
                             func=AF.Exp, scale=SCALE), r=[sk], w=[ptk])
                        if ci + 1 < nch:
                            scores_pe(ci + 1)
                        first, last = (ci == 0), (ci == nch - 1)
                        P.op("pe", lambda e, kc=kc, nq=nq, co=co, ptile=ptile, ops_=ops_, first=first, last=last: e.matmul(
                             ops_[:, co:co + nq], lhsT=vb_[:, kc, :], rhs=ptile[:, 0:nq], start=first, stop=last),
                             r=["vb", ptk], w=[ok])
                        P.op("pe", lambda e, nq=nq, co=co, ptile=ptile, lps=lps, first=first, last=last: e.matmul(
                             lps[:, co:co + nq], lhsT=ones_bf, rhs=ptile[:, 0:nq], start=first, stop=last),
                             r=["ones_bf", ptk], w=[lk])
                    P.op("dve", lambda e, lps=lps: e.reciprocal(out=rl, in_=lps[:, :]), r=[lk], w=["rl"])
                    P.op("dve", lambda e, hd=hd, q0=q0, ops_=ops_: e.tensor_tensor(out=yT[:, hd, q0:q0 + 512], in0=ops_[:, :],
                         in1=rl, op=ALU.mult), r=[ok, "rl"], w=[("yT", hd)])
            barrier()
            AR.pop()

        G = {}

        def phaseG():
            AR.off = off_mix
            AR.push()
            modb = AR.get(F32, [4 * D])
            g1b, sh2b, gs2b, g2b = modb[:, 0:D], modb[:, D:2 * D], modb[:, 2 * D:3 * D], modb[:, 3 * D:4 * D]
            G["g2b"] = g2b
            gates_all = AR.get(F32, [16, 4])
            G["off_keep"] = AR.off
            dma("sp", modb, modb_d, r=["modb_scr"], w=["modb"])
            wout = AR.get(BF16, [8, D])
            xt = [AR.get(F32, [D]), AR.get(F32, [D])]; x1t = [AR.get(F32, [D]), AR.get(F32, [D])]
            h2f = AR.get(F32, [D]); junk = AR.get(BF16, [D])
            h2b_all = AR.get(BF16, [16, D]); h2T = AR.get(F32, [8, 128]); wrf = AR.get(F32, [8, NE])
            brb = AR.get(F32, [NE]); lg = AR.get(F32, [NE]); top8 = AR.get(F32, [8]); idx8 = AR.get(U32, [8])
            ss = AR.get(F32, [16]); lnv = AR.get(F32, [16]); rs2 = AR.get(F32, [16])
            negv = AR.get(F32, [1]); e4 = AR.get(F32, [4]); se = AR.get(F32, [1]); rse = AR.get(F32, [1])
            idxf_all = AR.get(F32, [16, 4]); A_all = AR.get(BF16, [16, NE])
            dest_all = dest_sb[:, :, :]; io32 = AR.get(F32, [NE])
            eq = AR.get(F32, [NE]); s1_ = AR.get(F32, [1]); d1 = AR.get(F32, [1]); ov = AR.get(F32, [1])
            zf = AR.get(F32, [D]); pcol = AR.get(F32, [2]); tt_ = AR.get(F32, [1])
            cnt_b = AR.get(F32, [NE]); heavy_b = AR.get(F32, [NE]); hs_b = AR.get(F32, [NE]); one32 = AR.get(F32, [NE])
            hsk = AR.get(F32, [1]); lt_ = AR.get(F32, [1]); okh = AR.get(F32, [1]); dh_ = AR.get(F32, [1]); ehf = AR.get(F32, [1]); k128 = AR.get(F32, [8]); tmp8 = AR.get(F32, [8]); eq2 = AR.get(F32, [NE])
            eq4 = AR.get(F32, [4, NE]); eq4b = AR.get(F32, [4, NE]); hsk4 = AR.get(F32, [4]); s14 = AR.get(F32, [4]); lt4 = AR.get(F32, [4])
            d4 = AR.get(F32, [4]); dh4 = AR.get(F32, [4]); ok4 = AR.get(F32, [4]); ov4 = AR.get(F32, [4])
            G.update(gates_all=gates_all, dest_all=dest_all, h2b_all=h2b_all)
            dma("pool", wout, w_out.rearrange("(k p) n -> p k n", p=128), w=["wout"])
            dma("sp", wrf, w_router.rearrange("(k p) n -> p k n", p=128), w=["wrf"])
            dma("sp", brb, b_router_row.partition_broadcast(128), w=["brb"])
            dma("sp", io32, iota32_d, w=["io32"]); dma("sp", pcol, pcol_d, w=["pcol"]); dma("sp", k128, k128_d, w=["k128"])
            P.op("dve", lambda e: e.memset(zf, 0.0), w=["zf"])
            dma("sp", yall_d[NTOT:NTOT + 128, :], zf, r=["zf"], w=["Yall"])
            pbtF = [pbt[0][:, :].bitcast(F32), pbt[1][:, :].bitcast(F32)]
            h2f2 = [h2f, AR.get(F32, [D])]; h2T2 = [h2T, AR.get(F32, [8, 128])]
            lg2 = [lg, AR.get(F32, [NE])]; top82 = [top8, AR.get(F32, [8])]; idx82 = [idx8, AR.get(U32, [8])]
            negv2 = [negv, AR.get(F32, [1])]; e42 = [e4, AR.get(F32, [4])]; se2 = [se, AR.get(F32, [1])]; rse2 = [rse, AR.get(F32, [1])]

            def tile_steps(i):
                steps = []
                S = lambda *a_, **k_: steps.append((a_, k_))
                b = i % 2
                K = lambda n: (n, b)
                h2f_, h2T_, lg_, top8_, idx8_, negv_, e4_, se_, rse_ = h2f2[b], h2T2[b], lg2[b], top82[b], idx82[b], negv2[b], e42[b], se2[b], rse2[b]
                S("sp", (lambda e, b=b, i=i: e.dma_start(out=xt[b], in_=x_own[i * 128:(i + 1) * 128, :])), w=[("xt", b)], dma=True)
                ps = pb[b]; pk = "pb%d" % b
                for nb in range(2):
                    for k in range(8):
                        S("pe", lambda e, k=k, nb=nb, i=i, ps=ps: e.matmul(ps[:, :], lhsT=mixT[:, k, i * 128:(i + 1) * 128],
                             rhs=wout[:, k, nb * 512:(nb + 1) * 512], start=(k == 0), stop=(k == 7)), r=["mixT", "wout"], w=[pk])
                    S("dve", lambda e, nb=nb, b=b, ps=ps: e.tensor_tensor(out=x1t[b][:, nb * 512:(nb + 1) * 512], in0=ps[:, :],
                         in1=g1b[:, nb * 512:(nb + 1) * 512], op=ALU.mult), r=[pk, "modb"], w=[("x1t", b)])
                S("dve", lambda e, b=b: e.tensor_tensor(out=x1t[b], in0=x1t[b], in1=xt[b], op=ALU.add),
                     r=[("x1t", b), ("xt", b)], w=[("x1t", b)])
                S("act", (lambda e, b=b, i=i: e.dma_start(out=x1_d[i * 128:(i + 1) * 128, :], in_=x1t[b])), r=[("x1t", b)], w=["x1d"], dma=True)
                S("act", lambda e, b=b, i=i: e.activation(out=junk, in_=x1t[b], func=AF.Square, accum_out=ss[:, i:i + 1]),
                     r=[("x1t", b)], w=["junk", ("ss", i)])
                S("act", lambda e, i=i: e.activation(out=lnv[:, i:i + 1], in_=ss[:, i:i + 1], func=AF.Ln, scale=1.0 / D, bias=EPS),
                     r=[("ss", i)], w=[("lnv", i)])
                S("act", lambda e, i=i: e.activation(out=rs2[:, i:i + 1], in_=lnv[:, i:i + 1], func=AF.Exp, scale=-0.5),
                     r=[("lnv", i)], w=[("rs2", i)])
                S("dve", lambda e, b=b, i=i: e.scalar_tensor_tensor(out=h2f_, in0=x1t[b], scalar=rs2[:, i:i + 1], in1=gs2b,
                     op0=ALU.mult, op1=ALU.mult), r=[("x1t", b), ("rs2", i), "modb"], w=[K("h2f")])
                S("dve", lambda e: e.tensor_tensor(out=h2f_, in0=h2f_, in1=sh2b, op=ALU.add), r=[K("h2f"), "modb"], w=[K("h2f")])
                S("act", lambda e, i=i: e.activation(out=h2b_all[:, i, :], in_=h2f_, func=AF.Copy), r=[K("h2f")], w=[("h2b", i)])
                tpb = [pb[2 + 2 * b], pb[3 + 2 * b]]; tkk = ["pb%d" % (2 + 2 * b), "pb%d" % (3 + 2 * b)]
                for c in range(8):
                    tp = tpb[c // 4]; tk = tkk[c // 4]
                    S("pe", lambda e, c=c, tp=tp: e.transpose(tp[:, (c % 4) * 128:(c % 4 + 1) * 128],
                         h2f_[:, c * 128:(c + 1) * 128], ident_f), r=[K("h2f"), "ident_f"], w=[tk])
                S("act", lambda e: e.activation(out=h2T_[:, 0:4, :], in_=tpb[0][:, :].rearrange("p (a b) -> p a b", a=4),
                     func=AF.Copy), r=[tkk[0]], w=[("h2T", b, 0)])
                S("dve", lambda e: e.tensor_copy(out=h2T_[:, 4:8, :], in_=tpb[1][:, :].rearrange("p (a b) -> p a b", a=4)),
                     r=[tkk[1]], w=[("h2T", b, 1)])
                lp = pbtF[b]; lpk = "pbt%d" % b
                for k in range(8):
                    S("pe", lambda e, k=k: e.matmul(lp[:, 0:NE], lhsT=h2T_[:, k, :], rhs=wrf[:, k, :],
                         start=(k == 0), stop=(k == 7)), r=[("h2T", b, 0), ("h2T", b, 1), "wrf"], w=[lpk])
                S("dve", lambda e: e.tensor_tensor(out=lg_, in0=lp[:, 0:NE], in1=brb, op=ALU.add), r=[lpk, "brb"], w=[K("lg")])
                S("dve", lambda e: e.max(out=top8_, in_=lg_), r=[K("lg")], w=[K("top8")])
                S("dve", lambda e: e.max_index(out=idx8_, in_max=top8_, in_values=lg_), r=[K("lg"), K("top8")], w=[K("idx8")])
                S("dve", lambda e: e.tensor_scalar(out=negv_, in0=top8_[:, 0:1], scalar1=-1.0, scalar2=None, op0=ALU.mult),
                     r=[K("top8")], w=[K("negv")])
                S("act", lambda e: e.activation(out=e4_, in_=top8_[:, 0:4], func=AF.Exp, bias=negv_[:, 0:1], accum_out=se_),
                     r=[K("top8"), K("negv")], w=[K("e4"), K("se")])
                S("dve", lambda e: e.reciprocal(out=rse_, in_=se_), r=[K("se")], w=[K("rse")])
                S("dve", lambda e, i=i: e.tensor_scalar(out=gates_all[:, i, :], in0=e4_, scalar1=rse_[:, 0:1], scalar2=None,
                     op0=ALU.mult), r=[K("e4"), K("rse")], w=[("gates", i)])
                S("dve", lambda e, i=i: e.tensor_scalar(out=A_all[:, i, :], in0=lg_, scalar1=top8_[:, 3:4], scalar2=None,
                     op0=ALU.is_ge), r=[K("lg"), K("top8")], w=[("A", i)])
                S("dve", lambda e, i=i: e.tensor_copy(out=idxf_all[:, i, :], in_=idx8_[:, 0:4]), r=[K("idx8")], w=[("idxf", i)])
                return steps

            import itertools
            ch0 = []; ch1 = [None] * 24
            for i2 in range(0, NT, 2):
                ch0 += tile_steps(i2); ch1 += tile_steps(i2 + 1)
            for xs in itertools.zip_longest(ch0, ch1):
                for x in xs:
                    if x is not None:
                        P.op(*x[0], **x[1])
            for i in range(NT):
                P.op("pe", lambda e, i=i: e.matmul(pb[0][:, 0:NE], lhsT=ones_bf, rhs=A_all[:, i, :], start=(i == 0), stop=(i == NT - 1)),
                     r=["ones_bf", "A"], w=["pb0"])
            P.op("dve", lambda e: e.tensor_copy(out=cnt_b, in_=pb[0][:, 0:NE]), r=["pb0"], w=["cnt_b"])
            P.op("dve", lambda e: e.tensor_scalar(out=heavy_b, in0=cnt_b, scalar1=float(CAP), scalar2=None, op0=ALU.is_gt),
                 r=["cnt_b"], w=["heavy_b"])
            P.op("dve", lambda e: e.memset(one32, 1.0), w=["one32"])
            P.op("dve", lambda e: e.tensor_tensor_scan(out=hs_b, data0=one32, data1=heavy_b, initial=0.0, op0=ALU.mult, op1=ALU.add),
                 r=["one32", "heavy_b"], w=["hs_b"])
            P.op("dve", lambda e: e.tensor_tensor(out=hs_b, in0=hs_b, in1=heavy_b, op=ALU.subtract), r=["hs_b", "heavy_b"], w=["hs_b"])
            for h_ in range(NH):
                P.op("dve", lambda e, h_=h_: e.tensor_scalar(out=eq2, in0=hs_b, scalar1=float(h_), scalar2=None, op0=ALU.is_equal),
                     r=["hs_b"], w=["eq2"])
                P.op("dve", lambda e: e.tensor_tensor(out=eq2, in0=eq2, in1=heavy_b, op=ALU.mult), r=["eq2", "heavy_b"], w=["eq2"])
                P.op("dve", lambda e: e.tensor_tensor(out=eq2, in0=eq2, in1=io32, op=ALU.mult), r=["eq2", "io32"], w=["eq2"])
                P.op("dve", lambda e: e.tensor_reduce(out=ehf, in_=eq2, axis=AX.X, op=ALU.add), r=["eq2"], w=["ehf"])
                P.op("dve", lambda e: e.scalar_tensor_tensor(out=dh_, in0=ehf, scalar=1024.0, in1=pcol[:, 1:2], op0=ALU.mult, op1=ALU.add),
                     r=["ehf", "pcol"], w=["dh_"])
                P.op("dve", lambda e: e.tensor_scalar(out=tmp8, in0=k128, scalar1=dh_[:, 0:1], scalar2=None, op0=ALU.add),
                     r=["k128", "dh_"], w=["tmp8"])
                P.op("dve", lambda e, h_=h_: e.tensor_copy(out=idxw_sb[:, h_, :], in_=tmp8), r=["tmp8"], w=[("idxw", h_)])
                P.op("dve", lambda e: e.scalar_tensor_tensor(out=dh_, in0=ehf, scalar=128.0, in1=pcol[:, 1:2], op0=ALU.mult, op1=ALU.add),
                     r=["ehf", "pcol"], w=["dh_"])
                P.op("dve", lambda e, h_=h_: e.tensor_copy(out=idxb_sb[:, h_, 0:1], in_=dh_), r=["dh_"], w=[("idxb", h_)])
                P.op("dve", lambda e, h_=h_: e.tensor_copy(out=idxb_sb[:, h_, 1:2], in_=ehf), r=["ehf"], w=[("idxb", h_)])
            for i in range(NT):
                P.op("pe", lambda e, i=i: e.matmul(pb[5][:, 0:NE], lhsT=triu_bf, rhs=A_all[:, i, :], start=True, stop=(i == 0)),
                     r=["triu_bf", "A"], w=["pb5"])
                for j in range(i):
                    P.op("pe", lambda e, j=j, i=i: e.matmul(pb[5][:, 0:NE], lhsT=ones_bf, rhs=A_all[:, j, :], start=False,
                         stop=(j == i - 1)), r=["ones_bf", "A"], w=["pb5"])
                idx4 = idxf_all[:, i, :]
                P.op("dve", lambda e, idx4=idx4: e.tensor_tensor(out=eq4, in0=io32.unsqueeze(1).to_broadcast([128, 4, NE]),
                     in1=idx4.unsqueeze(2).to_broadcast([128, 4, NE]), op=ALU.is_equal), r=["io32", ("idxf", i)], w=["eq4"])
                P.op("dve", lambda e: e.tensor_tensor(out=eq4b, in0=eq4, in1=hs_b.unsqueeze(1).to_broadcast([128, 4, NE]), op=ALU.mult),
                     r=["eq4", "hs_b"], w=["eq4b"])
                P.op("dve", lambda e: e.tensor_reduce(out=hsk4, in_=eq4b, axis=AX.X, op=ALU.add), r=["eq4b"], w=["hsk4"])
                P.op("dve", lambda e: e.tensor_tensor(out=eq4, in0=eq4, in1=pb[5][:, 0:NE].unsqueeze(1).to_broadcast([128, 4, NE]),
                     op=ALU.mult), r=["eq4", "pb5"], w=["eq4"])
                P.op("dve", lambda e: e.tensor_reduce(out=s14, in_=eq4, axis=AX.X, op=ALU.add), r=["eq4"], w=["s14"])
                P.op("dve", lambda e: e.tensor_scalar(out=lt4, in0=s14, scalar1=float(CAP), scalar2=None, op0=ALU.is_le), r=["s14"], w=["lt4"])
                P.op("dve", lambda e, idx4=idx4: e.scalar_tensor_tensor(out=d4, in0=idx4, scalar=float(CAP), in1=s14, op0=ALU.mult, op1=ALU.add),
                     r=[("idxf", i), "s14"], w=["d4"])
                P.op("dve", lambda e: e.scalar_tensor_tensor(out=dh4, in0=hsk4, scalar=float(CH), in1=s14, op0=ALU.mult, op1=ALU.add),
                     r=["hsk4", "s14"], w=["dh4"])
                P.op("dve", lambda e: e.tensor_scalar(out=ok4, in0=hsk4, scalar1=float(NH) - 0.5, scalar2=None, op0=ALU.is_lt), r=["hsk4"], w=["ok4"])
                P.op("dve", lambda e: e.tensor_scalar(out=ov4, in0=s14, scalar1=float(CAP + CH), scalar2=None, op0=ALU.is_le), r=["s14"], w=["ov4"])
                P.op("dve", lambda e: e.tensor_tensor(out=ok4, in0=ok4, in1=ov4, op=ALU.mult), r=["ok4", "ov4"], w=["ok4"])
                P.op("dve", lambda e: e.tensor_scalar(out=ov4, in0=lt4, scalar1=-1.0, scalar2=1.0, op0=ALU.mult, op1=ALU.add), r=["lt4"], w=["ov4"])
                P.op("dve", lambda e: e.tensor_tensor(out=ok4, in0=ok4, in1=ov4, op=ALU.mult), r=["ok4", "ov4"], w=["ok4"])
                P.op("dve", lambda e: e.tensor_scalar(out=d4, in0=d4, scalar1=-1.0, scalar2=pcol[:, 0:1], op0=ALU.add, op1=ALU.subtract),
                     r=["d4", "pcol"], w=["d4"])
                P.op("dve", lambda e: e.tensor_scalar(out=dh4, in0=dh4, scalar1=float(NE * CAP - CAP - 1), scalar2=pcol[:, 0:1],
                     op0=ALU.add, op1=ALU.subtract), r=["dh4", "pcol"], w=["dh4"])
                P.op("dve", lambda e: e.tensor_tensor(out=d4, in0=d4, in1=lt4, op=ALU.mult), r=["d4", "lt4"], w=["d4"])
                P.op("dve", lambda e: e.tensor_tensor(out=dh4, in0=dh4, in1=ok4, op=ALU.mult), r=["dh4", "ok4"], w=["dh4"])
                P.op("dve", lambda e: e.tensor_tensor(out=d4, in0=d4, in1=dh4, op=ALU.add), r=["d4", "dh4"], w=["d4"])
                P.op("dve", lambda e: e.tensor_scalar(out=d4, in0=d4, scalar1=pcol[:, 0:1], scalar2=None, op0=ALU.add), r=["d4", "pcol"], w=["d4"])
                P.op("dve", lambda e, i=i: e.tensor_copy(out=dest_all[:, i, :], in_=d4), r=["d4"], w=[("dest", i)])
            for i in range(NT):
                sg_ = stage_sb[i % 2]; sgk = ("stage", i % 2)
                P.op("act", lambda e, i=i, sg_=sg_: e.activation(out=sg_[:, :], in_=h2b_all[:, i, :], func=AF.Copy),
                     r=[("h2b", i)], w=[sgk])
                for k in range(4):
                    P.op("pool", lambda e, i=i, k=k, sg_=sg_: e.indirect_dma_start(out=xg_d[:, :],
                         out_offset=bass.IndirectOffsetOnAxis(ap=dest_sb[:, i, k:k + 1], axis=0), in_=sg_[:, :],
                         in_offset=None),
                         r=[("dest", i), sgk], w=["Xg"], dma=True)
            if "gates" in dbg_out:
                finals.append(dma("sp", dbg_out["gates"], gates_all, r=["gates"]))
                finals.append(dma("sp", dbg_out["idxf"], idxf_all, r=["idxf"]))
                P.op("dve", lambda e: e.tensor_copy(out=gates_all, in_=dest_all), r=["dest", "gates"], w=["gates"])
                finals.append(dma("sp", dbg_out["dest"], gates_all, r=["gates"]))
            barrier()

        def phaseI():
            NS = CAP // 128
            AR.off = off_base
            xe = AR.get(BF16, [NS, D]); actT = AR.get(BF16, [8, CAP])
            assert AR.off <= off_mix
            AR.off = G["off_keep"]
            AR.push()
            wup = [AR.get(BF16, [8, 2 * D]), AR.get(BF16, [8, 2 * D])]
            wdn = [AR.get(BF16, [8, D]), AR.get(BF16, [8, D])]
            xeT = AR.get(BF16, [8, CAP])
            ye = [AR.get(F32, [D]), AR.get(F32, [D])]
            bdn = [AR.get(F32, [D]), AR.get(F32, [D])]
            bupc = AR.get(F32, [NE, 16]); buph = AR.get(F32, [NH, 16])
            gm = [AR.get(F32, [512]), AR.get(F32, [512])]; sgm = [AR.get(F32, [512]), AR.get(F32, [512])]
            l1 = [AR.get(F32, [512]), AR.get(F32, [512])]
            dma("sp", bupc, bup_col.rearrange("p (a b) -> p a b", a=NE), w=["bupc"])
            dbanks = [(pb[4], "pb4"), (pb[5], "pb5"), (pbt[0][:, :].bitcast(F32), "pbt0"), (pbt[1][:, :].bitcast(F32), "pbt1")]
            wupf = w_up.rearrange("e k n -> (e k) n"); wdnf = w_down.rearrange("e k n -> (e k) n")

            def load_w(e_):
                wb = e_ % 2
                wu3 = w_up[e_].rearrange("(k p) n -> p k n", p=128)
                for hh in range(2):
                    dma("pool", wup[wb][:, :, hh * D:(hh + 1) * D], wu3[:, :, hh * D:(hh + 1) * D], w=[("wup", wb)])
                dma("pool", wdn[wb], w_down[e_].rearrange("(k p) n -> p k n", p=128), w=[("wdn", wb)])
                dma("act", bdn[wb], b_down[e_:e_ + 1, :].partition_broadcast(128), w=[("bdn", wb)])

            def load_w_heavy(h_, wb):
                for k in range(8):
                    P.op("pool", lambda e, k=k: e.indirect_dma_start(out=wup[wb][:, k, :], out_offset=None, in_=wupf[:, :],
                         in_offset=bass.IndirectOffsetOnAxis(ap=idxw_sb[:, h_, k:k + 1], axis=0)), r=[("idxw", h_)], w=[("wup", wb)], dma=True)
                for k in range(8):
                    P.op("pool", lambda e, k=k: e.indirect_dma_start(out=wdn[wb][:, k, :], out_offset=None, in_=wdnf[:, :],
                         in_offset=bass.IndirectOffsetOnAxis(ap=idxw_sb[:, h_, k:k + 1], axis=0)), r=[("idxw", h_)], w=[("wdn", wb)], dma=True)
                P.op("pool", lambda e: e.indirect_dma_start(out=buph[:, h_, :], out_offset=None, in_=bq_d[:, :],
                     in_offset=bass.IndirectOffsetOnAxis(ap=idxb_sb[:, h_, 0:1], axis=0)), r=[("idxb", h_)], w=[("buph", h_)], dma=True)
                P.op("pool", lambda e: e.indirect_dma_start(out=bdn[wb], out_offset=None, in_=b_down[:, :],
                     in_offset=bass.IndirectOffsetOnAxis(ap=idxb_sb[:, h_, 1:2], axis=0)), r=[("idxb", h_)], w=[("bdn", wb)], dma=True)

            def transposes(row0, ns):
                dma("sp", xe[:, 0:ns, :], xg_d[row0:row0 + ns * 128, :].rearrange("(s p) d -> p s d", p=128), r=["Xg"], w=["xe"])
                for c in range(8):
                    tp = pbt[c % 2]; tk = "pbt%d" % (c % 2)
                    for s_ in range(ns):
                        P.op("pe", lambda e, c=c, s_=s_, tp=tp: e.transpose(tp[:, s_ * 128:(s_ + 1) * 128],
                             xe[:, s_, c * 128:(c + 1) * 128], ident_bf), r=["xe", "ident_bf"], w=[tk])
                    if c % 2 == 0:
                        P.op("act", lambda e, c=c, tp=tp: e.activation(out=xeT[:, c, 0:ns * 128], in_=tp[:, 0:ns * 128], func=AF.Copy),
                             r=[tk], w=[("xeT", c)])
                    else:
                        P.op("dve", lambda e, c=c, tp=tp: e.tensor_copy(out=xeT[:, c, 0:ns * 128], in_=tp[:, 0:ns * 128]), r=[tk], w=[("xeT", c)])

            def up(wb, nn, bcol, bkey):
                n0 = 0
                for f in range(8):
                    bb = f % 2
                    gps = pb[bb * 2]; gk = "pb%d" % (bb * 2); lps = pb[bb * 2 + 1]; lk = "pb%d" % (bb * 2 + 1)
                    for k in range(8):
                        P.op("pe", lambda e, k=k, f=f, gps=gps: e.matmul(gps[:, 0:nn],
                             lhsT=wup[wb][:, k, f * 128:(f + 1) * 128], rhs=xeT[:, k, n0:n0 + nn], start=(k == 0), stop=(k == 7)),
                             r=[("wup", wb), "xeT"], w=[gk])
                    for k in range(8):
                        P.op("pe", lambda e, k=k, f=f, lps=lps: e.matmul(lps[:, 0:nn],
                             lhsT=wup[wb][:, k, D + f * 128:D + (f + 1) * 128], rhs=xeT[:, k, n0:n0 + nn], start=(k == 0),
                             stop=(k == 7)), r=[("wup", wb), "xeT"], w=[lk])
                    g_, s__, l_ = gm[bb], sgm[bb], l1[bb]
                    P.op("dve", lambda e, f=f, gps=gps, g_=g_: e.tensor_scalar(out=g_[:, 0:nn], in0=gps[:, 0:nn],
                         scalar1=bcol(f), scalar2=7.0, op0=ALU.add, op1=ALU.min), r=[gk, bkey], w=[("gm", bb)])
                    P.op("act", lambda e, g_=g_, s__=s__: e.activation(out=s__[:, 0:nn], in_=g_[:, 0:nn], func=AF.Sigmoid,
                         scale=1.702), r=[("gm", bb)], w=[("sgm", bb)])
                    P.op("dve", lambda e, f=f, lps=lps, l_=l_: e.tensor_scalar(out=l_[:, 0:nn], in0=lps[:, 0:nn],
                         scalar1=bcol(8 + f), scalar2=7.0, op0=ALU.add, op1=ALU.min), r=[lk, bkey], w=[("l1", bb)])
                    P.op("dve", lambda e, l_=l_: e.tensor_scalar(out=l_[:, 0:nn], in0=l_[:, 0:nn], scalar1=-7.0, scalar2=1.0,
                         op0=ALU.max, op1=ALU.add), r=[("l1", bb)], w=[("l1", bb)])
                    P.op("dve", lambda e, g_=g_, s__=s__: e.tensor_tensor(out=g_[:, 0:nn], in0=g_[:, 0:nn], in1=s__[:, 0:nn],
                         op=ALU.mult), r=[("gm", bb), ("sgm", bb)], w=[("gm", bb)])
                    P.op("dve", lambda e, f=f, g_=g_, l_=l_: e.tensor_tensor(out=actT[:, f, n0:n0 + nn],
                         in0=g_[:, 0:nn], in1=l_[:, 0:nn], op=ALU.mult), r=[("gm", bb), ("l1", bb)], w=[("actT", f)])

            def down(wb, row0, ns):
                for s_ in range(ns):
                    yb = ye[s_ % 2]; yk = ("ye", s_ % 2)
                    for nb in range(2):
                        ps, pk = dbanks[(s_ * 2 + nb) % 4]
                        for f in range(8):
                            P.op("pe", lambda e, f=f, s_=s_, nb=nb, ps=ps: e.matmul(ps[:, :], lhsT=actT[:, f, s_ * 128:(s_ + 1) * 128],
                                 rhs=wdn[wb][:, f, nb * 512:(nb + 1) * 512], start=(f == 0), stop=(f == 7)),
                                 r=["actT", ("wdn", wb)], w=[pk])
                        P.op("dve", lambda e, nb=nb, ps=ps, yb=yb: e.tensor_tensor(out=yb[:, nb * 512:(nb + 1) * 512], in0=ps[:, :],
                             in1=bdn[wb][:, nb * 512:(nb + 1) * 512], op=ALU.add), r=[pk, ("bdn", wb)], w=[yk])
                    r0 = row0 + s_ * 128
                    dma("sp" if s_ % 2 == 0 else "act", yall_d[r0:r0 + 128, :], yb, r=[yk], w=["Yall"])

            NU = NE + NH
            def unit(u):
                if u < NE:
                    return dict(row0=u * CAP, ns=NS, nn=CAP, bcol=(lambda f, u=u: bupc[:, u, f:f + 1]), bkey="bupc")
                h_ = u - NE
                return dict(row0=NE * CAP + h_ * CH, ns=CH // 128, nn=CH, bcol=(lambda f, h_=h_: buph[:, h_, f:f + 1]), bkey=("buph", h_))

            def load_unit(u):
                if u < NE:
                    load_w(u)
                else:
                    load_w_heavy(u - NE, u % 2)

            load_unit(0)
            transposes(unit(0)["row0"], unit(0)["ns"])
            for u in range(NU):
                U = unit(u)
                if u + 1 < NU:
                    load_unit(u + 1)
                up(u % 2, U["nn"], U["bcol"], U["bkey"])
                if u + 1 < NU:
                    U2 = unit(u + 1)
                    transposes(U2["row0"], U2["ns"])
                down(u % 2, U["row0"], U["ns"])
            barrier()
            AR.pop()

        def phaseJ():
            AR.off = G["off_keep"]
            AR.push()
            gates_all = G["gates_all"]; g2b = G["g2b"]
            yg = [[AR.get(F32, [D]) for _ in range(4)] for _ in range(2)]
            x1t = [AR.get(F32, [D]), AR.get(F32, [D])]; acc = [AR.get(F32, [D]), AR.get(F32, [D])]
            for i in range(NT):
                b = i % 2
                dma("sp", x1t[b], x1_d[i * 128:(i + 1) * 128, :], r=["x1d"], w=[("x1t", b)])
                for k in range(4):
                    P.op("pool", lambda e, i=i, k=k, b=b: e.indirect_dma_start(out=yg[b][k], out_offset=None, in_=yall_d[:, :],
                         in_offset=bass.IndirectOffsetOnAxis(ap=dest_sb[:, i, k:k + 1], axis=0)),
                         r=["Yall", ("dest", i)], w=[("yg", b, k)], dma=True)
                a_ = acc[b]; ak = ("acc", b)
                P.op("dve", lambda e, i=i, b=b, a_=a_: e.tensor_scalar(out=a_, in0=yg[b][0], scalar1=gates_all[:, i, 0:1], scalar2=None,
                     op0=ALU.mult), r=[("yg", b, 0), "gates"], w=[ak])
                for k in (1, 2, 3):
                    P.op("dve", lambda e, i=i, b=b, k=k, a_=a_: e.scalar_tensor_tensor(out=a_, in0=yg[b][k],
                         scalar=gates_all[:, i, k:k + 1], in1=a_, op0=ALU.mult, op1=ALU.add), r=[("yg", b, k), "gates", ak], w=[ak])
                P.op("dve", lambda e, a_=a_: e.tensor_tensor(out=a_, in0=a_, in1=g2b, op=ALU.mult), r=[ak, "modb"], w=[ak])
                P.op("dve", lambda e, b=b, a_=a_: e.tensor_tensor(out=a_, in0=a_, in1=x1t[b], op=ALU.add), r=[ak, ("x1t", b)], w=[ak])
                finals.append(dma("act", out_d[i * 128:(i + 1) * 128, :], a_, r=[ak]))
            AR.pop()

        phases = [phaseB, phaseC, phaseD, phaseE, phaseF, phaseG, phaseI, phaseJ]
        for i, ph in enumerate(phases):
            if stage < i + 2:
                break
            ph()
        def dump_bf(name, src, n):
            if name not in dbg_out:
                return
            AR.push()
            stg = [AR.get(F32, [n]), AR.get(F32, [n])]
            for k in range(8):
                P.op("dve", lambda e, k=k: e.tensor_copy(out=stg[k % 2], in_=src[:, k, :]), r=[name], w=[("stg", k % 2)])
                finals.append(dma("sp", dbg_out[name][:, k, :], stg[k % 2], r=[("stg", k % 2)]))
            AR.pop()
        if stage < 7:
            dump_bf("hT", hT, LT); dump_bf("yT", yT, T); dump_bf("mixT", mixT, T)
        if "x1" in dbg_out:
            AR.push()
            stg = AR.get(F32, [D])
            for i in range(NT):
                dma("sp", stg, x1_d[i * 128:(i + 1) * 128, :], r=["x1d"], w=["stg"])
                finals.append(dma("sp", dbg_out["x1"][i * 128:(i + 1) * 128, :], stg, r=["stg"]))
            AR.pop()
        P.emit(finals)
    return nc


def _consts():
    bf = ml_dtypes.bfloat16
    c = {}
    c["ident_bf"] = np.eye(128, dtype=np.float32).astype(bf)
    c["ident_f"] = np.eye(128, dtype=np.float32)
    k = np.arange(128)
    c["triu_bf"] = (k[:, None] <= k[None, :]).astype(np.float32).astype(bf)
    tb = np.zeros((128, 256), np.float32)
    tb[:, :128] = np.where(k[:, None] <= k[None, :], 0.0, NEG)
    c["tribias"] = tb.astype(bf)
    rm = np.zeros((128, 32), np.float32)
    for m in range(16):
        rm[m + 16, m] = 1.0
        rm[m, m + 16] = 1.0
    c["rm_f"] = rm
    half = 16
    freqs = (np.float32(500000.0) ** (-np.arange(half, dtype=np.float32) / np.float32(half))).astype(np.float32)
    fc = np.zeros((32, 2), np.float32)
    fc[:, 0] = np.concatenate([freqs, freqs])
    fc[:16, 1] = -1.0
    fc[16:, 1] = 1.0
    c["freq_col"] = fc
    c["iota32"] = np.tile(np.arange(NE, dtype=np.float32)[None, :], (128, 1))
    c["pcol"] = np.stack([NTOT + np.arange(128, dtype=np.float32), np.arange(128, dtype=np.float32)], axis=1).copy()
    c["k128"] = np.tile((128.0 * np.arange(8, dtype=np.float32))[None, :], (128, 1))
    return c


def _col(v, nchunk):
    return np.ascontiguousarray(np.asarray(v, np.float32).reshape(nchunk, 128).T)


def prep_inputs(inp):
    f = lambda a: np.ascontiguousarray(np.asarray(a, np.float32))
    x = f(inp["x"]); c = f(inp["c"]); positions = np.asarray(inp["positions"]).astype(np.int32)
    shared = {
        "ada_w": f(inp["ada_w"][0]),
        "ada_b_col": _col(inp["ada_b"][0], 48),
        "ada_b_row": f(inp["ada_b"][0][None, 2 * D:]),
        "n1g_col": _col(inp["norm1_g"][0], 8),
        "n2g_row": f(inp["norm2_g"][0][None, :]),
        "w_in": f(inp["w_in"][0]),
        "convw_col": np.ascontiguousarray(f(inp["conv_w"][0]).T.reshape(8, 128, 4).transpose(1, 0, 2).reshape(128, 32)),
        "convb_col": _col(inp["conv_b"][0], 8),
        "w_rg_a": f(inp["w_rg_a"][0]), "brga_col": _col(inp["b_rg_a"][0], 8),
        "w_rg_x": f(inp["w_rg_x"][0]), "brgx_col": _col(inp["b_rg_x"][0], 8),
        "lam_col": _col(inp["lru_lambda"][0], 8),
        "qg_col": f(inp["q_norm_g"][0][:, None]), "kg_col": f(inp["k_norm_g"][0][:, None]),
        "bgate_col": np.ascontiguousarray(f(inp["b_gate"][0]).reshape(2, 8, 128).transpose(2, 0, 1).reshape(128, 16)),
        "w_branch": f(inp["w_branch"][0]), "w_out": f(inp["w_out"][0]),
        "w_router": f(inp["w_router"][0]), "b_router_row": f(inp["b_router"][0][None, :]),
        "w_up": f(inp["w_up"][0]),
        "bup_col": np.ascontiguousarray(f(inp["b_up"][0]).reshape(NE, 16, 128).transpose(2, 0, 1).reshape(128, NE * 16)),
        "w_down": f(inp["w_down"][0]), "b_down": f(inp["b_down"][0]),
        "bq": np.ascontiguousarray(f(inp["b_up"][0]).reshape(NE, 16, 128).transpose(0, 2, 1).reshape(NE * 128, 16)),
    }
    shared.update(_consts())
    maps = []
    for core in range(8):
        b, h = core // 2, core % 2
        m = dict(shared)
        m["x_own"] = np.ascontiguousarray(x[b, h * T:(h + 1) * T])
        m["x_pre"] = np.ascontiguousarray(x[b, 0:T])
        m["pos"] = np.ascontiguousarray(np.concatenate([positions[b, 0:T], positions[b, h * T:(h + 1) * T]])[None, :])
        m["c_col"] = _col(c[b], 8)
        m["hflag"] = np.full((128, 1), float(h), np.float32)
        vb = np.full((8, 16), NEG, np.float32)
        for j in range(8):
            for n in range(16):
                if n < 8 + j and (n >= 8 or h == 1):
                    vb[j, n] = 0.0
        m["vbias"] = np.ascontiguousarray(np.tile(vb.reshape(1, 128), (128, 1)))
        maps.append(m)
    return maps


_NC_CACHE = {}


def kernel(**inputs):
    maps = prep_inputs(inputs)
    if "nc" not in _NC_CACHE:
        _NC_CACHE["nc"] = build_program()
    res = run_bass_kernel_spmd(_NC_CACHE["nc"], maps, core_ids=list(range(8)))
    out = np.zeros((4, 2 * T, D), np.float32)
    for core in range(8):
        b, h = core // 2, core % 2
        out[b, h * T:(h + 1) * T] = res.results[core]["out"]
    return out
```

```python
import numpy as np
import ml_dtypes
from contextlib import ExitStack
import concourse.bass as bass
import concourse.mybir as mybir
from concourse.bass_utils import run_bass_kernel_spmd

F32 = mybir.dt.float32
BF16 = mybir.dt.bfloat16
I32 = mybir.dt.int32
U32 = mybir.dt.uint32
AF = mybir.ActivationFunctionType
ALU = mybir.AluOpType
AX = mybir.AxisListType

D = 1024
T = 2048
NT = 16
LT = 4096
NE = 32
CAP = 512
NH = 4
CH = 256
NTOT = NE * CAP + NH * CH
EPS = 1e-6
NEG = -30000.0
FLAGS = {}


class _Op:
    __slots__ = ("eng", "fn", "deps", "signal", "sidx", "dma", "sem", "semval", "n")


class Prog:
    CE = ("pe", "act", "dve", "pool", "sp")
    EPOCH = 12000

    def __init__(self, nc, stack):
        self.nc = nc
        self.stack = stack
        self.ops = {e: [] for e in ("pe", "act", "dve", "pool", "sp")}
        self.state = {}
        self.subs = {}
        self.ndma = {"sp": 0, "act": 0, "pool": 0}
        self.dma_last = {}
        self.NDS = 8
        self.n = 0

    def sb(self, name, shape, dt):
        return self.stack.enter_context(self.nc.sbuf_tensor(name, list(shape), dt))

    def ps(self, name, shape, dt):
        return self.stack.enter_context(self.nc.psum_tensor(name, list(shape), dt))

    def _keys(self, k):
        if isinstance(k, tuple):
            name = k[0]
            self.subs.setdefault(name, set()).add(k)
            return [k, name]
        else:
            return [k] + list(self.subs.get(k, ()))

    def op(self, eng, fn, r=(), w=(), dma=False):
        o = _Op()
        o.eng, o.fn, o.deps, o.signal, o.dma = eng, fn, set(), False, dma
        o.sem = o.semval = o.sidx = None
        o.n = self.n
        self.n += 1
        for k in r:
            for kk in self._keys(k):
                st = self.state.get(kk)
                if st and st[0] is not None:
                    o.deps.add(st[0])
        for k in w:
            for kk in self._keys(k):
                st = self.state.get(kk)
                if st:
                    if st[0] is not None:
                        o.deps.add(st[0])
                    for x in st[1]:
                        o.deps.add(x)
        for k in r:
            st = self.state.setdefault(k, [None, []])
            st[1].append(o)
        for k in w:
            self.state[k] = [o, []]
            if not isinstance(k, tuple):
                for kk in self.subs.get(k, ()):
                    self.state[kk] = [o, []]
        o.deps.discard(o)
        if dma:
            j = self.ndma[eng] % self.NDS
            self.ndma[eng] += 1
            key = (eng, j)
            prev = self.dma_last.get(key)
            if prev is not None:
                o.deps.add(prev)
                o.semval = prev.semval + 16
            else:
                o.semval = 16
            o.sem = key
            self.dma_last[key] = o
        for x in o.deps:
            if not x.dma and not (x.eng == "pe" and eng == "pe" and not dma):
                x.signal = True
        self.ops[eng].append(o)
        return o

    def emit(self, final_ops):
        nc = self.nc
        st = self.stack
        nsig = {}
        for e in self.CE:
            c = 0
            for o in self.ops[e]:
                if o.signal and not o.dma:
                    o.sidx = c
                    c += 1
            nsig[e] = c
        esem = {}
        for e in self.CE:
            for ep in range(nsig[e] // self.EPOCH + 1):
                esem[(e, ep)] = st.enter_context(nc.semaphore("s_%s_%d" % (e, ep)))
        dsem = {}
        for q in ("sp", "act", "pool"):
            for j in range(min(self.NDS, self.ndma[q])):
                dsem[(q, j)] = st.enter_context(nc.semaphore("d_%s_%d" % (q, j)))
        fin = st.enter_context(nc.semaphore("fin"))
        semname = {id(v): k for k, v in list(esem.items()) + list(dsem.items())}
        block = st.enter_context(nc.Block())
        EP = self.EPOCH

        def run(ename, eng, extra=None):
            waited = {}
            for o in self.ops[ename]:
                need = {}
                for x in o.deps:
                    if x.dma:
                        s, v = dsem[x.sem], x.semval
                    else:
                        if x.eng == "pe" and ename == "pe" and not o.dma:
                            continue
                        s, v = esem[(x.eng, x.sidx // EP)], x.sidx % EP + 1
                    if waited.get(s, 0) >= v:
                        continue
                    if need.get(s, 0) < v:
                        need[s] = v
                for s, v in need.items():
                    eng.wait_ge(s, v)
                    waited[s] = v
                if FLAGS.get("trace"):
                    print(ename, o.n, "waits", [(semname[id(s)], v) for s, v in need.items()],
                          "sig", (o.sidx if o.signal and not o.dma else None), "dma", (o.sem, o.semval) if o.dma else None)
                ins = o.fn(eng)
                if o.dma:
                    ins.then_inc(dsem[o.sem], 16)
                elif o.signal:
                    ins.then_inc(esem[(ename, o.sidx // EP)], 1)
            if extra:
                extra(eng, waited)

        def fin_sp(eng, waited):
            for x in final_ops:
                s, v = dsem[x.sem], x.semval
                if waited.get(s, 0) < v:
                    eng.wait_ge(s, v)
                    waited[s] = v

        @block.sync
        def _(e):
            run("sp", e, fin_sp)

        @block.scalar
        def _(e):
            run("act", e)

        @block.vector
        def _(e):
            run("dve", e)

        @block.gpsimd
        def _(e):
            run("pool", e)

        @block.tensor
        def _(e):
            run("pe", e)


ARENA_F32 = 51600


class Arena:
    def __init__(self, big):
        self.big = big
        self.off = 0
        self.marks = []

    def push(self):
        self.marks.append(self.off)

    def pop(self):
        self.off = self.marks.pop()

    def get(self, dt, shape, parts=128):
        n = 1
        for s in shape:
            n *= s
        esz = 4 if dt in (F32, I32, U32) else 2
        words = (n * esz + 3) // 4
        words = (words + 7) // 8 * 8
        a = self.big[0:parts, self.off:self.off + words]
        self.off += words
        assert self.off <= ARENA_F32, ("arena overflow", self.off)
        if dt != F32:
            a = a.bitcast(dt)
        a = a[:, 0:n]
        if len(shape) == 2:
            a = a.rearrange("p (a b) -> p a b", a=shape[0])
        elif len(shape) == 3:
            a = a.rearrange("p (a b c) -> p a b c", a=shape[0], b=shape[1])
        return a


def build_program(stage=99, dbg=None):
    nc = bass.Bass("TRN2", target_bir_lowering=False)
    dbg = dbg or {}

    def din(name, shape, dt=F32):
        return nc.dram_tensor(name, list(shape), dt, kind="ExternalInput").ap()

    x_own = din("x_own", [T, D]); x_pre = din("x_pre", [T, D])
    pos = din("pos", [1, LT], I32)
    c_col = din("c_col", [128, 8]); hflag_d = din("hflag", [128, 1]); vbias_d = din("vbias", [128, 8 * 16])
    ada_w = din("ada_w", [D, 6 * D]); ada_b_col = din("ada_b_col", [128, 48]); ada_b_row = din("ada_b_row", [1, 4 * D])
    n1g_col = din("n1g_col", [128, 8]); n2g_row = din("n2g_row", [1, D])
    w_in = din("w_in", [D, 7 * D])
    convw_col = din("convw_col", [128, 32]); convb_col = din("convb_col", [128, 8])
    w_rg_a = din("w_rg_a", [8, 128, 128]); brga_col = din("brga_col", [128, 8])
    w_rg_x = din("w_rg_x", [8, 128, 128]); brgx_col = din("brgx_col", [128, 8])
    lam_col = din("lam_col", [128, 8]); qg_col = din("qg_col", [128, 1]); kg_col = din("kg_col", [128, 1])
    bgate_col = din("bgate_col", [128, 16])
    w_branch = din("w_branch", [2, D, D]); w_out = din("w_out", [D, D])
    w_router = din("w_router", [D, NE]); b_router_row = din("b_router_row", [1, NE])
    w_up = din("w_up", [NE, D, 2 * D]); bup_col = din("bup_col", [128, NE * 16])
    w_down = din("w_down", [NE, D, D]); b_down = din("b_down", [NE, D])
    ident_bf_d = din("ident_bf", [128, 128], BF16); ident_f_d = din("ident_f", [128, 128])
    triu_bf_d = din("triu_bf", [128, 128], BF16); tribias_d = din("tribias", [128, 256], BF16)
    rm_f_d = din("rm_f", [128, 32]); freq_d = din("freq_col", [32, 2]); iota32_d = din("iota32", [128, NE]); pcol_d = din("pcol", [128, 2]); k128_d = din("k128", [128, 8]); bq_d = din("bq", [NE * 128, 16])
    out_d = nc.dram_tensor("out", [T, D], F32, kind="ExternalOutput").ap()
    xg_d = nc.dram_tensor("xg_scr", [NTOT + 128, D], BF16, kind="Internal").ap()
    yall_d = nc.dram_tensor("yall_scr", [NTOT + 128, D], F32, kind="Internal").ap()
    x1_d = nc.dram_tensor("x1_scr", [T, D], F32, kind="Internal").ap()
    modb_d = nc.dram_tensor("modb_scr", [128, 4 * D], F32, kind="Internal").ap()
    dbg_out = {}
    for k, shp in dbg.items():
        dbg_out[k] = nc.dram_tensor("dbg_" + k, list(shp), F32, kind="ExternalOutput").ap()

    st = ExitStack()
    with st:
        P = Prog(nc, st)
        big = P.sb("arena", [128, ARENA_F32], F32)
        AR = Arena(big)
        dest_sb = P.sb("dest_sb", [128, 16, 4], I32)
        idxw_sb = P.sb("idxw_sb", [128, NH, 8], I32)
        idxb_sb = P.sb("idxb_sb", [128, NH, 2], I32)
        stage_sb = [P.sb("stage_sb%d" % i, [128, D], BF16) for i in range(2)]
        pb = [P.ps("pb%d" % i, [128, 512], F32) for i in range(6)]
        pbt = [P.ps("pbt%d" % i, [128, 1024], BF16) for i in range(2)]
        finals = []
        cnt = [0]

        def uid(s):
            cnt[0] += 1
            return "%s_%d" % (s, cnt[0])

        def dma(q, out, in_, r=(), w=()):
            return P.op(q, lambda e: e.dma_start(out=out, in_=in_), r=r, w=w, dma=True)

        def barrier():
            lasts = []
            for e in ("pe", "act", "dve", "pool", "sp"):
                if P.ops[e]:
                    lasts.append(P.ops[e][-1])
            lasts += list(P.dma_last.values())
            for e in ("pe", "act", "dve", "pool", "sp"):
                o = P.op(e, lambda en: en.nop())
                for x in lasts:
                    if x is not o:
                        o.deps.add(x)
                        if not x.dma:
                            x.signal = True

        def dump(name, ap, key):
            if name not in dbg_out:
                return
            finals.append(dma("sp", dbg_out[name], ap, r=[key]))

        ident_bf = AR.get(BF16, [128]); ident_f = AR.get(F32, [128])
        ones_bf = AR.get(BF16, [128]); triu_bf = AR.get(BF16, [128]); tribias = AR.get(BF16, [256])
        hflag = AR.get(F32, [1]); vbias = AR.get(F32, [8, 16])
        sh1c = AR.get(F32, [8]); gs1c = AR.get(F32, [8])
        sccol = AR.get(F32, [8])
        dma("sp", ident_bf, ident_bf_d, w=["ident_bf"]); dma("sp", ident_f, ident_f_d, w=["ident_f"])
        dma("sp", triu_bf, triu_bf_d, w=["triu_bf"]); dma("sp", tribias, tribias_d, w=["tribias"])
        dma("sp", hflag, hflag_d, w=["hflag"])
        dma("sp", vbias, vbias_d.rearrange("p (a b) -> p a b", a=8), w=["vbias"])
        P.op("dve", lambda e: e.memset(ones_bf, 1.0), w=["ones_bf"])
        zt = AR.get(BF16, [2, D])
        P.op("dve", lambda e: e.memset(zt, 0.0), w=["zt"])

        AR.push()
        ccol = AR.get(F32, [8])
        abcol = AR.get(F32, [48]); n1g = AR.get(F32, [8])
        wA = [AR.get(F32, [8, 512]), AR.get(F32, [8, 512])]
        dma("sp", ccol, c_col, w=["ccol"]); dma("sp", abcol, ada_b_col, w=["abcol"]); dma("sp", n1g, n1g_col, w=["n1g"])
        P.op("act", lambda e: e.activation(out=sccol, in_=ccol, func=AF.Silu), r=["ccol"], w=["sccol"])
        adw = ada_w.rearrange("(k p) n -> p k n", p=128)
        for g in range(4):
            wb = wA[g % 2]; wk = ("wA", g % 2)
            dma("sp" if g % 2 == 0 else "act", wb, adw[:, :, g * 512:(g + 1) * 512], w=[wk])
            for j in range(4):
                for k in range(8):
                    P.op("pe", lambda e, j=j, k=k, g=g, wb=wb: e.matmul(pb[0][:, g * 4 + j:g * 4 + j + 1],
                         lhsT=wb[:, k, j * 128:(j + 1) * 128], rhs=sccol[:, k:k + 1], start=(k == 0), stop=(k == 7)),
                         r=[wk, "sccol"], w=[("pb0", g * 4 + j)])
        P.op("dve", lambda e: e.tensor_tensor(out=sh1c, in0=pb[0][:, 0:8], in1=abcol[:, 0:8], op=ALU.add),
             r=["pb0", "abcol"], w=["sh1c"])
        P.op("dve", lambda e: e.scalar_tensor_tensor(out=gs1c, in0=pb[0][:, 8:16], scalar=1.0, in1=abcol[:, 8:16],
             op0=ALU.add, op1=ALU.add), r=["pb0", "abcol"], w=["gs1c"])
        P.op("dve", lambda e: e.tensor_tensor(out=gs1c, in0=gs1c, in1=n1g, op=ALU.mult), r=["gs1c", "n1g"], w=["gs1c"])
        dump("sh1c", sh1c, "sh1c"); dump("gs1c", gs1c, "gs1c")
        barrier()
        AR.pop()
        off_base = AR.off
        mixT = AR.get(BF16, [8, T])
        off_mix = AR.off
        hT = AR.get(BF16, [8, LT])
        yT = AR.get(BF16, [8, T])
        win3 = w_in.rearrange("(k p) n -> p k n", p=128)

        def phaseB():
            AR.push()
            xt = [AR.get(F32, [D]), AR.get(F32, [D])]
            junk = AR.get(BF16, [D])
            xnb = [AR.get(BF16, [D]), AR.get(BF16, [D])]
            ss = AR.get(F32, [32]); lnv = AR.get(F32, [32]); rstd = AR.get(F32, [32])
            def tile_steps(i):
                steps = []
                S = lambda *a_, **k_: steps.append((a_, k_))
                b = i % 2
                src = x_pre if i < 16 else x_own
                r0 = (i % 16) * 128
                S("sp" if b == 0 else "act", (lambda e, b=b, r0=r0, src=src: e.dma_start(out=xt[b], in_=src[r0:r0 + 128, :])), w=[("xt", b)], dma=True)
                S("act", lambda e, b=b, i=i: e.activation(out=junk, in_=xt[b], func=AF.Square, accum_out=ss[:, i:i + 1]),
                     r=[("xt", b)], w=["junk", ("ss", i)])
                S("act", lambda e, i=i: e.activation(out=lnv[:, i:i + 1], in_=ss[:, i:i + 1], func=AF.Ln, scale=1.0 / D, bias=EPS),
                     r=[("ss", i)], w=[("lnv", i)])
                S("act", lambda e, i=i: e.activation(out=rstd[:, i:i + 1], in_=lnv[:, i:i + 1], func=AF.Exp, scale=-0.5),
                     r=[("lnv", i)], w=[("rstd", i)])
                S("dve", lambda e, b=b, i=i: e.tensor_scalar(out=xnb[b], in0=xt[b], scalar1=rstd[:, i:i + 1], scalar2=None,
                     op0=ALU.mult), r=[("xt", b), ("rstd", i)], w=[("xnb", b)])
                pbT = pbt[b]
                pk = "pbt%d" % b
                for c in range(8):
                    S("pe", lambda e, b=b, c=c, pbT=pbT: e.transpose(pbT[:, c * 128:(c + 1) * 128],
                         xnb[b][:, c * 128:(c + 1) * 128], ident_bf), r=[("xnb", b), "ident_bf"], w=[(pk, c)])
                for c in range(8):
                    if FLAGS.get("B_noevac") or c >= FLAGS.get("B_nevac", 8):
                        continue
                    dst = hT[:, c, i * 128:(i + 1) * 128]
                    if FLAGS.get("B_dst2"):
                        dst = junk[:, c * 128:(c + 1) * 128]
                    if FLAGS.get("B_evaccopy"):
                        srcp = pb[3][:, 0:128] if FLAGS.get("B_src2") else pbT[:, c * 128:(c + 1) * 128]
                        S("dve", lambda e, c=c, dst=dst, srcp=srcp: e.tensor_copy(out=dst, in_=srcp),
                             r=[pk if FLAGS.get("B_waitall") else (pk, c)], w=[("hT", i)])
                        continue
                    if c % 2 == 0 and not FLAGS.get("B_dveonly"):
                        S("act", lambda e, c=c, dst=dst, pbT=pbT: e.activation(out=dst, in_=pbT[:, c * 128:(c + 1) * 128],
                             func=AF.Identity, scale=gs1c[:, c:c + 1], bias=sh1c[:, c:c + 1]),
                             r=[pk, "gs1c", "sh1c"], w=[("hT", i)])
                    else:
                        S("dve", lambda e, c=c, dst=dst, pbT=pbT: e.tensor_scalar(out=dst, in0=pbT[:, c * 128:(c + 1) * 128],
                             scalar1=gs1c[:, c:c + 1], scalar2=sh1c[:, c:c + 1], op0=ALU.mult, op1=ALU.add),
                             r=[pk, "gs1c", "sh1c"], w=[("hT", i)])
                return steps

            modb = AR.get(F32, [4 * D]); scb = AR.get(F32, [8, 128]); n2gb = AR.get(F32, [D])
            wA = [AR.get(F32, [8, 256]), AR.get(F32, [8, 256])]
            adw = ada_w.rearrange("(k p) n -> p k n", p=128)
            dma("pool", modb, ada_b_row.partition_broadcast(128), w=["modb"])
            dma("pool", n2gb, n2g_row.partition_broadcast(128), w=["n2gb"])
            for k in range(8):
                P.op("dve", lambda e, k=k: e.tensor_copy(out=scb[:, k, :], in_=sccol[:, k:k + 1].to_broadcast([128, 128])),
                     r=["sccol"], w=[("scb", k)])

            def wload(gg):
                return [(("pool", (lambda e, gg=gg: e.dma_start(out=wA[gg % 2], in_=adw[:, :, 2 * D + gg * 256:2 * D + (gg + 1) * 256]))),
                         dict(w=[("wA", gg % 2)], dma=True))]

            def wmm(gg):
                st_ = []
                wb = wA[gg % 2]; wk = ("wA", gg % 2)
                pp = pb[4 + gg % 2]; pk = "pb%d" % (4 + gg % 2)
                for k in range(8):
                    st_.append((("pe", (lambda e, k=k, wb=wb, pp=pp: e.matmul(pp[:, 0:256], lhsT=scb[:, k, :], rhs=wb[:, k, :],
                               start=(k == 0), stop=(k == 7)))), dict(r=[wk, "scb"], w=[pk])))
                st_.append((("dve", (lambda e, gg=gg, pp=pp: e.tensor_tensor(out=modb[:, gg * 256:(gg + 1) * 256],
                           in0=pp[:, 0:256], in1=modb[:, gg * 256:(gg + 1) * 256], op=ALU.add))), dict(r=[pk, "modb"], w=["modb"])))
                return st_

            import itertools
            ch0 = []; ch1 = [None] * 11; ch2 = wload(0) + wload(1) + [None] * 6
            for i2 in range(0, 32, 2):
                ch0 += tile_steps(i2); ch1 += tile_steps(i2 + 1)
                gg = i2 // 2
                grp = wmm(gg) + (wload(gg + 2) if gg + 2 < 16 else [])
                ch2 += grp + [None] * max(0, 22 - len(grp))
            for xs in itertools.zip_longest(ch0, ch1, ch2):
                for x in xs:
                    if x is not None:
                        P.op(*x[0], **x[1])
            gs2b_ = modb[:, 2 * D:3 * D]
            P.op("dve", lambda e: e.scalar_tensor_tensor(out=gs2b_, in0=gs2b_, scalar=1.0, in1=n2gb, op0=ALU.add, op1=ALU.mult),
                 r=["modb", "n2gb"], w=["modb"])
            dma("sp", modb_d, modb, r=["modb"], w=["modb_scr"])
            barrier()
            AR.pop()

        def phaseC():
            AR.push()
            cw = AR.get(F32, [8, 4]); cb = AR.get(F32, [8]); nba = AR.get(F32, [8]); nbx = AR.get(F32, [8])
            lam = AR.get(F32, [8]); s1 = AR.get(F32, [8]); s2 = AR.get(F32, [8])
            wga = AR.get(BF16, [8, 128]); wgx = AR.get(BF16, [8, 128])
            wxr = AR.get(BF16, [8, 512]); wgr = AR.get(BF16, [8, 512])
            BUF = []

            def mkset():
                d_ = dict(
                    xb=AR.get(F32, [515]), hist=AR.get(F32, [3]), xc=AR.get(F32, [512]), xcb=AR.get(BF16, [512]),
                    er=AR.get(F32, [512]), eg=AR.get(F32, [512]), aa=AR.get(F32, [512]), a2=AR.get(F32, [512]),
                    hb=AR.get(F32, [512]), carry=AR.get(F32, [1]), fx=AR.get(F32, [1]))
                d_["uu"] = d_["er"]; d_["gz"] = d_["er"]
                return d_
            save_ = AR.off
            AR.off = off_base
            BUF.append(mkset()); BUF.append(mkset())
            assert AR.off <= off_mix
            AR.off = save_
            BUF.append(mkset()); BUF.append(mkset())
            PB = [(pb[0], "pb0"), (pb[1], "pb1"), (pb[2], "pb2"), (pb[3], "pb3"), (pb[4], "pb4"), (pb[5], "pb5"),
                  (pbt[0][:, :].bitcast(F32), "pbt0"), (pbt[1][:, :].bitcast(F32), "pbt1")]
            dma("sp", cw, convw_col.rearrange("p (a b) -> p a b", a=8), w=["cw"]); dma("sp", cb, convb_col, w=["cb"])
            dma("sp", nba, brga_col, w=["nba"]); dma("sp", nbx, brgx_col, w=["nbx"]); dma("sp", lam, lam_col, w=["lam"])
            dma("pool", wga, w_rg_a.rearrange("n w v -> w n v"), w=["wga"])
            dma("pool", wgx, w_rg_x.rearrange("n w v -> w n v"), w=["wgx"])
            P.op("dve", lambda e: e.tensor_scalar(out=nba, in0=nba, scalar1=-1.0, scalar2=None, op0=ALU.mult), r=["nba"], w=["nba"])
            P.op("dve", lambda e: e.tensor_scalar(out=nbx, in0=nbx, scalar1=-1.0, scalar2=None, op0=ALU.mult), r=["nbx"], w=["nbx"])
            P.op("act", lambda e: e.activation(out=s1, in_=lam, func=AF.Exp, scale=-1.0), r=["lam"], w=["s1"])
            P.op("act", lambda e: e.activation(out=s1, in_=s1, func=AF.Ln, bias=1.0), r=["s1"], w=["s1"])
            P.op("dve", lambda e: e.tensor_scalar(out=s2, in0=s1, scalar1=-16.0, scalar2=None, op0=ALU.mult), r=["s1"], w=["s2"])
            P.op("dve", lambda e: e.tensor_scalar(out=s1, in0=s1, scalar1=-8.0, scalar2=None, op0=ALU.mult), r=["s1", "s2"], w=["s1"])
            GC = 1.5957691216057308

            def piece(c, j, par):
                B = BUF[par]
                steps = []
                S = lambda *a_, **k_: steps.append((a_, k_))
                cc = c % 4
                K = lambda n: (n, par)
                xc, xcb, er, eg, aa, a2, uu, gz, carry, fx = (B[n] for n in ("xc", "xcb", "er", "eg", "aa", "a2", "uu", "gz", "carry", "fx"))
                mm, ig = a2, eg
                xps, xk = PB[2 * par]
                for k in range(8):
                    S("pe", lambda e, k=k, j=j, cc=cc, xps=xps: e.matmul(xps[:, :],
                         lhsT=wxr[:, k, cc * 128:(cc + 1) * 128], rhs=hT[:, k, j * 512:(j + 1) * 512],
                         start=(k == 0), stop=(k == 7)), r=["wxr", "hT"], w=[xk])
                xb = B["xb"]; xbk = ("xb", par); hist = B["hist"]; hk_ = ("hist", par)
                if j == 0:
                    S("dve", lambda e, xb=xb: e.memset(xb[:, 0:3], 0.0), w=[xbk])
                elif j == 4:
                    S("dve", lambda e, xb=xb, hist=hist: e.tensor_scalar(out=xb[:, 0:3], in0=hist,
                         scalar1=hflag[:, 0:1], scalar2=None, op0=ALU.mult), r=[hk_, "hflag"], w=[xbk])
                else:
                    S("dve", lambda e, xb=xb, hist=hist: e.tensor_copy(out=xb[:, 0:3], in_=hist), r=[hk_], w=[xbk])
                S("dve", lambda e, xb=xb, xps=xps: e.tensor_copy(out=xb[:, 3:515], in_=xps[:, :]),
                     r=[xk, xbk], w=[xbk])
                S("dve", lambda e, xb=xb, c=c: e.tensor_scalar(out=xc, in0=xb[:, 3:515], scalar1=cw[:, c, 0:1],
                     scalar2=cb[:, c:c + 1], op0=ALU.mult, op1=ALU.add), r=[xbk, "cw", "cb"], w=[K("xc")])
                for i in (1, 2, 3):
                    S("dve", lambda e, xb=xb, c=c, i=i: e.scalar_tensor_tensor(out=xc, in0=xb[:, 3 - i:515 - i],
                         scalar=cw[:, c, i:i + 1], in1=xc, op0=ALU.mult, op1=ALU.add), r=[xbk, "cw", K("xc")], w=[K("xc")])
                S("dve", lambda e, xb=xb, hist=hist: e.tensor_copy(out=hist, in_=xb[:, 512:515]), r=[xbk], w=[hk_])
                S("pool", lambda e: e.tensor_copy(out=xcb, in_=xc), r=[K("xc")], w=[K("xcb")])
                rps, rk = PB[2 * par + 1]; gps, gk = PB[2 * par]
                S("pe", lambda e, c=c, rps=rps: e.matmul(rps[:, :], lhsT=wga[:, c, :], rhs=xcb, start=True, stop=True),
                     r=["wga", K("xcb")], w=[rk])
                S("pe", lambda e, c=c, gps=gps: e.matmul(gps[:, :], lhsT=wgx[:, c, :], rhs=xcb, start=True, stop=True),
                     r=["wgx", K("xcb")], w=[gk])
                S("act", lambda e, c=c, rps=rps: e.activation(out=er, in_=rps[:, :], func=AF.Exp, scale=-1.0,
                     bias=nba[:, c:c + 1]), r=[rk, "nba"], w=[K("er")])
                S("act", lambda e: e.activation(out=er, in_=er, func=AF.Ln, bias=1.0), r=[K("er")], w=[K("er")])
                S("act", lambda e: e.activation(out=er, in_=er, func=AF.Exp, scale=-1.0), r=[K("er")], w=[K("er")])
                S("act", lambda e, c=c, gps=gps: e.activation(out=eg, in_=gps[:, :], func=AF.Exp, scale=-1.0,
                     bias=nbx[:, c:c + 1]), r=[gk, "nbx"], w=[K("eg")])
                S("act", lambda e: e.activation(out=eg, in_=eg, func=AF.Ln, bias=1.0), r=[K("eg")], w=[K("eg")])
                S("act", lambda e: e.activation(out=eg, in_=eg, func=AF.Exp, scale=-1.0), r=[K("eg")], w=[K("eg")])
                S("act", lambda e, c=c: e.activation(out=aa, in_=er, func=AF.Exp, scale=s1[:, c:c + 1]), r=[K("er"), "s1"], w=[K("aa")])
                S("act", lambda e, c=c: e.activation(out=a2, in_=er, func=AF.Exp, scale=s2[:, c:c + 1]), r=[K("er"), "s2"], w=[K("a2")])
                S("pool", lambda e: e.tensor_scalar(out=mm, in0=a2, scalar1=-1.0, scalar2=1.0, op0=ALU.mult, op1=ALU.add),
                     r=[K("a2")], w=[K("a2")])
                S("act", lambda e: e.activation(out=mm, in_=mm, func=AF.Ln), r=[K("a2")], w=[K("a2")])
                S("act", lambda e: e.activation(out=mm, in_=mm, func=AF.Exp, scale=0.5), r=[K("a2")], w=[K("a2")])
                if j == 0:
                    S("dve", lambda e: e.memset(mm[:, 0:1], 1.0), r=[K("a2")], w=[K("a2")])
                elif j == 4:
                    S("dve", lambda e: e.tensor_scalar(out=fx, in0=mm[:, 0:1], scalar1=-1.0, scalar2=hflag[:, 0:1],
                         op0=ALU.add, op1=ALU.mult), r=[K("a2"), "hflag"], w=[K("fx")])
                    S("dve", lambda e: e.tensor_scalar(out=mm[:, 0:1], in0=fx, scalar1=1.0, scalar2=None, op0=ALU.add),
                         r=[K("fx"), K("a2")], w=[K("a2")])
                S("pool", lambda e: e.tensor_tensor(out=uu, in0=mm, in1=ig, op=ALU.mult), r=[K("a2"), K("eg")], w=[K("er")])
                S("pool", lambda e: e.tensor_tensor(out=uu, in0=uu, in1=xc, op=ALU.mult), r=[K("er"), K("xc")], w=[K("er")])
                hdst = B["hb"]; hk = ("hb", par)
                if j == 0:
                    S("dve", lambda e, hdst=hdst: e.tensor_tensor_scan(out=hdst, data0=aa, data1=uu, initial=0.0,
                         op0=ALU.mult, op1=ALU.add), r=[K("aa"), K("er")], w=[hk])
                else:
                    S("dve", lambda e, hdst=hdst: e.tensor_tensor_scan(out=hdst, data0=aa, data1=uu, initial=carry,
                         op0=ALU.mult, op1=ALU.add), r=[K("aa"), K("er"), K("carry")], w=[hk])
                if j == 3:
                    S("dve", lambda e, hdst=hdst: e.tensor_scalar(out=carry, in0=hdst[:, 511:512], scalar1=hflag[:, 0:1],
                         scalar2=None, op0=ALU.mult), r=[hk, "hflag"], w=[K("carry")])
                elif j < 7:
                    S("dve", lambda e, hdst=hdst: e.tensor_copy(out=carry, in_=hdst[:, 511:512]), r=[hk], w=[K("carry")])
                if j >= 4:
                    jj = j - 4
                    gps2, gk2 = PB[2 * par]
                    for k in range(8):
                        S("pe", lambda e, k=k, jj=jj, cc=cc, gps2=gps2: e.matmul(gps2[:, :],
                             lhsT=wgr[:, k, cc * 128:(cc + 1) * 128], rhs=hT[:, k, T + jj * 512:T + (jj + 1) * 512],
                             start=(k == 0), stop=(k == 7)), r=["wgr", "hT"], w=[gk2])
                    S("act", lambda e, gps2=gps2: e.activation(out=gz, in_=gps2[:, :], func=AF.Square), r=[gk2], w=[K("er")])
                    S("pool", lambda e: e.tensor_scalar(out=gz, in0=gz, scalar1=0.044715, scalar2=1.0, op0=ALU.mult, op1=ALU.add),
                         r=[K("er")], w=[K("er")])
                    S("dve", lambda e, gps2=gps2: e.tensor_tensor(out=gz, in0=gz, in1=gps2[:, :], op=ALU.mult), r=[K("er"), gk2], w=[K("er")])
                    S("act", lambda e: e.activation(out=gz, in_=gz, func=AF.Exp, scale=-GC), r=[K("er")], w=[K("er")])
                    S("act", lambda e: e.activation(out=gz, in_=gz, func=AF.Ln, bias=1.0), r=[K("er")], w=[K("er")])
                    S("act", lambda e: e.activation(out=gz, in_=gz, func=AF.Exp, scale=-1.0), r=[K("er")], w=[K("er")])
                    S("dve", lambda e, gps2=gps2: e.tensor_tensor(out=gz, in0=gz, in1=gps2[:, :], op=ALU.mult), r=[K("er"), gk2], w=[K("er")])
                    S("dve", lambda e, jj=jj, c=c, hdst=hdst: e.tensor_tensor(out=yT[:, c, jj * 512:(jj + 1) * 512],
                         in0=hdst, in1=gz, op=ALU.mult), r=[hk, K("er")], w=[("yT", c)])
                return steps

            import itertools
            xg2 = xg_d[0:NTOT, :].rearrange("(e p r) d -> e p r d", p=128, r=2)
            for e_ in range(NTOT // 256):
                dma("sp", xg2[e_], zt, r=["zt"], w=["Xg"])
            for cg in range(2):
                dma("pool", wxr, win3[:, :, cg * 512:(cg + 1) * 512], w=["wxr"])
                dma("pool", wgr, win3[:, :, D + cg * 512:D + (cg + 1) * 512], w=["wgr"])
                c0 = cg * 4
                sts = []
                for q in range(4):
                    lst = [None] * (q * 12)
                    for j in range(8):
                        lst += piece(c0 + q, j, q)
                    sts.append(lst)
                for xs in itertools.zip_longest(*sts):
                    for x in xs:
                        if x is not None:
                            P.op(*x[0], **x[1])
            barrier()
            AR.pop()

        def merge(g, first):
            AR.push()
            wbr = [AR.get(BF16, [8, 512]), AR.get(BF16, [8, 512])]
            wgl = [AR.get(BF16, [8, 512]), AR.get(BF16, [8, 512])]
            bgc = AR.get(F32, [16]); sg = AR.get(F32, [512]); tmp = AR.get(F32, [512])
            dma("sp", bgc, bgate_col, w=["bgc"])
            wb3 = w_branch[g].rearrange("(k p) n -> p k n", p=128)
            for grp in range(2):
                dma("pool", wbr[grp], wb3[:, :, grp * 512:(grp + 1) * 512], w=[("wbr", grp)])
                c0 = 5 * D + g * D + grp * 512
                dma("pool", wgl[grp], win3[:, :, c0:c0 + 512], w=[("wgl", grp)])
            for m in range(8):
                grp, mm = m // 4, m % 4
                for pc in range(4):
                    zps = pb[pc % 2]; zk = "pb%d" % (pc % 2); gps = pb[2 + pc % 2]; gk = "pb%d" % (2 + pc % 2)
                    for k in range(8):
                        P.op("pe", lambda e, k=k, pc=pc, grp=grp, mm=mm, zps=zps: e.matmul(zps[:, :],
                             lhsT=wbr[grp][:, k, mm * 128:(mm + 1) * 128], rhs=yT[:, k, pc * 512:(pc + 1) * 512],
                             start=(k == 0), stop=(k == 7)), r=[("wbr", grp), "yT"], w=[zk])
                    for k in range(8):
                        P.op("pe", lambda e, k=k, pc=pc, grp=grp, mm=mm, gps=gps: e.matmul(gps[:, :],
                             lhsT=wgl[grp][:, k, mm * 128:(mm + 1) * 128], rhs=hT[:, k, T + pc * 512:T + (pc + 1) * 512],
                             start=(k == 0), stop=(k == 7)), r=[("wgl", grp), "hT"], w=[gk])
                    P.op("act", lambda e, gps=gps, m=m: e.activation(out=sg, in_=gps[:, :], func=AF.Sigmoid,
                         bias=bgc[:, g * 8 + m:g * 8 + m + 1]), r=[gk, "bgc"], w=["sg"])
                    dst = mixT[:, m, pc * 512:(pc + 1) * 512]
                    if first:
                        P.op("dve", lambda e, zps=zps, dst=dst: e.tensor_tensor(out=dst, in0=zps[:, :], in1=sg, op=ALU.mult),
                             r=[zk, "sg"], w=[("mixT", m)])
                    else:
                        P.op("dve", lambda e, zps=zps: e.tensor_tensor(out=tmp, in0=zps[:, :], in1=sg, op=ALU.mult),
                             r=[zk, "sg"], w=["tmp"])
                        P.op("dve", lambda e, dst=dst: e.tensor_tensor(out=dst, in0=dst, in1=tmp, op=ALU.add),
                             r=["tmp", ("mixT", m)], w=[("mixT", m)])
            barrier()
            AR.pop()

        def phaseD():
            merge(0, True)

        def phaseF():
            merge(1, False)

        def phaseE():
            AR.push()
            SCALE = 128.0 ** -0.5
            C32 = AR.get(BF16, [LT], parts=32); S32 = AR.get(BF16, [LT], parts=32)
            qgc = AR.get(F32, [1]); kgc = AR.get(F32, [1]); rmf = AR.get(F32, [32]); fq = AR.get(F32, [2], parts=32)
            dma("sp", qgc, qg_col, w=["qgc"]); dma("sp", kgc, kg_col, w=["kgc"])
            dma("sp", rmf, rm_f_d, w=["rmf"]); dma("sp", fq, freq_d, w=["fq"])
            AR.push()
            posi = AR.get(I32, [1024], parts=32); ang = AR.get(F32, [1024], parts=32); tq = AR.get(F32, [1024], parts=32)
            ki = AR.get(I32, [1024], parts=32); kf = AR.get(F32, [1024], parts=32); rr_ = AR.get(F32, [1024], parts=32)
            ones32 = AR.get(F32, [1], parts=32)
            P.op("dve", lambda e: e.memset(ones32, 1.0), w=["ones32"])
            TWO_PI = 6.283185307179586
            C1 = 6.28125
            C2 = TWO_PI - C1
            for pc in range(4):
                sl = slice(pc * 1024, (pc + 1) * 1024)
                dma("sp", posi, pos[:, sl].partition_broadcast(32), w=["posi"])
                P.op("dve", lambda e: e.tensor_copy(out=ang, in_=posi), r=["posi"], w=["ang"])
                P.op("dve", lambda e: e.tensor_scalar(out=ang, in0=ang, scalar1=fq[:, 0:1], scalar2=None, op0=ALU.mult),
                     r=["ang", "fq"], w=["ang"])
                for which in (0, 1):
                    off = 0.0 if which == 0 else np.pi / 2
                    P.op("dve", lambda e, off=off: e.tensor_scalar(out=tq, in0=ang, scalar1=float(off), scalar2=1.0 / TWO_PI,
                         op0=ALU.add, op1=ALU.mult), r=["ang"], w=["tq"])
                    P.op("dve", lambda e: e.tensor_copy(out=ki, in_=tq), r=["tq"], w=["ki"])
                    P.op("dve", lambda e: e.tensor_copy(out=kf, in_=ki), r=["ki"], w=["kf"])
                    P.op("dve", lambda e, off=off: e.scalar_tensor_tensor(out=rr_, in0=kf, scalar=-C1, in1=ang,
                         op0=ALU.mult, op1=ALU.add), r=["kf", "ang"], w=["rr"])
                    P.op("dve", lambda e, off=off: e.scalar_tensor_tensor(out=rr_, in0=kf, scalar=-C2, in1=rr_,
                         op0=ALU.mult, op1=ALU.add), r=["kf", "rr"], w=["rr"])
                    P.op("dve", lambda e, off=off: e.tensor_scalar(out=rr_, in0=rr_, scalar1=float(off), scalar2=3.14159,
                         op0=ALU.add, op1=ALU.min), r=["rr"], w=["rr"])
                    P.op("dve", lambda e: e.tensor_scalar(out=rr_, in0=rr_, scalar1=-3.14159, scalar2=None, op0=ALU.max),
                         r=["rr"], w=["rr"])
                    if which == 0:
                        P.op("act", lambda e, sl=sl: e.activation(out=S32[:, sl], in_=rr_, func=AF.Sin, scale=fq[:, 1:2]),
                             r=["rr", "fq"], w=[("S32", pc)])
                    else:
                        P.op("act", lambda e, sl=sl: e.activation(out=C32[:, sl], in_=rr_, func=AF.Sin), r=["rr"], w=[("C32", pc)])
            barrier()
            AR.pop()
            kT = AR.get(BF16, [LT]); vb_ = AR.get(BF16, [32, 128]); qT = AR.get(BF16, [T])
            mbT = AR.get(BF16, [T], parts=16); mball = AR.get(BF16, [16, 16])
            wq = AR.get(BF16, [8, 128]); wk_ = AR.get(BF16, [8, 128]); wv = AR.get(BF16, [8, 128])
            kraw2 = [AR.get(F32, [512]), AR.get(F32, [512])]; sq2 = [AR.get(BF16, [512]), AR.get(BF16, [512])]
            lnr = AR.get(F32, [512]); rstd = AR.get(F32, [512])
            t1 = AR.get(F32, [512], parts=32); t2 = AR.get(F32, [512], parts=32)
            kmT = AR.get(F32, [16])
            gb = AR.get(F32, [16]); mx = AR.get(F32, [8]); sel = AR.get(F32, [16])
            pT = [AR.get(BF16, [512]), AR.get(BF16, [512]), AR.get(BF16, [512])]
            rl = AR.get(F32, [512])

            pbtF = [pbt[0][:, :].bitcast(F32), pbt[1][:, :].bitcast(F32)]

            def piece_info(hd, j):
                if j < 8:
                    return wk_, kgc, "kgc", j * 512, kT[:, j * 512:(j + 1) * 512]
                jj = j - 8
                return wq, qgc, "qgc", T + jj * 512, qT[:, jj * 512:(jj + 1) * 512]

            def s1_proj(hd, j):
                wt, gcol, gkey, tok0, dstT = piece_info(hd, j)
                ps = pb[j % 2]; pk = "pb%d" % (j % 2)
                for k in range(8):
                    P.op("pe", lambda e, k=k, ps=ps, wt=wt, tok0=tok0: e.matmul(ps[:, :], lhsT=wt[:, k, :], rhs=hT[:, k, tok0:tok0 + 512],
                         start=(k == 0), stop=(k == 7)), r=["wqkv", "hT"], w=[pk])

            def s1_evac(hd, j):
                ps = pb[j % 2]; pk = "pb%d" % (j % 2)
                kr = kraw2[j % 2]; krk = ("kraw", j % 2); sq_ = sq2[j % 2]; sqk = ("sq", j % 2)
                P.op("act", lambda e, ps=ps, kr=kr: e.activation(out=kr, in_=ps[:, :], func=AF.Copy), r=[pk], w=[krk])
                P.op("act", lambda e, ps=ps, sq_=sq_: e.activation(out=sq_, in_=ps[:, :], func=AF.Square), r=[pk], w=[sqk])

            def s2a(hd, j):
                wt, gcol, gkey, tok0, dstT = piece_info(hd, j)
                kr = kraw2[j % 2]; krk = ("kraw", j % 2); sq_ = sq2[j % 2]; sqk = ("sq", j % 2)
                sp_ = pb[2 + j % 2]; sk = "pb%d" % (2 + j % 2)
                P.op("pe", lambda e, sp_=sp_, sq_=sq_: e.matmul(sp_[:, :], lhsT=ones_bf, rhs=sq_, start=True, stop=True),
                     r=["ones_bf", sqk], w=[sk])
                P.op("act", lambda e, sp_=sp_: e.activation(out=lnr, in_=sp_[:, :], func=AF.Ln, scale=1.0 / 128, bias=EPS),
                     r=[sk], w=["lnr"])
                P.op("act", lambda e: e.activation(out=rstd, in_=lnr, func=AF.Exp, scale=-0.5), r=["lnr"], w=["rstd"])
                P.op("dve", lambda e, kr=kr, gcol=gcol: e.scalar_tensor_tensor(out=kr, in0=kr, scalar=gcol[:, 0:1], in1=rstd,
                     op0=ALU.mult, op1=ALU.mult), r=[krk, gkey, "rstd"], w=[krk])

            def s2b(hd, j):
                wt, gcol, gkey, tok0, dstT = piece_info(hd, j)
                kr = kraw2[j % 2]; krk = ("kraw", j % 2)
                rp = pb[4 + j % 2]; rk = "pb%d" % (4 + j % 2)
                P.op("pe", lambda e, rp=rp, kr=kr: e.matmul(rp[0:32, :], lhsT=rmf, rhs=kr, start=True, stop=True),
                     r=["rmf", krk], w=[rk])
                P.op("dve", lambda e, kr=kr, tok0=tok0: e.tensor_tensor(out=t1, in0=kr[0:32, :], in1=C32[:, tok0:tok0 + 512],
                     op=ALU.mult), r=[krk, "C32"], w=["t1"])
                P.op("dve", lambda e, rp=rp, tok0=tok0: e.tensor_tensor(out=t2, in0=rp[0:32, :], in1=S32[:, tok0:tok0 + 512], op=ALU.mult),
                     r=[rk, "S32"], w=["t2"])
                P.op("dve", lambda e, kr=kr: e.tensor_tensor(out=kr[0:32, :], in0=t1, in1=t2, op=ALU.add), r=["t1", "t2", krk], w=[krk])
                P.op("act", lambda e, kr=kr, dstT=dstT: e.activation(out=dstT, in_=kr, func=AF.Copy), r=[krk], w=["qkT"])
                if j < 8:
                    P.op("dve", lambda e, j=j, kr=kr: e.tensor_reduce(out=kmT[:, 2 * j:2 * j + 2],
                         in_=kr.rearrange("p (b t) -> p b t", b=2), axis=AX.X, op=ALU.add), r=[krk], w=["kmT"])
                else:
                    jj = j - 8
                    gp = pb[4 + j % 2]; gk = rk
                    for qt in range(4):
                        P.op("pe", lambda e, qt=qt, gp=gp, kr=kr: e.matmul(gp[:, 64 + qt * 16:64 + (qt + 1) * 16],
                             lhsT=kr[:, qt * 128:(qt + 1) * 128], rhs=kmT, start=True, stop=True),
                             r=[krk, "kmT"], w=[gk])
                    for qt in range(4):
                        i = jj * 4 + qt
                        P.op("dve", lambda e, qt=qt, i=i, gp=gp: e.tensor_tensor(out=gb, in0=gp[:, 64 + qt * 16:64 + (qt + 1) * 16],
                             in1=vbias[:, i // 2, :], op=ALU.add), r=[gk, "vbias"], w=["gb"])
                        P.op("dve", lambda e: e.max(out=mx, in_=gb), r=["gb"], w=["mx"])
                        P.op("dve", lambda e: e.tensor_scalar(out=sel, in0=gb, scalar1=mx[:, 2:3], scalar2=-1.0,
                             op0=ALU.is_ge, op1=ALU.add), r=["gb", "mx"], w=["sel"])
                        P.op("dve", lambda e, i=i: e.scalar_tensor_tensor(out=mball[:, i, :], in0=sel, scalar=-NEG,
                             in1=vbias[:, i // 2, :], op0=ALU.mult, op1=ALU.min), r=["sel", "vbias"], w=["mball"])

            def v_group(hd, tg):
                vps = pbtF[tg % 2]; vk = "pbt%d" % (tg % 2)
                for tt in range(4):
                    tile_i = tg * 4 + tt
                    for k in range(8):
                        P.op("pe", lambda e, k=k, tt=tt, tile_i=tile_i, vps=vps: e.matmul(vps[:, tt * 128:(tt + 1) * 128],
                             lhsT=hT[:, k, tile_i * 128:(tile_i + 1) * 128], rhs=wv[:, k, :], start=(k == 0), stop=(k == 7)),
                             r=["wqkv", "hT"], w=[vk])
                P.op("act", lambda e, tg=tg, vps=vps: e.activation(out=vb_[:, tg * 4:(tg + 1) * 4, :],
                     in_=vps.rearrange("p (a b) -> p a b", a=4), func=AF.Copy), r=[vk], w=["vb"])

            for hd in range(8):
                dma("pool", wq, win3[:, :, 2 * D + hd * 128:2 * D + (hd + 1) * 128], w=["wqkv"])
                dma("pool", wk_, win3[:, :, 3 * D + hd * 128:3 * D + (hd + 1) * 128], w=["wqkv"])
                dma("pool", wv, win3[:, :, 4 * D + hd * 128:4 * D + (hd + 1) * 128], w=["wqkv"])
                NP_ = 12
                s1_proj(hd, 0)
                s1_evac(hd, 0)
                for j in range(NP_):
                    if j + 1 < NP_:
                        s1_proj(hd, j + 1)
                    if j < 8:
                        v_group(hd, j)
                    if j >= 1:
                        s2b(hd, j - 1)
                    if j + 1 < NP_:
                        s1_evac(hd, j + 1)
                    s2a(hd, j)
                s2b(hd, NP_ - 1)
                for half in range(2):
                    tp = pbt[half]; tk = "pbt%d" % half
                    for ii in range(8):
                        i = half * 8 + ii
                        P.op("pe", lambda e, i=i, ii=ii, tp=tp: e.transpose(tp[0:16, ii * 128:(ii + 1) * 128], mball[:, i, :], ident_bf),
                             r=["mball", "ident_bf"], w=[tk])
                    P.op("dve", lambda e, half=half, tp=tp: e.tensor_copy(out=mbT[:, half * 1024:(half + 1) * 1024], in_=tp[0:16, :]),
                         r=[tk], w=["mbT"])
                for g4 in range(4):
                    qbA = 8 + 2 * g4
                    q0 = g4 * 512
                    ops_ = pb[2 + g4 % 2]; ok = "pb%d" % (2 + g4 % 2)
                    lps = pb[4 + g4 % 2]; lk = "pb%d" % (4 + g4 % 2)
                    chunks = [("A0", 2 * qbA, 0, 512), ("A1", 2 * qbA + 1, 128, 384),
                              ("B0", 2 * qbA + 2, 256, 256), ("B1", 2 * qbA + 3, 384, 128)]
                    chunks += [("P", c, 0, 512) for c in range(2 * qbA)]

                    def scores_pe(ci):
                        kind, kc, co, nq = chunks[ci]
                        sps = pb[ci % 2]; sk = "pb%d" % (ci % 2)
                        qs = q0 + co
                        P.op("pe", lambda e, kc=kc, qs=qs, nq=nq, sps=sps: e.matmul(sps[:, 0:nq], lhsT=kT[:, kc * 128:(kc + 1) * 128],
                             rhs=qT[:, qs:qs + nq], start=True, stop=False), r=["qkT"], w=[sk])
                        if kind == "P":
                            n = kc // 2
                            P.op("pe", lambda e, n=n, qs=qs, nq=nq, sps=sps: e.matmul(sps[:, 0:nq],
                                 lhsT=ident_bf[0:16, n:n + 1].to_broadcast([16, 128]), rhs=mbT[:, qs:qs + nq],
                                 start=False, stop=True), r=["mbT", "ident_bf"], w=[sk])
                        elif kind in ("A0", "A1"):
                            ntri = 256 if kind == "A0" else 128
                            P.op("pe", lambda e, ntri=ntri, sps=sps: e.matmul(sps[:, 0:ntri], lhsT=ident_bf, rhs=tribias[:, 0:ntri],
                                 start=False, stop=False), r=["tribias", "ident_bf"], w=[sk])
                            P.op("pe", lambda e, ntri=ntri, nq=nq, sps=sps, qbA=qbA, q0=q0: e.matmul(sps[:, ntri:nq],
                                 lhsT=ident_bf[0:16, qbA:qbA + 1].to_broadcast([16, 128]), rhs=mbT[:, q0 + 256:q0 + 512],
                                 start=False, stop=True), r=["mbT", "ident_bf"], w=[sk])
                        else:
                            P.op("pe", lambda e, nq=nq, sps=sps: e.matmul(sps[:, 0:nq], lhsT=ident_bf, rhs=tribias[:, 0:nq],
                                 start=False, stop=True), r=["tribias", "ident_bf"], w=[sk])

                    nch = len(chunks)
                    scores_pe(0)
                    for ci in range(nch):
                        kind, kc, co, nq = chunks[ci]
                        sps = pb[ci % 2]; sk = "pb%d" % (ci % 2)
                        ptile = pT[ci % 3]; ptk = ("pT", ci % 3)
                        P.op("act", lambda e, nq=nq, sps=sps, ptile=ptile: e.activation(out=ptile[:, 0:nq], in_=sps[:, 0:nq],
                             func=AF.Exp, scale=SCALE), r=[sk], w=[ptk])
                        if ci + 1 < nch:
                            scores_pe(ci + 1)
                        first, last = (ci == 0), (ci == nch - 1)
                        P.op("pe", lambda e, kc=kc, nq=nq, co=co, ptile=ptile, ops_=ops_, first=first, last=last: e.matmul(
                             ops_[:, co:co + nq], lhsT=vb_[:, kc, :], rhs=ptile[:, 0:nq], start=first, stop=last),
                             r=["vb", ptk], w=[ok])
                        P.op("pe", lambda e, nq=nq, co=co, ptile=ptile, lps=lps, first=first, last=last: e.matmul(
                             lps[:, co:co + nq], lhsT=ones_bf, rhs=ptile[:, 0:nq], start=first, stop=last),
                             r=["ones_bf", ptk], w=[lk])
                    P.op("dve", lambda e, lps=lps: e.reciprocal(out=rl, in_=lps[:, :]), r=[lk], w=["rl"])
                    P.op("dve", lambda e, hd=hd, q0=q0, ops_=ops_: e.tensor_tensor(out=yT[:, hd, q0:q0 + 512], in0=ops_[:, :],
                         in1=rl, op=ALU.mult), r=[ok, "rl"], w=[("yT", hd)])
            barrier()
            AR.pop()

        G = {}

        def phaseG():
            AR.off = off_mix
            AR.push()
            modb = AR.get(F32, [4 * D])
            g1b, sh2b, gs2b, g2b = modb[:, 0:D], modb[:, D:2 * D], modb[:, 2 * D:3 * D], modb[:, 3 * D:4 * D]
            G["g2b"] = g2b
            gates_all = AR.get(F32, [16, 4])
            G["off_keep"] = AR.off
            dma("sp", modb, modb_d, r=["modb_scr"], w=["modb"])
            wout = AR.get(BF16, [8, D])
            xt = [AR.get(F32, [D]), AR.get(F32, [D])]; x1t = [AR.get(F32, [D]), AR.get(F32, [D])]
            h2f = AR.get(F32, [D]); junk = AR.get(BF16, [D])
            h2b_all = AR.get(BF16, [16, D]); h2T = AR.get(F32, [8, 128]); wrf = AR.get(F32, [8, NE])
            brb = AR.get(F32, [NE]); lg = AR.get(F32, [NE]); top8 = AR.get(F32, [8]); idx8 = AR.get(U32, [8])
            ss = AR.get(F32, [16]); lnv = AR.get(F32, [16]); rs2 = AR.get(F32, [16])
            negv = AR.get(F32, [1]); e4 = AR.get(F32, [4]); se = AR.get(F32, [1]); rse = AR.get(F32, [1])
            idxf_all = AR.get(F32, [16, 4]); A_all = AR.get(BF16, [16, NE])
            dest_all = dest_sb[:, :, :]; io32 = AR.get(F32, [NE])
            eq = AR.get(F32, [NE]); s1_ = AR.get(F32, [1]); d1 = AR.get(F32, [1]); ov = AR.get(F32, [1])
            zf = AR.get(F32, [D]); pcol = AR.get(F32, [2]); tt_ = AR.get(F32, [1])
            cnt_b = AR.get(F32, [NE]); heavy_b = AR.get(F32, [NE]); hs_b = AR.get(F32, [NE]); one32 = AR.get(F32, [NE])
            hsk = AR.get(F32, [1]); lt_ = AR.get(F32, [1]); okh = AR.get(F32, [1]); dh_ = AR.get(F32, [1]); ehf = AR.get(F32, [1]); k128 = AR.get(F32, [8]); tmp8 = AR.get(F32, [8]); eq2 = AR.get(F32, [NE])
            eq4 = AR.get(F32, [4, NE]); eq4b = AR.get(F32, [4, NE]); hsk4 = AR.get(F32, [4]); s14 = AR.get(F32, [4]); lt4 = AR.get(F32, [4])
            d4 = AR.get(F32, [4]); dh4 = AR.get(F32, [4]); ok4 = AR.get(F32, [4]); ov4 = AR.get(F32, [4])
            G.update(gates_all=gates_all, dest_all=dest_all, h2b_all=h2b_all)
            dma("pool", wout, w_out.rearrange("(k p) n -> p k n", p=128), w=["wout"])
            dma("sp", wrf, w_router.rearrange("(k p) n -> p k n", p=128), w=["wrf"])
            dma("sp", brb, b_router_row.partition_broadcast(128), w=["brb"])
            dma("sp", io32, iota32_d, w=["io32"]); dma("sp", pcol, pcol_d, w=["pcol"]); dma("sp", k128, k128_d, w=["k128"])
            P.op("dve", lambda e: e.memset(zf, 0.0), w=["zf"])
            dma("sp", yall_d[NTOT:NTOT + 128, :], zf, r=["zf"], w=["Yall"])
            pbtF = [pbt[0][:, :].bitcast(F32), pbt[1][:, :].bitcast(F32)]
            h2f2 = [h2f, AR.get(F32, [D])]; h2T2 = [h2T, AR.get(F32, [8, 128])]
            lg2 = [lg, AR.get(F32, [NE])]; top82 = [top8, AR.get(F32, [8])]; idx82 = [idx8, AR.get(U32, [8])]
            negv2 = [negv, AR.get(F32, [1])]; e42 = [e4, AR.get(F32, [4])]; se2 = [se, AR.get(F32, [1])]; rse2 = [rse, AR.get(F32, [1])]

            def tile_steps(i):
                steps = []
                S = lambda *a_, **k_: steps.append((a_, k_))
                b = i % 2
                K = lambda n: (n, b)
                h2f_, h2T_, lg_, top8_, idx8_, negv_, e4_, se_, rse_ = h2f2[b], h2T2[b], lg2[b], top82[b], idx82[b], negv2[b], e42[b], se2[b], rse2[b]
                S("sp", (lambda e, b=b, i=i: e.dma_start(out=xt[b], in_=x_own[i * 128:(i + 1) * 128, :])), w=[("xt", b)], dma=True)
                ps = pb[b]; pk = "pb%d" % b
                for nb in range(2):
                    for k in range(8):
                        S("pe", lambda e, k=k, nb=nb, i=i, ps=ps: e.matmul(ps[:, :], lhsT=mixT[:, k, i * 128:(i + 1) * 128],
                             rhs=wout[:, k, nb * 512:(nb + 1) * 512], start=(k == 0), stop=(k == 7)), r=["mixT", "wout"], w=[pk])
                    S("dve", lambda e, nb=nb, b=b, ps=ps: e.tensor_tensor(out=x1t[b][:, nb * 512:(nb + 1) * 512], in0=ps[:, :],
                         in1=g1b[:, nb * 512:(nb + 1) * 512], op=ALU.mult), r=[pk, "modb"], w=[("x1t", b)])
                S("dve", lambda e, b=b: e.tensor_tensor(out=x1t[b], in0=x1t[b], in1=xt[b], op=ALU.add),
                     r=[("x1t", b), ("xt", b)], w=[("x1t", b)])
                S("act", (lambda e, b=b, i=i: e.dma_start(out=x1_d[i * 128:(i + 1) * 128, :], in_=x1t[b])), r=[("x1t", b)], w=["x1d"], dma=True)
                S("act", lambda e, b=b, i=i: e.activation(out=junk, in_=x1t[b], func=AF.Square, accum_out=ss[:, i:i + 1]),
                     r=[("x1t", b)], w=["junk", ("ss", i)])
                S("act", lambda e, i=i: e.activation(out=lnv[:, i:i + 1], in_=ss[:, i:i + 1], func=AF.Ln, scale=1.0 / D, bias=EPS),
                     r=[("ss", i)], w=[("lnv", i)])
                S("act", lambda e, i=i: e.activation(out=rs2[:, i:i + 1], in_=lnv[:, i:i + 1], func=AF.Exp, scale=-0.5),
                     r=[("lnv", i)], w=[("rs2", i)])
                S("dve", lambda e, b=b, i=i: e.scalar_tensor_tensor(out=h2f_, in0=x1t[b], scalar=rs2[:, i:i + 1], in1=gs2b,
                     op0=ALU.mult, op1=ALU.mult), r=[("x1t", b), ("rs2", i), "modb"], w=[K("h2f")])
                S("dve", lambda e: e.tensor_tensor(out=h2f_, in0=h2f_, in1=sh2b, op=ALU.add), r=[K("h2f"), "modb"], w=[K("h2f")])
                S("act", lambda e, i=i: e.activation(out=h2b_all[:, i, :], in_=h2f_, func=AF.Copy), r=[K("h2f")], w=[("h2b", i)])
                tpb = [pb[2 + 2 * b], pb[3 + 2 * b]]; tkk = ["pb%d" % (2 + 2 * b), "pb%d" % (3 + 2 * b)]
                for c in range(8):
                    tp = tpb[c // 4]; tk = tkk[c // 4]
                    S("pe", lambda e, c=c, tp=tp: e.transpose(tp[:, (c % 4) * 128:(c % 4 + 1) * 128],
                         h2f_[:, c * 128:(c + 1) * 128], ident_f), r=[K("h2f"), "ident_f"], w=[tk])
                S("act", lambda e: e.activation(out=h2T_[:, 0:4, :], in_=tpb[0][:, :].rearrange("p (a b) -> p a b", a=4),
                     func=AF.Copy), r=[tkk[0]], w=[("h2T", b, 0)])
                S("dve", lambda e: e.tensor_copy(out=h2T_[:, 4:8, :], in_=tpb[1][:, :].rearrange("p (a b) -> p a b", a=4)),
                     r=[tkk[1]], w=[("h2T", b, 1)])
                lp = pbtF[b]; lpk = "pbt%d" % b
                for k in range(8):
                    S("pe", lambda e, k=k: e.matmul(lp[:, 0:NE], lhsT=h2T_[:, k, :], rhs=wrf[:, k, :],
                         start=(k == 0), stop=(k == 7)), r=[("h2T", b, 0), ("h2T", b, 1), "wrf"], w=[lpk])
                S("dve", lambda e: e.tensor_tensor(out=lg_, in0=lp[:, 0:NE], in1=brb, op=ALU.add), r=[lpk, "brb"], w=[K("lg")])
                S("dve", lambda e: e.max(out=top8_, in_=lg_), r=[K("lg")], w=[K("top8")])
                S("dve", lambda e: e.max_index(out=idx8_, in_max=top8_, in_values=lg_), r=[K("lg"), K("top8")], w=[K("idx8")])
                S("dve", lambda e: e.tensor_scalar(out=negv_, in0=top8_[:, 0:1], scalar1=-1.0, scalar2=None, op0=ALU.mult),
                     r=[K("top8")], w=[K("negv")])
                S("act", lambda e: e.activation(out=e4_, in_=top8_[:, 0:4], func=AF.Exp, bias=negv_[:, 0:1], accum_out=se_),
                     r=[K("top8"), K("negv")], w=[K("e4"), K("se")])
                S("dve", lambda e: e.reciprocal(out=rse_, in_=se_), r=[K("se")], w=[K("rse")])
                S("dve", lambda e, i=i: e.tensor_scalar(out=gates_all[:, i, :], in0=e4_, scalar1=rse_[:, 0:1], scalar2=None,
                     op0=ALU.mult), r=[K("e4"), K("rse")], w=[("gates", i)])
                S("dve", lambda e, i=i: e.tensor_scalar(out=A_all[:, i, :], in0=lg_, scalar1=top8_[:, 3:4], scalar2=None,
                     op0=ALU.is_ge), r=[K("lg"), K("top8")], w=[("A", i)])
                S("dve", lambda e, i=i: e.tensor_copy(out=idxf_all[:, i, :], in_=idx8_[:, 0:4]), r=[K("idx8")], w=[("idxf", i)])
                return steps

            import itertools
            ch0 = []; ch1 = [None] * 24
            for i2 in range(0, NT, 2):
                ch0 += tile_steps(i2); ch1 += tile_steps(i2 + 1)
            for xs in itertools.zip_longest(ch0, ch1):
                for x in xs:
                    if x is not None:
                        P.op(*x[0], **x[1])
            for i in range(NT):
                P.op("pe", lambda e, i=i: e.matmul(pb[0][:, 0:NE], lhsT=ones_bf, rhs=A_all[:, i, :], start=(i == 0), stop=(i == NT - 1)),
                     r=["ones_bf", "A"], w=["pb0"])
            P.op("dve", lambda e: e.tensor_copy(out=cnt_b, in_=pb[0][:, 0:NE]), r=["pb0"], w=["cnt_b"])
            P.op("dve", lambda e: e.tensor_scalar(out=heavy_b, in0=cnt_b, scalar1=float(CAP), scalar2=None, op0=ALU.is_gt),
                 r=["cnt_b"], w=["heavy_b"])
            P.op("dve", lambda e: e.memset(one32, 1.0), w=["one32"])
            P.op("dve", lambda e: e.tensor_tensor_scan(out=hs_b, data0=one32, data1=heavy_b, initial=0.0, op0=ALU.mult, op1=ALU.add),
                 r=["one32", "heavy_b"], w=["hs_b"])
            P.op("dve", lambda e: e.tensor_tensor(out=hs_b, in0=hs_b, in1=heavy_b, op=ALU.subtract), r=["hs_b", "heavy_b"], w=["hs_b"])
            for h_ in range(NH):
                P.op("dve", lambda e, h_=h_: e.tensor_scalar(out=eq2, in0=hs_b, scalar1=float(h_), scalar2=None, op0=ALU.is_equal),
                     r=["hs_b"], w=["eq2"])
                P.op("dve", lambda e: e.tensor_tensor(out=eq2, in0=eq2, in1=heavy_b, op=ALU.mult), r=["eq2", "heavy_b"], w=["eq2"])
                P.op("dve", lambda e: e.tensor_tensor(out=eq2, in0=eq2, in1=io32, op=ALU.mult), r=["eq2", "io32"], w=["eq2"])
                P.op("dve", lambda e: e.tensor_reduce(out=ehf, in_=eq2, axis=AX.X, op=ALU.add), r=["eq2"], w=["ehf"])
                P.op("dve", lambda e: e.scalar_tensor_tensor(out=dh_, in0=ehf, scalar=1024.0, in1=pcol[:, 1:2], op0=ALU.mult, op1=ALU.add),
                     r=["ehf", "pcol"], w=["dh_"])
                P.op("dve", lambda e: e.tensor_scalar(out=tmp8, in0=k128, scalar1=dh_[:, 0:1], scalar2=None, op0=ALU.add),
                     r=["k128", "dh_"], w=["tmp8"])
                P.op("dve", lambda e, h_=h_: e.tensor_copy(out=idxw_sb[:, h_, :], in_=tmp8), r=["tmp8"], w=[("idxw", h_)])
                P.op("dve", lambda e: e.scalar_tensor_tensor(out=dh_, in0=ehf, scalar=128.0, in1=pcol[:, 1:2], op0=ALU.mult, op1=ALU.add),
                     r=["ehf", "pcol"], w=["dh_"])
                P.op("dve", lambda e, h_=h_: e.tensor_copy(out=idxb_sb[:, h_, 0:1], in_=dh_), r=["dh_"], w=[("idxb", h_)])
                P.op("dve", lambda e, h_=h_: e.tensor_copy(out=idxb_sb[:, h_, 1:2], in_=ehf), r=["ehf"], w=[("idxb", h_)])
            for i in range(NT):
                P.op("pe", lambda e, i=i: e.matmul(pb[5][:, 0:NE], lhsT=triu_bf, rhs=A_all[:, i, :], start=True, stop=(i == 0)),
                     r=["triu_bf", "A"], w=["pb5"])
                for j in range(i):
                    P.op("pe", lambda e, j=j, i=i: e.matmul(pb[5][:, 0:NE], lhsT=ones_bf, rhs=A_all[:, j, :], start=False,
                         stop=(j == i - 1)), r=["ones_bf", "A"], w=["pb5"])
                idx4 = idxf_all[:, i, :]
                P.op("dve", lambda e, idx4=idx4: e.tensor_tensor(out=eq4, in0=io32.unsqueeze(1).to_broadcast([128, 4, NE]),
                     in1=idx4.unsqueeze(2).to_broadcast([128, 4, NE]), op=ALU.is_equal), r=["io32", ("idxf", i)], w=["eq4"])
                P.op("dve", lambda e: e.tensor_tensor(out=eq4b, in0=eq4, in1=hs_b.unsqueeze(1).to_broadcast([128, 4, NE]), op=ALU.mult),
                     r=["eq4", "hs_b"], w=["eq4b"])
                P.op("dve", lambda e: e.tensor_reduce(out=hsk4, in_=eq4b, axis=AX.X, op=ALU.add), r=["eq4b"], w=["hsk4"])
                P.op("dve", lambda e: e.tensor_tensor(out=eq4, in0=eq4, in1=pb[5][:, 0:NE].unsqueeze(1).to_broadcast([128, 4, NE]),
                     op=ALU.mult), r=["eq4", "pb5"], w=["eq4"])
                P.op("dve", lambda e: e.tensor_reduce(out=s14, in_=eq4, axis=AX.X, op=ALU.add), r=["eq4"], w=["s14"])
                P.op("dve", lambda e: e.tensor_scalar(out=lt4, in0=s14, scalar1=float(CAP), scalar2=None, op0=ALU.is_le), r=["s14"], w=["lt4"])
                P.op("dve", lambda e, idx4=idx4: e.scalar_tensor_tensor(out=d4, in0=idx4, scalar=float(CAP), in1=s14, op0=ALU.mult, op1=ALU.add),
                     r=[("idxf", i), "s14"], w=["d4"])
                P.op("dve", lambda e: e.scalar_tensor_tensor(out=dh4, in0=hsk4, scalar=float(CH), in1=s14, op0=ALU.mult, op1=ALU.add),
                     r=["hsk4", "s14"], w=["dh4"])
                P.op("dve", lambda e: e.tensor_scalar(out=ok4, in0=hsk4, scalar1=float(NH) - 0.5, scalar2=None, op0=ALU.is_lt), r=["hsk4"], w=["ok4"])
                P.op("dve", lambda e: e.tensor_scalar(out=ov4, in0=s14, scalar1=float(CAP + CH), scalar2=None, op0=ALU.is_le), r=["s14"], w=["ov4"])
                P.op("dve", lambda e: e.tensor_tensor(out=ok4, in0=ok4, in1=ov4, op=ALU.mult), r=["ok4", "ov4"], w=["ok4"])
                P.op("dve", lambda e: e.tensor_scalar(out=ov4, in0=lt4, scalar1=-1.0, scalar2=1.0, op0=ALU.mult, op1=ALU.add), r=["lt4"], w=["ov4"])
                P.op("dve", lambda e: e.tensor_tensor(out=ok4, in0=ok4, in1=ov4, op=ALU.mult), r=["ok4", "ov4"], w=["ok4"])
                P.op("dve", lambda e: e.tensor_scalar(out=d4, in0=d4, scalar1=-1.0, scalar2=pcol[:, 0:1], op0=ALU.add, op1=ALU.subtract),
                     r=["d4", "pcol"], w=["d4"])
                P.op("dve", lambda e: e.tensor_scalar(out=dh4, in0=dh4, scalar1=float(NE * CAP - CAP - 1), scalar2=pcol[:, 0:1],
                     op0=ALU.add, op1=ALU.subtract), r=["dh4", "pcol"], w=["dh4"])
                P.op("dve", lambda e: e.tensor_tensor(out=d4, in0=d4, in1=lt4, op=ALU.mult), r=["d4", "lt4"], w=["d4"])
                P.op("dve", lambda e: e.tensor_tensor(out=dh4, in0=dh4, in1=ok4, op=ALU.mult), r=["dh4", "ok4"], w=["dh4"])
                P.op("dve", lambda e: e.tensor_tensor(out=d4, in0=d4, in1=dh4, op=ALU.add), r=["d4", "dh4"], w=["d4"])
                P.op("dve", lambda e: e.tensor_scalar(out=d4, in0=d4, scalar1=pcol[:, 0:1], scalar2=None, op0=ALU.add), r=["d4", "pcol"], w=["d4"])
                P.op("dve", lambda e, i=i: e.tensor_copy(out=dest_all[:, i, :], in_=d4), r=["d4"], w=[("dest", i)])
            for i in range(NT):
                sg_ = stage_sb[i % 2]; sgk = ("stage", i % 2)
                P.op("act", lambda e, i=i, sg_=sg_: e.activation(out=sg_[:, :], in_=h2b_all[:, i, :], func=AF.Copy),
                     r=[("h2b", i)], w=[sgk])
                for k in range(4):
                    P.op("pool", lambda e, i=i, k=k, sg_=sg_: e.indirect_dma_start(out=xg_d[:, :],
                         out_offset=bass.IndirectOffsetOnAxis(ap=dest_sb[:, i, k:k + 1], axis=0), in_=sg_[:, :],
                         in_offset=None),
                         r=[("dest", i), sgk], w=["Xg"], dma=True)
            if "gates" in dbg_out:
                finals.append(dma("sp", dbg_out["gates"], gates_all, r=["gates"]))
                finals.append(dma("sp", dbg_out["idxf"], idxf_all, r=["idxf"]))
                P.op("dve", lambda e: e.tensor_copy(out=gates_all, in_=dest_all), r=["dest", "gates"], w=["gates"])
                finals.append(dma("sp", dbg_out["dest"], gates_all, r=["gates"]))
            barrier()

        def phaseI():
            NS = CAP // 128
            AR.off = off_base
            xe = AR.get(BF16, [NS, D]); actT = AR.get(BF16, [8, CAP])
            assert AR.off <= off_mix
            AR.off = G["off_keep"]
            AR.push()
            wup = [AR.get(BF16, [8, 2 * D]), AR.get(BF16, [8, 2 * D])]
            wdn = [AR.get(BF16, [8, D]), AR.get(BF16, [8, D])]
            xeT = AR.get(BF16, [8, CAP])
            ye = [AR.get(F32, [D]), AR.get(F32, [D])]
            bdn = [AR.get(F32, [D]), AR.get(F32, [D])]
            bupc = AR.get(F32, [NE, 16]); buph = AR.get(F32, [NH, 16])
            gm = [AR.get(F32, [512]), AR.get(F32, [512])]; sgm = [AR.get(F32, [512]), AR.get(F32, [512])]
            l1 = [AR.get(F32, [512]), AR.get(F32, [512])]
            dma("sp", bupc, bup_col.rearrange("p (a b) -> p a b", a=NE), w=["bupc"])
            dbanks = [(pb[4], "pb4"), (pb[5], "pb5"), (pbt[0][:, :].bitcast(F32), "pbt0"), (pbt[1][:, :].bitcast(F32), "pbt1")]
            wupf = w_up.rearrange("e k n -> (e k) n"); wdnf = w_down.rearrange("e k n -> (e k) n")

            def load_w(e_, wb):
                wu3 = w_up[e_].rearrange("(k p) n -> p k n", p=128)
                for hh in range(2):
                    dma("pool", wup[wb][:, :, hh * D:(hh + 1) * D], wu3[:, :, hh * D:(hh + 1) * D], w=[("wup", wb)])
                dma("pool", wdn[wb], w_down[e_].rearrange("(k p) n -> p k n", p=128), w=[("wdn", wb)])
                dma("act", bdn[wb], b_down[e_:e_ + 1, :].partition_broadcast(128), w=[("bdn", wb)])

            def load_w_heavy(h_, wb):
                for k in range(8):
                    P.op("pool", lambda e, k=k: e.indirect_dma_start(out=wup[wb][:, k, :], out_offset=None, in_=wupf[:, :],
                         in_offset=bass.IndirectOffsetOnAxis(ap=idxw_sb[:, h_, k:k + 1], axis=0)), r=[("idxw", h_)], w=[("wup", wb)], dma=True)
                for k in range(8):
                    P.op("pool", lambda e, k=k: e.indirect_dma_start(out=wdn[wb][:, k, :], out_offset=None, in_=wdnf[:, :],
                         in_offset=bass.IndirectOffsetOnAxis(ap=idxw_sb[:, h_, k:k + 1], axis=0)), r=[("idxw", h_)], w=[("wdn", wb)], dma=True)
                P.op("pool", lambda e: e.indirect_dma_start(out=buph[:, h_, :], out_offset=None, in_=bq_d[:, :],
                     in_offset=bass.IndirectOffsetOnAxis(ap=idxb_sb[:, h_, 0:1], axis=0)), r=[("idxb", h_)], w=[("buph", h_)], dma=True)
                P.op("pool", lambda e: e.indirect_dma_start(out=bdn[wb], out_offset=None, in_=b_down[:, :],
                     in_offset=bass.IndirectOffsetOnAxis(ap=idxb_sb[:, h_, 1:2], axis=0)), r=[("idxb", h_)], w=[("bdn", wb)], dma=True)

            def transposes(row0, ns):
                dma("sp", xe[:, 0:ns, :], xg_d[row0:row0 + ns * 128, :].rearrange("(s p) d -> p s d", p=128), r=["Xg"], w=["xe"])
                for c in range(8):
                    tp = pbt[c % 2]; tk = "pbt%d" % (c % 2)
                    for s_ in range(ns):
                        P.op("pe", lambda e, c=c, s_=s_, tp=tp: e.transpose(tp[:, s_ * 128:(s_ + 1) * 128],
                             xe[:, s_, c * 128:(c + 1) * 128], ident_bf), r=["xe", "ident_bf"], w=[tk])
                    if c % 2 == 0:
                        P.op("act", lambda e, c=c, tp=tp: e.activation(out=xeT[:, c, 0:ns * 128], in_=tp[:, 0:ns * 128], func=AF.Copy),
                             r=[tk], w=[("xeT", c)])
                    else:
                        P.op("dve", lambda e, c=c, tp=tp: e.tensor_copy(out=xeT[:, c, 0:ns * 128], in_=tp[:, 0:ns * 128]), r=[tk], w=[("xeT", c)])

            def up(wb, nn, bcol, bkey):
                n0 = 0
                for f in range(8):
                    bb = f % 2
                    gps = pb[bb * 2]; gk = "pb%d" % (bb * 2); lps = pb[bb * 2 + 1]; lk = "pb%d" % (bb * 2 + 1)
                    for k in range(8):
                        P.op("pe", lambda e, k=k, f=f, gps=gps: e.matmul(gps[:, 0:nn],
                             lhsT=wup[wb][:, k, f * 128:(f + 1) * 128], rhs=xeT[:, k, n0:n0 + nn], start=(k == 0), stop=(k == 7)),
                             r=[("wup", wb), "xeT"], w=[gk])
                    for k in range(8):
                        P.op("pe", lambda e, k=k, f=f, lps=lps: e.matmul(lps[:, 0:nn],
                             lhsT=wup[wb][:, k, D + f * 128:D + (f + 1) * 128], rhs=xeT[:, k, n0:n0 + nn], start=(k == 0),
                             stop=(k == 7)), r=[("wup", wb), "xeT"], w=[lk])
                    g_, s__, l_ = gm[bb], sgm[bb], l1[bb]
                    P.op("dve", lambda e, f=f, gps=gps, g_=g_: e.tensor_scalar(out=g_[:, 0:nn], in0=gps[:, 0:nn],
                         scalar1=bcol(f), scalar2=7.0, op0=ALU.add, op1=ALU.min), r=[gk, bkey], w=[("gm", bb)])
                    P.op("act", lambda e, g_=g_, s__=s__: e.activation(out=s__[:, 0:nn], in_=g_[:, 0:nn], func=AF.Sigmoid,
                         scale=1.702), r=[("gm", bb)], w=[("sgm", bb)])
                    P.op("dve", lambda e, f=f, lps=lps, l_=l_: e.tensor_scalar(out=l_[:, 0:nn], in0=lps[:, 0:nn],
                         scalar1=bcol(8 + f), scalar2=7.0, op0=ALU.add, op1=ALU.min), r=[lk, bkey], w=[("l1", bb)])
                    P.op("dve", lambda e, l_=l_: e.tensor_scalar(out=l_[:, 0:nn], in0=l_[:, 0:nn], scalar1=-7.0, scalar2=1.0,
                         op0=ALU.max, op1=ALU.add), r=[("l1", bb)], w=[("l1", bb)])
                    P.op("dve", lambda e, g_=g_, s__=s__: e.tensor_tensor(out=g_[:, 0:nn], in0=g_[:, 0:nn], in1=s__[:, 0:nn],
                         op=ALU.mult), r=[("gm", bb), ("sgm", bb)], w=[("gm", bb)])
                    P.op("dve", lambda e, f=f, g_=g_, l_=l_: e.tensor_tensor(out=actT[:, f, n0:n0 + nn],
                         in0=g_[:, 0:nn], in1=l_[:, 0:nn], op=ALU.mult), r=[("gm", bb), ("l1", bb)], w=[("actT", f)])

            def down(wb, row0, ns):
                for s_ in range(ns):
                    yb = ye[s_ % 2]; yk = ("ye", s_ % 2)
                    for nb in range(2):
                        ps, pk = dbanks[(s_ * 2 + nb) % 4]
                        for f in range(8):
                            P.op("pe", lambda e, f=f, s_=s_, nb=nb, ps=ps: e.matmul(ps[:, :], lhsT=actT[:, f, s_ * 128:(s_ + 1) * 128],
                                 rhs=wdn[wb][:, f, nb * 512:(nb + 1) * 512], start=(f == 0), stop=(f == 7)),
                                 r=["actT", ("wdn", wb)], w=[pk])
                        P.op("dve", lambda e, nb=nb, ps=ps, yb=yb: e.tensor_tensor(out=yb[:, nb * 512:(nb + 1) * 512], in0=ps[:, :],
                             in1=bdn[wb][:, nb * 512:(nb + 1) * 512], op=ALU.add), r=[pk, ("bdn", wb)], w=[yk])
                    r0 = row0 + s_ * 128
                    dma("sp" if s_ % 2 == 0 else "act", yall_d[r0:r0 + 128, :], yb, r=[yk], w=["Yall"])

            NU = NE + NH
            def unit(u):
                if u < NE:
                    return dict(row0=u * CAP, ns=NS, nn=CAP, bcol=(lambda f, u=u: bupc[:, u, f:f + 1]), bkey="bupc")
                h_ = u - NE
                return dict(row0=NE * CAP + h_ * CH, ns=CH // 128, nn=CH, bcol=(lambda f, h_=h_: buph[:, h_, f:f + 1]), bkey=("buph", h_))

            def load_unit(u, wb):
                if u < NE:
                    load_w(u, wb)
                else:
                    load_w_heavy(u - NE, wb)

            hpos = {6: 0, 13: 1, 20: 2, 27: 3}
            order = []
            for e_ in range(NE):
                order.append(e_)
                if e_ in hpos:
                    order.append(NE + hpos[e_])
            load_unit(order[0], 0)
            transposes(unit(order[0])["row0"], unit(order[0])["ns"])
            for pos_, u in enumerate(order):
                U = unit(u)
                wb = pos_ % 2
                if pos_ + 1 < len(order):
                    load_unit(order[pos_ + 1], (pos_ + 1) % 2)
                up(wb, U["nn"], U["bcol"], U["bkey"])
                if pos_ + 1 < len(order):
                    U2 = unit(order[pos_ + 1])
                    transposes(U2["row0"], U2["ns"])
                down(wb, U["row0"], U["ns"])
            barrier()
            AR.pop()

        def phaseJ():
            AR.off = G["off_keep"]
            AR.push()
            gates_all = G["gates_all"]; g2b = G["g2b"]
            yg = [[AR.get(F32, [D]) for _ in range(4)] for _ in range(2)]
            x1t = [AR.get(F32, [D]), AR.get(F32, [D])]; acc = [AR.get(F32, [D]), AR.get(F32, [D])]
            for i in range(NT):
                b = i % 2
                dma("sp", x1t[b], x1_d[i * 128:(i + 1) * 128, :], r=["x1d"], w=[("x1t", b)])
                for k in range(4):
                    P.op("pool", lambda e, i=i, k=k, b=b: e.indirect_dma_start(out=yg[b][k], out_offset=None, in_=yall_d[:, :],
                         in_offset=bass.IndirectOffsetOnAxis(ap=dest_sb[:, i, k:k + 1], axis=0)),
                         r=["Yall", ("dest", i)], w=[("yg", b, k)], dma=True)
                a_ = acc[b]; ak = ("acc", b)
                P.op("dve", lambda e, i=i, b=b, a_=a_: e.tensor_scalar(out=a_, in0=yg[b][0], scalar1=gates_all[:, i, 0:1], scalar2=None,
                     op0=ALU.mult), r=[("yg", b, 0), "gates"], w=[ak])
                for k in (1, 2, 3):
                    P.op("dve", lambda e, i=i, b=b, k=k, a_=a_: e.scalar_tensor_tensor(out=a_, in0=yg[b][k],
                         scalar=gates_all[:, i, k:k + 1], in1=a_, op0=ALU.mult, op1=ALU.add), r=[("yg", b, k), "gates", ak], w=[ak])
                P.op("dve", lambda e, a_=a_: e.tensor_tensor(out=a_, in0=a_, in1=g2b, op=ALU.mult), r=[ak, "modb"], w=[ak])
                P.op("dve", lambda e, b=b, a_=a_: e.tensor_tensor(out=a_, in0=a_, in1=x1t[b], op=ALU.add), r=[ak, ("x1t", b)], w=[ak])
                finals.append(dma("act", out_d[i * 128:(i + 1) * 128, :], a_, r=[ak]))
            AR.pop()

        phases = [phaseB, phaseC, phaseD, phaseE, phaseF, phaseG, phaseI, phaseJ]
        for i, ph in enumerate(phases):
            if stage < i + 2:
                break
            ph()
        def dump_bf(name, src, n):
            if name not in dbg_out:
                return
            AR.push()
            stg = [AR.get(F32, [n]), AR.get(F32, [n])]
            for k in range(8):
                P.op("dve", lambda e, k=k: e.tensor_copy(out=stg[k % 2], in_=src[:, k, :]), r=[name], w=[("stg", k % 2)])
                finals.append(dma("sp", dbg_out[name][:, k, :], stg[k % 2], r=[("stg", k % 2)]))
            AR.pop()
        if stage < 7:
            dump_bf("hT", hT, LT); dump_bf("yT", yT, T); dump_bf("mixT", mixT, T)
        if "x1" in dbg_out:
            AR.push()
            stg = AR.get(F32, [D])
            for i in range(NT):
                dma("sp", stg, x1_d[i * 128:(i + 1) * 128, :], r=["x1d"], w=["stg"])
                finals.append(dma("sp", dbg_out["x1"][i * 128:(i + 1) * 128, :], stg, r=["stg"]))
            AR.pop()
        P.emit(finals)
    return nc


def _consts():
    bf = ml_dtypes.bfloat16
    c = {}
    c["ident_bf"] = np.eye(128, dtype=np.float32).astype(bf)
    c["ident_f"] = np.eye(128, dtype=np.float32)
    k = np.arange(128)
    c["triu_bf"] = (k[:, None] <= k[None, :]).astype(np.float32).astype(bf)
    tb = np.zeros((128, 256), np.float32)
    tb[:, :128] = np.where(k[:, None] <= k[None, :], 0.0, NEG)
    c["tribias"] = tb.astype(bf)
    rm = np.zeros((128, 32), np.float32)
    for m in range(16):
        rm[m + 16, m] = 1.0
        rm[m, m + 16] = 1.0
    c["rm_f"] = rm
    half = 16
    freqs = (np.float32(500000.0) ** (-np.arange(half, dtype=np.float32) / np.float32(half))).astype(np.float32)
    fc = np.zeros((32, 2), np.float32)
    fc[:, 0] = np.concatenate([freqs, freqs])
    fc[:16, 1] = -1.0
    fc[16:, 1] = 1.0
    c["freq_col"] = fc
    c["iota32"] = np.tile(np.arange(NE, dtype=np.float32)[None, :], (128, 1))
    c["pcol"] = np.stack([NTOT + np.arange(128, dtype=np.float32), np.arange(128, dtype=np.float32)], axis=1).copy()
    c["k128"] = np.tile((128.0 * np.arange(8, dtype=np.float32))[None, :], (128, 1))
    return c


def _col(v, nchunk):
    return np.ascontiguousarray(np.asarray(v, np.float32).reshape(nchunk, 128).T)


def prep_inputs(inp):
    f = lambda a: np.ascontiguousarray(np.asarray(a, np.float32))
    x = f(inp["x"]); c = f(inp["c"]); positions = np.asarray(inp["positions"]).astype(np.int32)
    shared = {
        "ada_w": f(inp["ada_w"][0]),
        "ada_b_col": _col(inp["ada_b"][0], 48),
        "ada_b_row": f(inp["ada_b"][0][None, 2 * D:]),
        "n1g_col": _col(inp["norm1_g"][0], 8),
        "n2g_row": f(inp["norm2_g"][0][None, :]),
        "w_in": f(inp["w_in"][0]),
        "convw_col": np.ascontiguousarray(f(inp["conv_w"][0]).T.reshape(8, 128, 4).transpose(1, 0, 2).reshape(128, 32)),
        "convb_col": _col(inp["conv_b"][0], 8),
        "w_rg_a": f(inp["w_rg_a"][0]), "brga_col": _col(inp["b_rg_a"][0], 8),
        "w_rg_x": f(inp["w_rg_x"][0]), "brgx_col": _col(inp["b_rg_x"][0], 8),
        "lam_col": _col(inp["lru_lambda"][0], 8),
        "qg_col": f(inp["q_norm_g"][0][:, None]), "kg_col": f(inp["k_norm_g"][0][:, None]),
        "bgate_col": np.ascontiguousarray(f(inp["b_gate"][0]).reshape(2, 8, 128).transpose(2, 0, 1).reshape(128, 16)),
        "w_branch": f(inp["w_branch"][0]), "w_out": f(inp["w_out"][0]),
        "w_router": f(inp["w_router"][0]), "b_router_row": f(inp["b_router"][0][None, :]),
        "w_up": f(inp["w_up"][0]),
        "bup_col": np.ascontiguousarray(f(inp["b_up"][0]).reshape(NE, 16, 128).transpose(2, 0, 1).reshape(128, NE * 16)),
        "w_down": f(inp["w_down"][0]), "b_down": f(inp["b_down"][0]),
        "bq": np.ascontiguousarray(f(inp["b_up"][0]).reshape(NE, 16, 128).transpose(0, 2, 1).reshape(NE * 128, 16)),
    }
    shared.update(_consts())
    maps = []
    for core in range(8):
        b, h = core // 2, core % 2
        m = dict(shared)
        m["x_own"] = np.ascontiguousarray(x[b, h * T:(h + 1) * T])
        m["x_pre"] = np.ascontiguousarray(x[b, 0:T])
        m["pos"] = np.ascontiguousarray(np.concatenate([positions[b, 0:T], positions[b, h * T:(h + 1) * T]])[None, :])
        m["c_col"] = _col(c[b], 8)
        m["hflag"] = np.full((128, 1), float(h), np.float32)
        vb = np.full((8, 16), NEG, np.float32)
        for j in range(8):
            for n in range(16):
                if n < 8 + j and (n >= 8 or h == 1):
                    vb[j, n] = 0.0
        m["vbias"] = np.ascontiguousarray(np.tile(vb.reshape(1, 128), (128, 1)))
        maps.append(m)
    return maps


_NC_CACHE = {}


def kernel(**inputs):
    maps = prep_inputs(inputs)
    if "nc" not in _NC_CACHE:
        _NC_CACHE["nc"] = build_program()
    res = run_bass_kernel_spmd(_NC_CACHE["nc"], maps, core_ids=list(range(8)))
    out = np.zeros((4, 2 * T, D), np.float32)
    for core in range(8):
        b, h = core // 2, core % 2
        out[b, h * T:(h + 1) * T] = res.results[core]["out"]
    return out
```

```python
import numpy as np
import ml_dtypes
from contextlib import ExitStack
import concourse.bass as bass
import concourse.mybir as mybir
from concourse.bass_utils import run_bass_kernel_spmd

F32 = mybir.dt.float32
BF16 = mybir.dt.bfloat16
I32 = mybir.dt.int32
U32 = mybir.dt.uint32
AF = mybir.ActivationFunctionType
ALU = mybir.AluOpType
AX = mybir.AxisListType

D = 1024
T = 2048
NT = 16
LT = 4096
NE = 32
CAP = 512
NH = 4
CH = 256
NTOT = NE * CAP + NH * CH
EPS = 1e-6
NEG = -30000.0
FLAGS = {}


class _Op:
    __slots__ = ("eng", "fn", "deps", "signal", "sidx", "dma", "sem", "semval", "n")


class Prog:
    CE = ("pe", "act", "dve", "pool", "sp")
    EPOCH = 12000

    def __init__(self, nc, stack):
        self.nc = nc
        self.stack = stack
        self.ops = {e: [] for e in ("pe", "act", "dve", "pool", "sp")}
        self.state = {}
        self.subs = {}
        self.ndma = {"sp": 0, "act": 0, "pool": 0}
        self.dma_last = {}
        self.NDS = 8
        self.n = 0

    def sb(self, name, shape, dt):
        return self.stack.enter_context(self.nc.sbuf_tensor(name, list(shape), dt))

    def ps(self, name, shape, dt):
        return self.stack.enter_context(self.nc.psum_tensor(name, list(shape), dt))

    def _keys(self, k):
        if isinstance(k, tuple):
            name = k[0]
            self.subs.setdefault(name, set()).add(k)
            return [k, name]
        else:
            return [k] + list(self.subs.get(k, ()))

    def op(self, eng, fn, r=(), w=(), dma=False):
        o = _Op()
        o.eng, o.fn, o.deps, o.signal, o.dma = eng, fn, set(), False, dma
        o.sem = o.semval = o.sidx = None
        o.n = self.n
        self.n += 1
        for k in r:
            for kk in self._keys(k):
                st = self.state.get(kk)
                if st and st[0] is not None:
                    o.deps.add(st[0])
        for k in w:
            for kk in self._keys(k):
                st = self.state.get(kk)
                if st:
                    if st[0] is not None:
                        o.deps.add(st[0])
                    for x in st[1]:
                        o.deps.add(x)
        for k in r:
            st = self.state.setdefault(k, [None, []])
            st[1].append(o)
        for k in w:
            self.state[k] = [o, []]
            if not isinstance(k, tuple):
                for kk in self.subs.get(k, ()):
                    self.state[kk] = [o, []]
        o.deps.discard(o)
        if dma:
            j = self.ndma[eng] % self.NDS
            self.ndma[eng] += 1
            key = (eng, j)
            prev = self.dma_last.get(key)
            if prev is not None:
                o.deps.add(prev)
                o.semval = prev.semval + 16
            else:
                o.semval = 16
            o.sem = key
            self.dma_last[key] = o
        for x in o.deps:
            if not x.dma and not (x.eng == "pe" and eng == "pe" and not dma):
                x.signal = True
        self.ops[eng].append(o)
        return o

    def emit(self, final_ops):
        nc = self.nc
        st = self.stack
        nsig = {}
        for e in self.CE:
            c = 0
            for o in self.ops[e]:
                if o.signal and not o.dma:
                    o.sidx = c
                    c += 1
            nsig[e] = c
        esem = {}
        for e in self.CE:
            for ep in range(nsig[e] // self.EPOCH + 1):
                esem[(e, ep)] = st.enter_context(nc.semaphore("s_%s_%d" % (e, ep)))
        dsem = {}
        for q in ("sp", "act", "pool"):
            for j in range(min(self.NDS, self.ndma[q])):
                dsem[(q, j)] = st.enter_context(nc.semaphore("d_%s_%d" % (q, j)))
        fin = st.enter_context(nc.semaphore("fin"))
        semname = {id(v): k for k, v in list(esem.items()) + list(dsem.items())}
        block = st.enter_context(nc.Block())
        EP = self.EPOCH

        def run(ename, eng, extra=None):
            waited = {}
            for o in self.ops[ename]:
                need = {}
                for x in o.deps:
                    if x.dma:
                        s, v = dsem[x.sem], x.semval
                    else:
                        if x.eng == "pe" and ename == "pe" and not o.dma:
                            continue
                        s, v = esem[(x.eng, x.sidx // EP)], x.sidx % EP + 1
                    if waited.get(s, 0) >= v:
                        continue
                    if need.get(s, 0) < v:
                        need[s] = v
                for s, v in need.items():
                    eng.wait_ge(s, v)
                    waited[s] = v
                if FLAGS.get("trace"):
                    print(ename, o.n, "waits", [(semname[id(s)], v) for s, v in need.items()],
                          "sig", (o.sidx if o.signal and not o.dma else None), "dma", (o.sem, o.semval) if o.dma else None)
                ins = o.fn(eng)
                if o.dma:
                    ins.then_inc(dsem[o.sem], 16)
                elif o.signal:
                    ins.then_inc(esem[(ename, o.sidx // EP)], 1)
            if extra:
                extra(eng, waited)

        def fin_sp(eng, waited):
            for x in final_ops:
                s, v = dsem[x.sem], x.semval
                if waited.get(s, 0) < v:
                    eng.wait_ge(s, v)
                    waited[s] = v

        @block.sync
        def _(e):
            run("sp", e, fin_sp)

        @block.scalar
        def _(e):
            run("act", e)

        @block.vector
        def _(e):
            run("dve", e)

        @block.gpsimd
        def _(e):
            run("pool", e)

        @block.tensor
        def _(e):
            run("pe", e)


ARENA_F32 = 51600


class Arena:
    def __init__(self, big):
        self.big = big
        self.off = 0
        self.marks = []

    def push(self):
        self.marks.append(self.off)

    def pop(self):
        self.off = self.marks.pop()

    def get(self, dt, shape, parts=128):
        n = 1
        for s in shape:
            n *= s
        esz = 4 if dt in (F32, I32, U32) else 2
        words = (n * esz + 3) // 4
        words = (words + 7) // 8 * 8
        a = self.big[0:parts, self.off:self.off + words]
        self.off += words
        assert self.off <= ARENA_F32, ("arena overflow", self.off)
        if dt != F32:
            a = a.bitcast(dt)
        a = a[:, 0:n]
        if len(shape) == 2:
            a = a.rearrange("p (a b) -> p a b", a=shape[0])
        elif len(shape) == 3:
            a = a.rearrange("p (a b c) -> p a b c", a=shape[0], b=shape[1])
        return a


def build_program(stage=99, dbg=None):
    nc = bass.Bass("TRN2", target_bir_lowering=False)
    dbg = dbg or {}

    def din(name, shape, dt=F32):
        return nc.dram_tensor(name, list(shape), dt, kind="ExternalInput").ap()

    x_own = din("x_own", [T, D]); x_pre = din("x_pre", [T, D])
    pos = din("pos", [1, LT], I32)
    c_col = din("c_col", [128, 8]); hflag_d = din("hflag", [128, 1]); vbias_d = din("vbias", [128, 8 * 16])
    ada_w = din("ada_w", [D, 6 * D]); ada_b_col = din("ada_b_col", [128, 48]); ada_b_row = din("ada_b_row", [1, 4 * D])
    n1g_col = din("n1g_col", [128, 8]); n2g_row = din("n2g_row", [1, D])
    w_in = din("w_in", [D, 7 * D])
    convw_col = din("convw_col", [128, 32]); convb_col = din("convb_col", [128, 8])
    w_rg_a = din("w_rg_a", [8, 128, 128]); brga_col = din("brga_col", [128, 8])
    w_rg_x = din("w_rg_x", [8, 128, 128]); brgx_col = din("brgx_col", [128, 8])
    lam_col = din("lam_col", [128, 8]); qg_col = din("qg_col", [128, 1]); kg_col = din("kg_col", [128, 1])
    bgate_col = din("bgate_col", [128, 16])
    w_branch = din("w_branch", [2, D, D]); w_out = din("w_out", [D, D])
    w_router = din("w_router", [D, NE]); b_router_row = din("b_router_row", [1, NE])
    w_up = din("w_up", [NE, D, 2 * D]); bup_col = din("bup_col", [128, NE * 16])
    w_down = din("w_down", [NE, D, D]); b_down = din("b_down", [NE, D])
    ident_bf_d = din("ident_bf", [128, 128], BF16); ident_f_d = din("ident_f", [128, 128])
    triu_bf_d = din("triu_bf", [128, 128], BF16); tribias_d = din("tribias", [128, 256], BF16)
    rm_f_d = din("rm_f", [128, 32]); freq_d = din("freq_col", [32, 2]); iota32_d = din("iota32", [128, NE]); pcol_d = din("pcol", [128, 2]); k128_d = din("k128", [128, 8]); bq_d = din("bq", [NE * 128, 16])
    out_d = nc.dram_tensor("out", [T, D], F32, kind="ExternalOutput").ap()
    xg_d = nc.dram_tensor("xg_scr", [NTOT + 128, D], BF16, kind="Internal").ap()
    yall_d = nc.dram_tensor("yall_scr", [NTOT + 128, D], F32, kind="Internal").ap()
    x1_d = nc.dram_tensor("x1_scr", [T, D], F32, kind="Internal").ap()
    modb_d = nc.dram_tensor("modb_scr", [128, 4 * D], F32, kind="Internal").ap()
    dbg_out = {}
    for k, shp in dbg.items():
        dbg_out[k] = nc.dram_tensor("dbg_" + k, list(shp), F32, kind="ExternalOutput").ap()

    st = ExitStack()
    with st:
        P = Prog(nc, st)
        big = P.sb("arena", [128, ARENA_F32], F32)
        AR = Arena(big)
        dest_sb = P.sb("dest_sb", [128, 16, 4], I32)
        idxw_sb = P.sb("idxw_sb", [128, NH, 8], I32)
        idxb_sb = P.sb("idxb_sb", [128, NH, 2], I32)
        stage_sb = [P.sb("stage_sb%d" % i, [128, D], BF16) for i in range(2)]
        pb = [P.ps("pb%d" % i, [128, 512], F32) for i in range(6)]
        pbt = [P.ps("pbt%d" % i, [128, 1024], BF16) for i in range(2)]
        finals = []
        cnt = [0]

        def uid(s):
            cnt[0] += 1
            return "%s_%d" % (s, cnt[0])

        def dma(q, out, in_, r=(), w=()):
            return P.op(q, lambda e: e.dma_start(out=out, in_=in_), r=r, w=w, dma=True)

        def barrier():
            lasts = []
            for e in ("pe", "act", "dve", "pool", "sp"):
                if P.ops[e]:
                    lasts.append(P.ops[e][-1])
            lasts += list(P.dma_last.values())
            for e in ("pe", "act", "dve", "pool", "sp"):
                o = P.op(e, lambda en: en.nop())
                for x in lasts:
                    if x is not o:
                        o.deps.add(x)
                        if not x.dma:
                            x.signal = True

        def dump(name, ap, key):
            if name not in dbg_out:
                return
            finals.append(dma("sp", dbg_out[name], ap, r=[key]))

        ident_bf = AR.get(BF16, [128]); ident_f = AR.get(F32, [128])
        ones_bf = AR.get(BF16, [128]); triu_bf = AR.get(BF16, [128]); tribias = AR.get(BF16, [256])
        hflag = AR.get(F32, [1]); vbias = AR.get(F32, [8, 16])
        sh1c = AR.get(F32, [8]); gs1c = AR.get(F32, [8])
        sccol = AR.get(F32, [8])
        dma("sp", ident_bf, ident_bf_d, w=["ident_bf"]); dma("sp", ident_f, ident_f_d, w=["ident_f"])
        dma("sp", triu_bf, triu_bf_d, w=["triu_bf"]); dma("sp", tribias, tribias_d, w=["tribias"])
        dma("sp", hflag, hflag_d, w=["hflag"])
        dma("sp", vbias, vbias_d.rearrange("p (a b) -> p a b", a=8), w=["vbias"])
        P.op("dve", lambda e: e.memset(ones_bf, 1.0), w=["ones_bf"])
        zt = AR.get(BF16, [2, D])
        P.op("dve", lambda e: e.memset(zt, 0.0), w=["zt"])

        AR.push()
        ccol = AR.get(F32, [8])
        abcol = AR.get(F32, [48]); n1g = AR.get(F32, [8])
        wA = [AR.get(F32, [8, 512]), AR.get(F32, [8, 512])]
        dma("sp", ccol, c_col, w=["ccol"]); dma("sp", abcol, ada_b_col, w=["abcol"]); dma("sp", n1g, n1g_col, w=["n1g"])
        P.op("act", lambda e: e.activation(out=sccol, in_=ccol, func=AF.Silu), r=["ccol"], w=["sccol"])
        adw = ada_w.rearrange("(k p) n -> p k n", p=128)
        for g in range(4):
            wb = wA[g % 2]; wk = ("wA", g % 2)
            dma("sp" if g % 2 == 0 else "act", wb, adw[:, :, g * 512:(g + 1) * 512], w=[wk])
            for j in range(4):
                for k in range(8):
                    P.op("pe", lambda e, j=j, k=k, g=g, wb=wb: e.matmul(pb[0][:, g * 4 + j:g * 4 + j + 1],
                         lhsT=wb[:, k, j * 128:(j + 1) * 128], rhs=sccol[:, k:k + 1], start=(k == 0), stop=(k == 7)),
                         r=[wk, "sccol"], w=[("pb0", g * 4 + j)])
        P.op("dve", lambda e: e.tensor_tensor(out=sh1c, in0=pb[0][:, 0:8], in1=abcol[:, 0:8], op=ALU.add),
             r=["pb0", "abcol"], w=["sh1c"])
        P.op("dve", lambda e: e.scalar_tensor_tensor(out=gs1c, in0=pb[0][:, 8:16], scalar=1.0, in1=abcol[:, 8:16],
             op0=ALU.add, op1=ALU.add), r=["pb0", "abcol"], w=["gs1c"])
        P.op("dve", lambda e: e.tensor_tensor(out=gs1c, in0=gs1c, in1=n1g, op=ALU.mult), r=["gs1c", "n1g"], w=["gs1c"])
        dump("sh1c", sh1c, "sh1c"); dump("gs1c", gs1c, "gs1c")
        barrier()
        AR.pop()
        off_base = AR.off
        mixT = AR.get(BF16, [8, T])
        off_mix = AR.off
        hT = AR.get(BF16, [8, LT])
        yT = AR.get(BF16, [8, T])
        win3 = w_in.rearrange("(k p) n -> p k n", p=128)

        def phaseB():
            AR.push()
            xt = [AR.get(F32, [D]), AR.get(F32, [D])]
            junk = AR.get(BF16, [D])
            xnb = [AR.get(BF16, [D]), AR.get(BF16, [D])]
            ss = AR.get(F32, [32]); lnv = AR.get(F32, [32]); rstd = AR.get(F32, [32])
            def tile_steps(i):
                steps = []
                S = lambda *a_, **k_: steps.append((a_, k_))
                b = i % 2
                src = x_pre if i < 16 else x_own
                r0 = (i % 16) * 128
                S("sp", (lambda e, b=b, r0=r0, src=src: e.dma_start(out=xt[b], in_=src[r0:r0 + 128, :])), w=[("xt", b)], dma=True)
                S("act", lambda e, b=b, i=i: e.activation(out=junk, in_=xt[b], func=AF.Square, accum_out=ss[:, i:i + 1]),
                     r=[("xt", b)], w=["junk", ("ss", i)])
                S("act", lambda e, i=i: e.activation(out=lnv[:, i:i + 1], in_=ss[:, i:i + 1], func=AF.Ln, scale=1.0 / D, bias=EPS),
                     r=[("ss", i)], w=[("lnv", i)])
                S("act", lambda e, i=i: e.activation(out=rstd[:, i:i + 1], in_=lnv[:, i:i + 1], func=AF.Exp, scale=-0.5),
                     r=[("lnv", i)], w=[("rstd", i)])
                S("dve", lambda e, b=b, i=i: e.tensor_scalar(out=xnb[b], in0=xt[b], scalar1=rstd[:, i:i + 1], scalar2=None,
                     op0=ALU.mult), r=[("xt", b), ("rstd", i)], w=[("xnb", b)])
                pbT = pbt[b]
                pk = "pbt%d" % b
                for c in range(8):
                    S("pe", lambda e, b=b, c=c, pbT=pbT: e.transpose(pbT[:, c * 128:(c + 1) * 128],
                         xnb[b][:, c * 128:(c + 1) * 128], ident_bf), r=[("xnb", b), "ident_bf"], w=[(pk, c)])
                for c in range(8):
                    if FLAGS.get("B_noevac") or c >= FLAGS.get("B_nevac", 8):
                        continue
                    dst = hT[:, c, i * 128:(i + 1) * 128]
                    if FLAGS.get("B_dst2"):
                        dst = junk[:, c * 128:(c + 1) * 128]
                    if FLAGS.get("B_evaccopy"):
                        srcp = pb[3][:, 0:128] if FLAGS.get("B_src2") else pbT[:, c * 128:(c + 1) * 128]
                        S("dve", lambda e, c=c, dst=dst, srcp=srcp: e.tensor_copy(out=dst, in_=srcp),
                             r=[pk if FLAGS.get("B_waitall") else (pk, c)], w=[("hT", i)])
                        continue
                    if c % 2 == 0 and not FLAGS.get("B_dveonly"):
                        S("act", lambda e, c=c, dst=dst, pbT=pbT: e.activation(out=dst, in_=pbT[:, c * 128:(c + 1) * 128],
                             func=AF.Identity, scale=gs1c[:, c:c + 1], bias=sh1c[:, c:c + 1]),
                             r=[pk, "gs1c", "sh1c"], w=[("hT", i)])
                    else:
                        S("dve", lambda e, c=c, dst=dst, pbT=pbT: e.tensor_scalar(out=dst, in0=pbT[:, c * 128:(c + 1) * 128],
                             scalar1=gs1c[:, c:c + 1], scalar2=sh1c[:, c:c + 1], op0=ALU.mult, op1=ALU.add),
                             r=[pk, "gs1c", "sh1c"], w=[("hT", i)])
                return steps

            modb = AR.get(F32, [4 * D]); scb = AR.get(F32, [8, 128]); n2gb = AR.get(F32, [D])
            wA = [AR.get(F32, [8, 256]), AR.get(F32, [8, 256])]
            adw = ada_w.rearrange("(k p) n -> p k n", p=128)
            dma("pool", modb, ada_b_row.partition_broadcast(128), w=["modb"])
            dma("pool", n2gb, n2g_row.partition_broadcast(128), w=["n2gb"])
            for k in range(8):
                P.op("dve", lambda e, k=k: e.tensor_copy(out=scb[:, k, :], in_=sccol[:, k:k + 1].to_broadcast([128, 128])),
                     r=["sccol"], w=[("scb", k)])

            def wload(gg):
                return [(("pool", (lambda e, gg=gg: e.dma_start(out=wA[gg % 2], in_=adw[:, :, 2 * D + gg * 256:2 * D + (gg + 1) * 256]))),
                         dict(w=[("wA", gg % 2)], dma=True))]

            def wmm(gg):
                st_ = []
                wb = wA[gg % 2]; wk = ("wA", gg % 2)
                pp = pb[4 + gg % 2]; pk = "pb%d" % (4 + gg % 2)
                for k in range(8):
                    st_.append((("pe", (lambda e, k=k, wb=wb, pp=pp: e.matmul(pp[:, 0:256], lhsT=scb[:, k, :], rhs=wb[:, k, :],
                               start=(k == 0), stop=(k == 7)))), dict(r=[wk, "scb"], w=[pk])))
                st_.append((("dve", (lambda e, gg=gg, pp=pp: e.tensor_tensor(out=modb[:, gg * 256:(gg + 1) * 256],
                           in0=pp[:, 0:256], in1=modb[:, gg * 256:(gg + 1) * 256], op=ALU.add))), dict(r=[pk, "modb"], w=["modb"])))
                return st_

            import itertools
            ch0 = []; ch1 = [None] * 11; ch2 = wload(0) + wload(1) + [None] * 6
            for i2 in range(0, 32, 2):
                ch0 += tile_steps(i2); ch1 += tile_steps(i2 + 1)
                gg = i2 // 2
                grp = wmm(gg) + (wload(gg + 2) if gg + 2 < 16 else [])
                ch2 += grp + [None] * max(0, 22 - len(grp))
            for xs in itertools.zip_longest(ch0, ch1, ch2):
                for x in xs:
                    if x is not None:
                        P.op(*x[0], **x[1])
            gs2b_ = modb[:, 2 * D:3 * D]
            P.op("dve", lambda e: e.scalar_tensor_tensor(out=gs2b_, in0=gs2b_, scalar=1.0, in1=n2gb, op0=ALU.add, op1=ALU.mult),
                 r=["modb", "n2gb"], w=["modb"])
            dma("sp", modb_d, modb, r=["modb"], w=["modb_scr"])
            barrier()
            AR.pop()

        def phaseC():
            AR.push()
            cw = AR.get(F32, [8, 4]); cb = AR.get(F32, [8]); nba = AR.get(F32, [8]); nbx = AR.get(F32, [8])
            lam = AR.get(F32, [8]); s1 = AR.get(F32, [8]); s2 = AR.get(F32, [8])
            wga = AR.get(BF16, [8, 128]); wgx = AR.get(BF16, [8, 128])
            wxr = AR.get(BF16, [8, 512]); wgr = AR.get(BF16, [8, 512])
            BUF = []

            def mkset():
                d_ = dict(
                    xb=AR.get(F32, [515]), hist=AR.get(F32, [3]), xc=AR.get(F32, [512]), xcb=AR.get(BF16, [512]),
                    er=AR.get(F32, [512]), eg=AR.get(F32, [512]), aa=AR.get(F32, [512]), a2=AR.get(F32, [512]),
                    hb=AR.get(F32, [512]), carry=AR.get(F32, [1]), fx=AR.get(F32, [1]))
                d_["uu"] = d_["er"]; d_["gz"] = d_["er"]
                return d_
            save_ = AR.off
            AR.off = off_base
            BUF.append(mkset()); BUF.append(mkset())
            assert AR.off <= off_mix
            AR.off = save_
            BUF.append(mkset()); BUF.append(mkset())
            PB = [(pb[0], "pb0"), (pb[1], "pb1"), (pb[2], "pb2"), (pb[3], "pb3"), (pb[4], "pb4"), (pb[5], "pb5"),
                  (pbt[0][:, :].bitcast(F32), "pbt0"), (pbt[1][:, :].bitcast(F32), "pbt1")]
            dma("sp", cw, convw_col.rearrange("p (a b) -> p a b", a=8), w=["cw"]); dma("sp", cb, convb_col, w=["cb"])
            dma("sp", nba, brga_col, w=["nba"]); dma("sp", nbx, brgx_col, w=["nbx"]); dma("sp", lam, lam_col, w=["lam"])
            dma("pool", wga, w_rg_a.rearrange("n w v -> w n v"), w=["wga"])
            dma("pool", wgx, w_rg_x.rearrange("n w v -> w n v"), w=["wgx"])
            P.op("dve", lambda e: e.tensor_scalar(out=nba, in0=nba, scalar1=-1.0, scalar2=None, op0=ALU.mult), r=["nba"], w=["nba"])
            P.op("dve", lambda e: e.tensor_scalar(out=nbx, in0=nbx, scalar1=-1.0, scalar2=None, op0=ALU.mult), r=["nbx"], w=["nbx"])
            P.op("act", lambda e: e.activation(out=s1, in_=lam, func=AF.Exp, scale=-1.0), r=["lam"], w=["s1"])
            P.op("act", lambda e: e.activation(out=s1, in_=s1, func=AF.Ln, bias=1.0), r=["s1"], w=["s1"])
            P.op("dve", lambda e: e.tensor_scalar(out=s2, in0=s1, scalar1=-16.0, scalar2=None, op0=ALU.mult), r=["s1"], w=["s2"])
            P.op("dve", lambda e: e.tensor_scalar(out=s1, in0=s1, scalar1=-8.0, scalar2=None, op0=ALU.mult), r=["s1", "s2"], w=["s1"])
            GC = 1.5957691216057308

            def piece(c, j, par):
                B = BUF[par]
                steps = []
                S = lambda *a_, **k_: steps.append((a_, k_))
                cc = c % 4
                K = lambda n: (n, par)
                xc, xcb, er, eg, aa, a2, uu, gz, carry, fx = (B[n] for n in ("xc", "xcb", "er", "eg", "aa", "a2", "uu", "gz", "carry", "fx"))
                mm, ig = a2, eg
                xps, xk = PB[2 * par]
                for k in range(8):
                    S("pe", lambda e, k=k, j=j, cc=cc, xps=xps: e.matmul(xps[:, :],
                         lhsT=wxr[:, k, cc * 128:(cc + 1) * 128], rhs=hT[:, k, j * 512:(j + 1) * 512],
                         start=(k == 0), stop=(k == 7)), r=["wxr", "hT"], w=[xk])
                xb = B["xb"]; xbk = ("xb", par); hist = B["hist"]; hk_ = ("hist", par)
                if j == 0:
                    S("dve", lambda e, xb=xb: e.memset(xb[:, 0:3], 0.0), w=[xbk])
                elif j == 4:
                    S("dve", lambda e, xb=xb, hist=hist: e.tensor_scalar(out=xb[:, 0:3], in0=hist,
                         scalar1=hflag[:, 0:1], scalar2=None, op0=ALU.mult), r=[hk_, "hflag"], w=[xbk])
                else:
                    S("dve", lambda e, xb=xb, hist=hist: e.tensor_copy(out=xb[:, 0:3], in_=hist), r=[hk_], w=[xbk])
                S("dve", lambda e, xb=xb, xps=xps: e.tensor_copy(out=xb[:, 3:515], in_=xps[:, :]),
                     r=[xk, xbk], w=[xbk])
                S("dve", lambda e, xb=xb, c=c: e.tensor_scalar(out=xc, in0=xb[:, 3:515], scalar1=cw[:, c, 0:1],
                     scalar2=cb[:, c:c + 1], op0=ALU.mult, op1=ALU.add), r=[xbk, "cw", "cb"], w=[K("xc")])
                for i in (1, 2, 3):
                    S("dve", lambda e, xb=xb, c=c, i=i: e.scalar_tensor_tensor(out=xc, in0=xb[:, 3 - i:515 - i],
                         scalar=cw[:, c, i:i + 1], in1=xc, op0=ALU.mult, op1=ALU.add), r=[xbk, "cw", K("xc")], w=[K("xc")])
                S("dve", lambda e, xb=xb, hist=hist: e.tensor_copy(out=hist, in_=xb[:, 512:515]), r=[xbk], w=[hk_])
                S("pool", lambda e: e.tensor_copy(out=xcb, in_=xc), r=[K("xc")], w=[K("xcb")])
                rps, rk = PB[2 * par + 1]; gps, gk = PB[2 * par]
                S("pe", lambda e, c=c, rps=rps: e.matmul(rps[:, :], lhsT=wga[:, c, :], rhs=xcb, start=True, stop=True),
                     r=["wga", K("xcb")], w=[rk])
                S("pe", lambda e, c=c, gps=gps: e.matmul(gps[:, :], lhsT=wgx[:, c, :], rhs=xcb, start=True, stop=True),
                     r=["wgx", K("xcb")], w=[gk])
                S("act", lambda e, c=c, rps=rps: e.activation(out=er, in_=rps[:, :], func=AF.Exp, scale=-1.0,
                     bias=nba[:, c:c + 1]), r=[rk, "nba"], w=[K("er")])
                S("act", lambda e: e.activation(out=er, in_=er, func=AF.Ln, bias=1.0), r=[K("er")], w=[K("er")])
                S("act", lambda e: e.activation(out=er, in_=er, func=AF.Exp, scale=-1.0), r=[K("er")], w=[K("er")])
                S("act", lambda e, c=c, gps=gps: e.activation(out=eg, in_=gps[:, :], func=AF.Exp, scale=-1.0,
                     bias=nbx[:, c:c + 1]), r=[gk, "nbx"], w=[K("eg")])
                S("act", lambda e: e.activation(out=eg, in_=eg, func=AF.Ln, bias=1.0), r=[K("eg")], w=[K("eg")])
                S("act", lambda e: e.activation(out=eg, in_=eg, func=AF.Exp, scale=-1.0), r=[K("eg")], w=[K("eg")])
                S("act", lambda e, c=c: e.activation(out=aa, in_=er, func=AF.Exp, scale=s1[:, c:c + 1]), r=[K("er"), "s1"], w=[K("aa")])
                S("act", lambda e, c=c: e.activation(out=a2, in_=er, func=AF.Exp, scale=s2[:, c:c + 1]), r=[K("er"), "s2"], w=[K("a2")])
                S("pool", lambda e: e.tensor_scalar(out=mm, in0=a2, scalar1=-1.0, scalar2=1.0, op0=ALU.mult, op1=ALU.add),
                     r=[K("a2")], w=[K("a2")])
                S("act", lambda e: e.activation(out=mm, in_=mm, func=AF.Ln), r=[K("a2")], w=[K("a2")])
                S("act", lambda e: e.activation(out=mm, in_=mm, func=AF.Exp, scale=0.5), r=[K("a2")], w=[K("a2")])
                if j == 0:
                    S("dve", lambda e: e.memset(mm[:, 0:1], 1.0), r=[K("a2")], w=[K("a2")])
                elif j == 4:
                    S("dve", lambda e: e.tensor_scalar(out=fx, in0=mm[:, 0:1], scalar1=-1.0, scalar2=hflag[:, 0:1],
                         op0=ALU.add, op1=ALU.mult), r=[K("a2"), "hflag"], w=[K("fx")])
                    S("dve", lambda e: e.tensor_scalar(out=mm[:, 0:1], in0=fx, scalar1=1.0, scalar2=None, op0=ALU.add),
                         r=[K("fx"), K("a2")], w=[K("a2")])
                S("pool", lambda e: e.tensor_tensor(out=uu, in0=mm, in1=ig, op=ALU.mult), r=[K("a2"), K("eg")], w=[K("er")])
                S("pool", lambda e: e.tensor_tensor(out=uu, in0=uu, in1=xc, op=ALU.mult), r=[K("er"), K("xc")], w=[K("er")])
                hdst = B["hb"]; hk = ("hb", par)
                if j == 0:
                    S("dve", lambda e, hdst=hdst: e.tensor_tensor_scan(out=hdst, data0=aa, data1=uu, initial=0.0,
                         op0=ALU.mult, op1=ALU.add), r=[K("aa"), K("er")], w=[hk])
                else:
                    S("dve", lambda e, hdst=hdst: e.tensor_tensor_scan(out=hdst, data0=aa, data1=uu, initial=carry,
                         op0=ALU.mult, op1=ALU.add), r=[K("aa"), K("er"), K("carry")], w=[hk])
                if j == 3:
                    S("dve", lambda e, hdst=hdst: e.tensor_scalar(out=carry, in0=hdst[:, 511:512], scalar1=hflag[:, 0:1],
                         scalar2=None, op0=ALU.mult), r=[hk, "hflag"], w=[K("carry")])
                elif j < 7:
                    S("dve", lambda e, hdst=hdst: e.tensor_copy(out=carry, in_=hdst[:, 511:512]), r=[hk], w=[K("carry")])
                if j >= 4:
                    jj = j - 4
                    gps2, gk2 = PB[2 * par]
                    for k in range(8):
                        S("pe", lambda e, k=k, jj=jj, cc=cc, gps2=gps2: e.matmul(gps2[:, :],
                             lhsT=wgr[:, k, cc * 128:(cc + 1) * 128], rhs=hT[:, k, T + jj * 512:T + (jj + 1) * 512],
                             start=(k == 0), stop=(k == 7)), r=["wgr", "hT"], w=[gk2])
                    S("act", lambda e, gps2=gps2: e.activation(out=gz, in_=gps2[:, :], func=AF.Square), r=[gk2], w=[K("er")])
                    S("pool", lambda e: e.tensor_scalar(out=gz, in0=gz, scalar1=0.044715, scalar2=1.0, op0=ALU.mult, op1=ALU.add),
                         r=[K("er")], w=[K("er")])
                    S("dve", lambda e, gps2=gps2: e.tensor_tensor(out=gz, in0=gz, in1=gps2[:, :], op=ALU.mult), r=[K("er"), gk2], w=[K("er")])
                    S("act", lambda e: e.activation(out=gz, in_=gz, func=AF.Exp, scale=-GC), r=[K("er")], w=[K("er")])
                    S("act", lambda e: e.activation(out=gz, in_=gz, func=AF.Ln, bias=1.0), r=[K("er")], w=[K("er")])
                    S("act", lambda e: e.activation(out=gz, in_=gz, func=AF.Exp, scale=-1.0), r=[K("er")], w=[K("er")])
                    S("dve", lambda e, gps2=gps2: e.tensor_tensor(out=gz, in0=gz, in1=gps2[:, :], op=ALU.mult), r=[K("er"), gk2], w=[K("er")])
                    S("dve", lambda e, jj=jj, c=c, hdst=hdst: e.tensor_tensor(out=yT[:, c, jj * 512:(jj + 1) * 512],
                         in0=hdst, in1=gz, op=ALU.mult), r=[hk, K("er")], w=[("yT", c)])
                return steps

            import itertools
            xg2 = xg_d[0:NTOT, :].rearrange("(e p r) d -> e p r d", p=128, r=2)
            for e_ in range(NTOT // 256):
                dma("sp", xg2[e_], zt, r=["zt"], w=["Xg"])
            for cg in range(2):
                dma("pool", wxr, win3[:, :, cg * 512:(cg + 1) * 512], w=["wxr"])
                dma("pool", wgr, win3[:, :, D + cg * 512:D + (cg + 1) * 512], w=["wgr"])
                c0 = cg * 4
                sts = []
                for q in range(4):
                    lst = [None] * (q * 12)
                    for j in range(8):
                        lst += piece(c0 + q, j, q)
                    sts.append(lst)
                for xs in itertools.zip_longest(*sts):
                    for x in xs:
                        if x is not None:
                            P.op(*x[0], **x[1])
            barrier()
            AR.pop()

        def merge(g, first):
            AR.push()
            wbr = [AR.get(BF16, [8, 512]), AR.get(BF16, [8, 512])]
            wgl = [AR.get(BF16, [8, 512]), AR.get(BF16, [8, 512])]
            bgc = AR.get(F32, [16]); sg = AR.get(F32, [512]); tmp = AR.get(F32, [512])
            dma("sp", bgc, bgate_col, w=["bgc"])
            wb3 = w_branch[g].rearrange("(k p) n -> p k n", p=128)
            for grp in range(2):
                dma("pool", wbr[grp], wb3[:, :, grp * 512:(grp + 1) * 512], w=[("wbr", grp)])
                c0 = 5 * D + g * D + grp * 512
                dma("pool", wgl[grp], win3[:, :, c0:c0 + 512], w=[("wgl", grp)])
            for m in range(8):
                grp, mm = m // 4, m % 4
                for pc in range(4):
                    zps = pb[pc % 2]; zk = "pb%d" % (pc % 2); gps = pb[2 + pc % 2]; gk = "pb%d" % (2 + pc % 2)
                    for k in range(8):
                        P.op("pe", lambda e, k=k, pc=pc, grp=grp, mm=mm, zps=zps: e.matmul(zps[:, :],
                             lhsT=wbr[grp][:, k, mm * 128:(mm + 1) * 128], rhs=yT[:, k, pc * 512:(pc + 1) * 512],
                             start=(k == 0), stop=(k == 7)), r=[("wbr", grp), "yT"], w=[zk])
                    for k in range(8):
                        P.op("pe", lambda e, k=k, pc=pc, grp=grp, mm=mm, gps=gps: e.matmul(gps[:, :],
                             lhsT=wgl[grp][:, k, mm * 128:(mm + 1) * 128], rhs=hT[:, k, T + pc * 512:T + (pc + 1) * 512],
                             start=(k == 0), stop=(k == 7)), r=[("wgl", grp), "hT"], w=[gk])
                    P.op("act", lambda e, gps=gps, m=m: e.activation(out=sg, in_=gps[:, :], func=AF.Sigmoid,
                         bias=bgc[:, g * 8 + m:g * 8 + m + 1]), r=[gk, "bgc"], w=["sg"])
                    dst = mixT[:, m, pc * 512:(pc + 1) * 512]
                    if first:
                        P.op("dve", lambda e, zps=zps, dst=dst: e.tensor_tensor(out=dst, in0=zps[:, :], in1=sg, op=ALU.mult),
                             r=[zk, "sg"], w=[("mixT", m)])
                    else:
                        P.op("dve", lambda e, zps=zps: e.tensor_tensor(out=tmp, in0=zps[:, :], in1=sg, op=ALU.mult),
                             r=[zk, "sg"], w=["tmp"])
                        P.op("dve", lambda e, dst=dst: e.tensor_tensor(out=dst, in0=dst, in1=tmp, op=ALU.add),
                             r=["tmp", ("mixT", m)], w=[("mixT", m)])
            barrier()
            AR.pop()

        def phaseD():
            merge(0, True)

        def phaseF():
            merge(1, False)

        def phaseE():
            AR.push()
            SCALE = 128.0 ** -0.5
            C32 = AR.get(BF16, [LT], parts=32); S32 = AR.get(BF16, [LT], parts=32)
            qgc = AR.get(F32, [1]); kgc = AR.get(F32, [1]); rmf = AR.get(F32, [32]); fq = AR.get(F32, [2], parts=32)
            dma("sp", qgc, qg_col, w=["qgc"]); dma("sp", kgc, kg_col, w=["kgc"])
            dma("sp", rmf, rm_f_d, w=["rmf"]); dma("sp", fq, freq_d, w=["fq"])
            AR.push()
            posi = AR.get(I32, [1024], parts=32); ang = AR.get(F32, [1024], parts=32); tq = AR.get(F32, [1024], parts=32)
            ki = AR.get(I32, [1024], parts=32); kf = AR.get(F32, [1024], parts=32); rr_ = AR.get(F32, [1024], parts=32)
            ones32 = AR.get(F32, [1], parts=32)
            P.op("dve", lambda e: e.memset(ones32, 1.0), w=["ones32"])
            TWO_PI = 6.283185307179586
            C1 = 6.28125
            C2 = TWO_PI - C1
            for pc in range(4):
                sl = slice(pc * 1024, (pc + 1) * 1024)
                dma("sp", posi, pos[:, sl].partition_broadcast(32), w=["posi"])
                P.op("dve", lambda e: e.tensor_copy(out=ang, in_=posi), r=["posi"], w=["ang"])
                P.op("dve", lambda e: e.tensor_scalar(out=ang, in0=ang, scalar1=fq[:, 0:1], scalar2=None, op0=ALU.mult),
                     r=["ang", "fq"], w=["ang"])
                for which in (0, 1):
                    off = 0.0 if which == 0 else np.pi / 2
                    P.op("dve", lambda e, off=off: e.tensor_scalar(out=tq, in0=ang, scalar1=float(off), scalar2=1.0 / TWO_PI,
                         op0=ALU.add, op1=ALU.mult), r=["ang"], w=["tq"])
                    P.op("dve", lambda e: e.tensor_copy(out=ki, in_=tq), r=["tq"], w=["ki"])
                    P.op("dve", lambda e: e.tensor_copy(out=kf, in_=ki), r=["ki"], w=["kf"])
                    P.op("dve", lambda e, off=off: e.scalar_tensor_tensor(out=rr_, in0=kf, scalar=-C1, in1=ang,
                         op0=ALU.mult, op1=ALU.add), r=["kf", "ang"], w=["rr"])
                    P.op("dve", lambda e, off=off: e.scalar_tensor_tensor(out=rr_, in0=kf, scalar=-C2, in1=rr_,
                         op0=ALU.mult, op1=ALU.add), r=["kf", "rr"], w=["rr"])
                    P.op("dve", lambda e, off=off: e.tensor_scalar(out=rr_, in0=rr_, scalar1=float(off), scalar2=3.14159,
                         op0=ALU.add, op1=ALU.min), r=["rr"], w=["rr"])
                    P.op("dve", lambda e: e.tensor_scalar(out=rr_, in0=rr_, scalar1=-3.14159, scalar2=None, op0=ALU.max),
                         r=["rr"], w=["rr"])
                    if which == 0:
                        P.op("act", lambda e, sl=sl: e.activation(out=S32[:, sl], in_=rr_, func=AF.Sin, scale=fq[:, 1:2]),
                             r=["rr", "fq"], w=[("S32", pc)])
                    else:
                        P.op("act", lambda e, sl=sl: e.activation(out=C32[:, sl], in_=rr_, func=AF.Sin), r=["rr"], w=[("C32", pc)])
            barrier()
            AR.pop()
            kT = AR.get(BF16, [LT]); vb_ = AR.get(BF16, [32, 128]); qT = AR.get(BF16, [T])
            mbT = AR.get(BF16, [T], parts=16); mball = AR.get(BF16, [16, 16])
            wq = AR.get(BF16, [8, 128]); wk_ = AR.get(BF16, [8, 128]); wv = AR.get(BF16, [8, 128])
            kraw2 = [AR.get(F32, [512]), AR.get(F32, [512])]; sq2 = [AR.get(BF16, [512]), AR.get(BF16, [512])]
            lnr = AR.get(F32, [512]); rstd = AR.get(F32, [512])
            t1 = AR.get(F32, [512], parts=32); t2 = AR.get(F32, [512], parts=32)
            kmT = AR.get(F32, [16])
            gb = AR.get(F32, [16]); mx = AR.get(F32, [8]); sel = AR.get(F32, [16])
            pT = [AR.get(BF16, [512]), AR.get(BF16, [512]), AR.get(BF16, [512])]
            rl = AR.get(F32, [512])

            pbtF = [pbt[0][:, :].bitcast(F32), pbt[1][:, :].bitcast(F32)]

            def piece_info(hd, j):
                if j < 8:
                    return wk_, kgc, "kgc", j * 512, kT[:, j * 512:(j + 1) * 512]
                jj = j - 8
                return wq, qgc, "qgc", T + jj * 512, qT[:, jj * 512:(jj + 1) * 512]

            def s1_proj(hd, j):
                wt, gcol, gkey, tok0, dstT = piece_info(hd, j)
                ps = pb[j % 2]; pk = "pb%d" % (j % 2)
                for k in range(8):
                    P.op("pe", lambda e, k=k, ps=ps, wt=wt, tok0=tok0: e.matmul(ps[:, :], lhsT=wt[:, k, :], rhs=hT[:, k, tok0:tok0 + 512],
                         start=(k == 0), stop=(k == 7)), r=["wqkv", "hT"], w=[pk])

            def s1_evac(hd, j):
                ps = pb[j % 2]; pk = "pb%d" % (j % 2)
                kr = kraw2[j % 2]; krk = ("kraw", j % 2); sq_ = sq2[j % 2]; sqk = ("sq", j % 2)
                P.op("act", lambda e, ps=ps, kr=kr: e.activation(out=kr, in_=ps[:, :], func=AF.Copy), r=[pk], w=[krk])
                P.op("act", lambda e, ps=ps, sq_=sq_: e.activation(out=sq_, in_=ps[:, :], func=AF.Square), r=[pk], w=[sqk])

            def s2a(hd, j):
                wt, gcol, gkey, tok0, dstT = piece_info(hd, j)
                kr = kraw2[j % 2]; krk = ("kraw", j % 2); sq_ = sq2[j % 2]; sqk = ("sq", j % 2)
                sp_ = pb[2 + j % 2]; sk = "pb%d" % (2 + j % 2)
                P.op("pe", lambda e, sp_=sp_, sq_=sq_: e.matmul(sp_[:, :], lhsT=ones_bf, rhs=sq_, start=True, stop=True),
                     r=["ones_bf", sqk], w=[sk])
                P.op("act", lambda e, sp_=sp_: e.activation(out=lnr, in_=sp_[:, :], func=AF.Ln, scale=1.0 / 128, bias=EPS),
                     r=[sk], w=["lnr"])
                P.op("act", lambda e: e.activation(out=rstd, in_=lnr, func=AF.Exp, scale=-0.5), r=["lnr"], w=["rstd"])
                P.op("dve", lambda e, kr=kr, gcol=gcol: e.scalar_tensor_tensor(out=kr, in0=kr, scalar=gcol[:, 0:1], in1=rstd,
                     op0=ALU.mult, op1=ALU.mult), r=[krk, gkey, "rstd"], w=[krk])

            def s2b(hd, j):
                wt, gcol, gkey, tok0, dstT = piece_info(hd, j)
                kr = kraw2[j % 2]; krk = ("kraw", j % 2)
                rp = pb[4 + j % 2]; rk = "pb%d" % (4 + j % 2)
                P.op("pe", lambda e, rp=rp, kr=kr: e.matmul(rp[0:32, :], lhsT=rmf, rhs=kr, start=True, stop=True),
                     r=["rmf", krk], w=[rk])
                P.op("dve", lambda e, kr=kr, tok0=tok0: e.tensor_tensor(out=t1, in0=kr[0:32, :], in1=C32[:, tok0:tok0 + 512],
                     op=ALU.mult), r=[krk, "C32"], w=["t1"])
                P.op("dve", lambda e, rp=rp, tok0=tok0: e.tensor_tensor(out=t2, in0=rp[0:32, :], in1=S32[:, tok0:tok0 + 512], op=ALU.mult),
                     r=[rk, "S32"], w=["t2"])
                P.op("dve", lambda e, kr=kr: e.tensor_tensor(out=kr[0:32, :], in0=t1, in1=t2, op=ALU.add), r=["t1", "t2", krk], w=[krk])
                P.op("act", lambda e, kr=kr, dstT=dstT: e.activation(out=dstT, in_=kr, func=AF.Copy), r=[krk], w=["qkT"])
                if j < 8:
                    P.op("dve", lambda e, j=j, kr=kr: e.tensor_reduce(out=kmT[:, 2 * j:2 * j + 2],
                         in_=kr.rearrange("p (b t) -> p b t", b=2), axis=AX.X, op=ALU.add), r=[krk], w=["kmT"])
                else:
                    jj = j - 8
                    gp = pb[4 + j % 2]; gk = rk
                    for qt in range(4):
                        P.op("pe", lambda e, qt=qt, gp=gp, kr=kr: e.matmul(gp[:, 64 + qt * 16:64 + (qt + 1) * 16],
                             lhsT=kr[:, qt * 128:(qt + 1) * 128], rhs=kmT, start=True, stop=True),
                             r=[krk, "kmT"], w=[gk])
                    for qt in range(4):
                        i = jj * 4 + qt
                        P.op("dve", lambda e, qt=qt, i=i, gp=gp: e.tensor_tensor(out=gb, in0=gp[:, 64 + qt * 16:64 + (qt + 1) * 16],
                             in1=vbias[:, i // 2, :], op=ALU.add), r=[gk, "vbias"], w=["gb"])
                        P.op("dve", lambda e: e.max(out=mx, in_=gb), r=["gb"], w=["mx"])
                        P.op("dve", lambda e: e.tensor_scalar(out=sel, in0=gb, scalar1=mx[:, 2:3], scalar2=-1.0,
                             op0=ALU.is_ge, op1=ALU.add), r=["gb", "mx"], w=["sel"])
                        P.op("dve", lambda e, i=i: e.scalar_tensor_tensor(out=mball[:, i, :], in0=sel, scalar=-NEG,
                             in1=vbias[:, i // 2, :], op0=ALU.mult, op1=ALU.min), r=["sel", "vbias"], w=["mball"])

            def v_group(hd, tg):
                vps = pbtF[tg % 2]; vk = "pbt%d" % (tg % 2)
                for tt in range(4):
                    tile_i = tg * 4 + tt
                    for k in range(8):
                        P.op("pe", lambda e, k=k, tt=tt, tile_i=tile_i, vps=vps: e.matmul(vps[:, tt * 128:(tt + 1) * 128],
                             lhsT=hT[:, k, tile_i * 128:(tile_i + 1) * 128], rhs=wv[:, k, :], start=(k == 0), stop=(k == 7)),
                             r=["wqkv", "hT"], w=[vk])
                P.op("act", lambda e, tg=tg, vps=vps: e.activation(out=vb_[:, tg * 4:(tg + 1) * 4, :],
                     in_=vps.rearrange("p (a b) -> p a b", a=4), func=AF.Copy), r=[vk], w=["vb"])

            for hd in range(8):
                dma("pool", wq, win3[:, :, 2 * D + hd * 128:2 * D + (hd + 1) * 128], w=["wqkv"])
                dma("pool", wk_, win3[:, :, 3 * D + hd * 128:3 * D + (hd + 1) * 128], w=["wqkv"])
                dma("pool", wv, win3[:, :, 4 * D + hd * 128:4 * D + (hd + 1) * 128], w=["wqkv"])
                NP_ = 12
                s1_proj(hd, 0)
                s1_evac(hd, 0)
                for j in range(NP_):
                    if j + 1 < NP_:
                        s1_proj(hd, j + 1)
                    if j < 8:
                        v_group(hd, j)
                    if j >= 1:
                        s2b(hd, j - 1)
                    if j + 1 < NP_:
                        s1_evac(hd, j + 1)
                    s2a(hd, j)
                s2b(hd, NP_ - 1)
                for half in range(2):
                    tp = pbt[half]; tk = "pbt%d" % half
                    for ii in range(8):
                        i = half * 8 + ii
                        P.op("pe", lambda e, i=i, ii=ii, tp=tp: e.transpose(tp[0:16, ii * 128:(ii + 1) * 128], mball[:, i, :], ident_bf),
                             r=["mball", "ident_bf"], w=[tk])
                    P.op("dve", lambda e, half=half, tp=tp: e.tensor_copy(out=mbT[:, half * 1024:(half + 1) * 1024], in_=tp[0:16, :]),
                         r=[tk], w=["mbT"])
                for g4 in range(4):
                    qbA = 8 + 2 * g4
                    q0 = g4 * 512
                    ops_ = pb[2 + g4 % 2]; ok = "pb%d" % (2 + g4 % 2)
                    lps = pb[4 + g4 % 2]; lk = "pb%d" % (4 + g4 % 2)
                    chunks = [("A0", 2 * qbA, 0, 512), ("A1", 2 * qbA + 1, 128, 384),
                              ("B0", 2 * qbA + 2, 256, 256), ("B1", 2 * qbA + 3, 384, 128)]
                    chunks += [("P", c, 0, 512) for c in range(2 * qbA)]

                    def scores_pe(ci):
                        kind, kc, co, nq = chunks[ci]
                        sps = pb[ci % 2]; sk = "pb%d" % (ci % 2)
                        qs = q0 + co
                        P.op("pe", lambda e, kc=kc, qs=qs, nq=nq, sps=sps: e.matmul(sps[:, 0:nq], lhsT=kT[:, kc * 128:(kc + 1) * 128],
                             rhs=qT[:, qs:qs + nq], start=True, stop=False), r=["qkT"], w=[sk])
                        if kind == "P":
                            n = kc // 2
                            P.op("pe", lambda e, n=n, qs=qs, nq=nq, sps=sps: e.matmul(sps[:, 0:nq],
                                 lhsT=ident_bf[0:16, n:n + 1].to_broadcast([16, 128]), rhs=mbT[:, qs:qs + nq],
                                 start=False, stop=True), r=["mbT", "ident_bf"], w=[sk])
                        elif kind in ("A0", "A1"):
                            ntri = 256 if kind == "A0" else 128
                            P.op("pe", lambda e, ntri=ntri, sps=sps: e.matmul(sps[:, 0:ntri], lhsT=ident_bf, rhs=tribias[:, 0:ntri],
                                 start=False, stop=False), r=["tribias", "ident_bf"], w=[sk])
                            P.op("pe", lambda e, ntri=ntri, nq=nq, sps=sps, qbA=qbA, q0=q0: e.matmul(sps[:, ntri:nq],
                                 lhsT=ident_bf[0:16, qbA:qbA + 1].to_broadcast([16, 128]), rhs=mbT[:, q0 + 256:q0 + 512],
                                 start=False, stop=True), r=["mbT", "ident_bf"], w=[sk])
                        else:
                            P.op("pe", lambda e, nq=nq, sps=sps: e.matmul(sps[:, 0:nq], lhsT=ident_bf, rhs=tribias[:, 0:nq],
                                 start=False, stop=True), r=["tribias", "ident_bf"], w=[sk])

                    nch = len(chunks)
                    scores_pe(0)
                    for ci in range(nch):
                        kind, kc, co, nq = chunks[ci]
                        sps = pb[ci % 2]; sk = "pb%d" % (ci % 2)
                        ptile = pT[ci % 3]; ptk = ("pT", ci % 3)
                        P.op("act", lambda e, nq=nq, sps=sps, ptile=ptile: e.activation(out=ptile[:, 0:nq], in_=sps[:, 0:nq],
                             func=AF.Exp, scale=SCALE), r=[sk], w=[ptk])
                        if ci + 1 < nch:
                            scores_pe(ci + 1)
                        first, last = (ci == 0), (ci == nch - 1)
                        P.op("pe", lambda e, kc=kc, nq=nq, co=co, ptile=ptile, ops_=ops_, first=first, last=last: e.matmul(
                             ops_[:, co:co + nq], lhsT=vb_[:, kc, :], rhs=ptile[:, 0:nq], start=first, stop=last),
                             r=["vb", ptk], w=[ok])
                        P.op("pe", lambda e, nq=nq, co=co, ptile=ptile, lps=lps, first=first, last=last: e.matmul(
                             lps[:, co:co + nq], lhsT=ones_bf, rhs=ptile[:, 0:nq], start=first, stop=last),
                             r=["ones_bf", ptk], w=[lk])
                    P.op("dve", lambda e, lps=lps: e.reciprocal(out=rl, in_=lps[:, :]), r=[lk], w=["rl"])
                    P.op("dve", lambda e, hd=hd, q0=q0, ops_=ops_: e.tensor_tensor(out=yT[:, hd, q0:q0 + 512], in0=ops_[:, :],
                         in1=rl, op=ALU.mult), r=[ok, "rl"], w=[("yT", hd)])
            barrier()
            AR.pop()

        G = {}

        def phaseG():
            AR.off = off_mix
            AR.push()
            modb = AR.get(F32, [4 * D])
            g1b, sh2b, gs2b, g2b = modb[:, 0:D], modb[:, D:2 * D], modb[:, 2 * D:3 * D], modb[:, 3 * D:4 * D]
            G["g2b"] = g2b
            gates_all = AR.get(F32, [16, 4])
            G["off_keep"] = AR.off
            dma("sp", modb, modb_d, r=["modb_scr"], w=["modb"])
            wout = AR.get(BF16, [8, D])
            xt = [AR.get(F32, [D]), AR.get(F32, [D])]; x1t = [AR.get(F32, [D]), AR.get(F32, [D])]
            h2f = AR.get(F32, [D]); junk = AR.get(BF16, [D])
            h2b_all = AR.get(BF16, [16, D]); h2T = AR.get(F32, [8, 128]); wrf = AR.get(F32, [8, NE])
            brb = AR.get(F32, [NE]); lg = AR.get(F32, [NE]); top8 = AR.get(F32, [8]); idx8 = AR.get(U32, [8])
            ss = AR.get(F32, [16]); lnv = AR.get(F32, [16]); rs2 = AR.get(F32, [16])
            negv = AR.get(F32, [1]); e4 = AR.get(F32, [4]); se = AR.get(F32, [1]); rse = AR.get(F32, [1])
            idxf_all = AR.get(F32, [16, 4]); A_all = AR.get(BF16, [16, NE])
            dest_all = dest_sb[:, :, :]; io32 = AR.get(F32, [NE])
            eq = AR.get(F32, [NE]); s1_ = AR.get(F32, [1]); d1 = AR.get(F32, [1]); ov = AR.get(F32, [1])
            zf = AR.get(F32, [D]); pcol = AR.get(F32, [2]); tt_ = AR.get(F32, [1])
            cnt_b = AR.get(F32, [NE]); heavy_b = AR.get(F32, [NE]); hs_b = AR.get(F32, [NE]); one32 = AR.get(F32, [NE])
            hsk = AR.get(F32, [1]); lt_ = AR.get(F32, [1]); okh = AR.get(F32, [1]); dh_ = AR.get(F32, [1]); ehf = AR.get(F32, [1]); k128 = AR.get(F32, [8]); tmp8 = AR.get(F32, [8]); eq2 = AR.get(F32, [NE])
            eq4 = AR.get(F32, [4, NE]); eq4b = AR.get(F32, [4, NE]); hsk4 = AR.get(F32, [4]); s14 = AR.get(F32, [4]); lt4 = AR.get(F32, [4])
            d4 = AR.get(F32, [4]); dh4 = AR.get(F32, [4]); ok4 = AR.get(F32, [4]); ov4 = AR.get(F32, [4])
            G.update(gates_all=gates_all, dest_all=dest_all, h2b_all=h2b_all)
            dma("pool", wout, w_out.rearrange("(k p) n -> p k n", p=128), w=["wout"])
            dma("sp", wrf, w_router.rearrange("(k p) n -> p k n", p=128), w=["wrf"])
            dma("sp", brb, b_router_row.partition_broadcast(128), w=["brb"])
            dma("sp", io32, iota32_d, w=["io32"]); dma("sp", pcol, pcol_d, w=["pcol"]); dma("sp", k128, k128_d, w=["k128"])
            P.op("dve", lambda e: e.memset(zf, 0.0), w=["zf"])
            dma("sp", yall_d[NTOT:NTOT + 128, :], zf, r=["zf"], w=["Yall"])
            pbtF = [pbt[0][:, :].bitcast(F32), pbt[1][:, :].bitcast(F32)]
            h2f2 = [h2f, AR.get(F32, [D])]; h2T2 = [h2T, AR.get(F32, [8, 128])]
            lg2 = [lg, AR.get(F32, [NE])]; top82 = [top8, AR.get(F32, [8])]; idx82 = [idx8, AR.get(U32, [8])]
            negv2 = [negv, AR.get(F32, [1])]; e42 = [e4, AR.get(F32, [4])]; se2 = [se, AR.get(F32, [1])]; rse2 = [rse, AR.get(F32, [1])]

            def tile_steps(i):
                steps = []
                S = lambda *a_, **k_: steps.append((a_, k_))
                b = i % 2
                K = lambda n: (n, b)
                h2f_, h2T_, lg_, top8_, idx8_, negv_, e4_, se_, rse_ = h2f2[b], h2T2[b], lg2[b], top82[b], idx82[b], negv2[b], e42[b], se2[b], rse2[b]
                S("sp", (lambda e, b=b, i=i: e.dma_start(out=xt[b], in_=x_own[i * 128:(i + 1) * 128, :])), w=[("xt", b)], dma=True)
                ps = pb[b]; pk = "pb%d" % b
                for nb in range(2):
                    for k in range(8):
                        S("pe", lambda e, k=k, nb=nb, i=i, ps=ps: e.matmul(ps[:, :], lhsT=mixT[:, k, i * 128:(i + 1) * 128],
                             rhs=wout[:, k, nb * 512:(nb + 1) * 512], start=(k == 0), stop=(k == 7)), r=["mixT", "wout"], w=[pk])
                    S("dve", lambda e, nb=nb, b=b, ps=ps: e.tensor_tensor(out=x1t[b][:, nb * 512:(nb + 1) * 512], in0=ps[:, :],
                         in1=g1b[:, nb * 512:(nb + 1) * 512], op=ALU.mult), r=[pk, "modb"], w=[("x1t", b)])
                S("dve", lambda e, b=b: e.tensor_tensor(out=x1t[b], in0=x1t[b], in1=xt[b], op=ALU.add),
                     r=[("x1t", b), ("xt", b)], w=[("x1t", b)])
                S("sp", (lambda e, b=b, i=i: e.dma_start(out=x1_d[i * 128:(i + 1) * 128, :], in_=x1t[b])), r=[("x1t", b)], w=["x1d"], dma=True)
                S("act", lambda e, b=b, i=i: e.activation(out=junk, in_=x1t[b], func=AF.Square, accum_out=ss[:, i:i + 1]),
                     r=[("x1t", b)], w=["junk", ("ss", i)])
                S("act", lambda e, i=i: e.activation(out=lnv[:, i:i + 1], in_=ss[:, i:i + 1], func=AF.Ln, scale=1.0 / D, bias=EPS),
                     r=[("ss", i)], w=[("lnv", i)])
                S("act", lambda e, i=i: e.activation(out=rs2[:, i:i + 1], in_=lnv[:, i:i + 1], func=AF.Exp, scale=-0.5),
                     r=[("lnv", i)], w=[("rs2", i)])
                S("dve", lambda e, b=b, i=i: e.scalar_tensor_tensor(out=h2f_, in0=x1t[b], scalar=rs2[:, i:i + 1], in1=gs2b,
                     op0=ALU.mult, op1=ALU.mult), r=[("x1t", b), ("rs2", i), "modb"], w=[K("h2f")])
                S("dve", lambda e: e.tensor_tensor(out=h2f_, in0=h2f_, in1=sh2b, op=ALU.add), r=[K("h2f"), "modb"], w=[K("h2f")])
                S("act", lambda e, i=i: e.activation(out=h2b_all[:, i, :], in_=h2f_, func=AF.Copy), r=[K("h2f")], w=[("h2b", i)])
                tpb = [pb[2 + 2 * b], pb[3 + 2 * b]]; tkk = ["pb%d" % (2 + 2 * b), "pb%d" % (3 + 2 * b)]
                for c in range(8):
                    tp = tpb[c // 4]; tk = tkk[c // 4]
                    S("pe", lambda e, c=c, tp=tp: e.transpose(tp[:, (c % 4) * 128:(c % 4 + 1) * 128],
                         h2f_[:, c * 128:(c + 1) * 128], ident_f), r=[K("h2f"), "ident_f"], w=[tk])
                S("act", lambda e: e.activation(out=h2T_[:, 0:4, :], in_=tpb[0][:, :].rearrange("p (a b) -> p a b", a=4),
                     func=AF.Copy), r=[tkk[0]], w=[("h2T", b, 0)])
                S("dve", lambda e: e.tensor_copy(out=h2T_[:, 4:8, :], in_=tpb[1][:, :].rearrange("p (a b) -> p a b", a=4)),
                     r=[tkk[1]], w=[("h2T", b, 1)])
                lp = pbtF[b]; lpk = "pbt%d" % b
                for k in range(8):
                    S("pe", lambda e, k=k: e.matmul(lp[:, 0:NE], lhsT=h2T_[:, k, :], rhs=wrf[:, k, :],
                         start=(k == 0), stop=(k == 7)), r=[("h2T", b, 0), ("h2T", b, 1), "wrf"], w=[lpk])
                S("dve", lambda e: e.tensor_tensor(out=lg_, in0=lp[:, 0:NE], in1=brb, op=ALU.add), r=[lpk, "brb"], w=[K("lg")])
                S("dve", lambda e: e.max(out=top8_, in_=lg_), r=[K("lg")], w=[K("top8")])
                S("dve", lambda e: e.max_index(out=idx8_, in_max=top8_, in_values=lg_), r=[K("lg"), K("top8")], w=[K("idx8")])
                S("dve", lambda e: e.tensor_scalar(out=negv_, in0=top8_[:, 0:1], scalar1=-1.0, scalar2=None, op0=ALU.mult),
                     r=[K("top8")], w=[K("negv")])
                S("act", lambda e: e.activation(out=e4_, in_=top8_[:, 0:4], func=AF.Exp, bias=negv_[:, 0:1], accum_out=se_),
                     r=[K("top8"), K("negv")], w=[K("e4"), K("se")])
                S("dve", lambda e: e.reciprocal(out=rse_, in_=se_), r=[K("se")], w=[K("rse")])
                S("dve", lambda e, i=i: e.tensor_scalar(out=gates_all[:, i, :], in0=e4_, scalar1=rse_[:, 0:1], scalar2=None,
                     op0=ALU.mult), r=[K("e4"), K("rse")], w=[("gates", i)])
                S("dve", lambda e, i=i: e.tensor_scalar(out=A_all[:, i, :], in0=lg_, scalar1=top8_[:, 3:4], scalar2=None,
                     op0=ALU.is_ge), r=[K("lg"), K("top8")], w=[("A", i)])
                S("dve", lambda e, i=i: e.tensor_copy(out=idxf_all[:, i, :], in_=idx8_[:, 0:4]), r=[K("idx8")], w=[("idxf", i)])
                return steps

            import itertools
            ch0 = []; ch1 = [None] * 24
            for i2 in range(0, NT, 2):
                ch0 += tile_steps(i2); ch1 += tile_steps(i2 + 1)
            for xs in itertools.zip_longest(ch0, ch1):
                for x in xs:
                    if x is not None:
                        P.op(*x[0], **x[1])
            for i in range(NT):
                P.op("pe", lambda e, i=i: e.matmul(pb[0][:, 0:NE], lhsT=ones_bf, rhs=A_all[:, i, :], start=(i == 0), stop=(i == NT - 1)),
                     r=["ones_bf", "A"], w=["pb0"])
            P.op("dve", lambda e: e.tensor_copy(out=cnt_b, in_=pb[0][:, 0:NE]), r=["pb0"], w=["cnt_b"])
            P.op("dve", lambda e: e.tensor_scalar(out=heavy_b, in0=cnt_b, scalar1=float(CAP), scalar2=None, op0=ALU.is_gt),
                 r=["cnt_b"], w=["heavy_b"])
            P.op("dve", lambda e: e.memset(one32, 1.0), w=["one32"])
            P.op("dve", lambda e: e.tensor_tensor_scan(out=hs_b, data0=one32, data1=heavy_b, initial=0.0, op0=ALU.mult, op1=ALU.add),
                 r=["one32", "heavy_b"], w=["hs_b"])
            P.op("dve", lambda e: e.tensor_tensor(out=hs_b, in0=hs_b, in1=heavy_b, op=ALU.subtract), r=["hs_b", "heavy_b"], w=["hs_b"])
            for h_ in range(NH):
                P.op("dve", lambda e, h_=h_: e.tensor_scalar(out=eq2, in0=hs_b, scalar1=float(h_), scalar2=None, op0=ALU.is_equal),
                     r=["hs_b"], w=["eq2"])
                P.op("dve", lambda e: e.tensor_tensor(out=eq2, in0=eq2, in1=heavy_b, op=ALU.mult), r=["eq2", "heavy_b"], w=["eq2"])
                P.op("dve", lambda e: e.tensor_tensor(out=eq2, in0=eq2, in1=io32, op=ALU.mult), r=["eq2", "io32"], w=["eq2"])
                P.op("dve", lambda e: e.tensor_reduce(out=ehf, in_=eq2, axis=AX.X, op=ALU.add), r=["eq2"], w=["ehf"])
                P.op("dve", lambda e: e.scalar_tensor_tensor(out=dh_, in0=ehf, scalar=1024.0, in1=pcol[:, 1:2], op0=ALU.mult, op1=ALU.add),
                     r=["ehf", "pcol"], w=["dh_"])
                P.op("dve", lambda e: e.tensor_scalar(out=tmp8, in0=k128, scalar1=dh_[:, 0:1], scalar2=None, op0=ALU.add),
                     r=["k128", "dh_"], w=["tmp8"])
                P.op("dve", lambda e, h_=h_: e.tensor_copy(out=idxw_sb[:, h_, :], in_=tmp8), r=["tmp8"], w=[("idxw", h_)])
                P.op("dve", lambda e: e.scalar_tensor_tensor(out=dh_, in0=ehf, scalar=128.0, in1=pcol[:, 1:2], op0=ALU.mult, op1=ALU.add),
                     r=["ehf", "pcol"], w=["dh_"])
                P.op("dve", lambda e, h_=h_: e.tensor_copy(out=idxb_sb[:, h_, 0:1], in_=dh_), r=["dh_"], w=[("idxb", h_)])
                P.op("dve", lambda e, h_=h_: e.tensor_copy(out=idxb_sb[:, h_, 1:2], in_=ehf), r=["ehf"], w=[("idxb", h_)])
            for i in range(NT):
                P.op("pe", lambda e, i=i: e.matmul(pb[5][:, 0:NE], lhsT=triu_bf, rhs=A_all[:, i, :], start=True, stop=(i == 0)),
                     r=["triu_bf", "A"], w=["pb5"])
                for j in range(i):
                    P.op("pe", lambda e, j=j, i=i: e.matmul(pb[5][:, 0:NE], lhsT=ones_bf, rhs=A_all[:, j, :], start=False,
                         stop=(j == i - 1)), r=["ones_bf", "A"], w=["pb5"])
                idx4 = idxf_all[:, i, :]
                P.op("dve", lambda e, idx4=idx4: e.tensor_tensor(out=eq4, in0=io32.unsqueeze(1).to_broadcast([128, 4, NE]),
                     in1=idx4.unsqueeze(2).to_broadcast([128, 4, NE]), op=ALU.is_equal), r=["io32", ("idxf", i)], w=["eq4"])
                P.op("dve", lambda e: e.tensor_tensor(out=eq4b, in0=eq4, in1=hs_b.unsqueeze(1).to_broadcast([128, 4, NE]), op=ALU.mult),
                     r=["eq4", "hs_b"], w=["eq4b"])
                P.op("dve", lambda e: e.tensor_reduce(out=hsk4, in_=eq4b, axis=AX.X, op=ALU.add), r=["eq4b"], w=["hsk4"])
                P.op("dve", lambda e: e.tensor_tensor(out=eq4, in0=eq4, in1=pb[5][:, 0:NE].unsqueeze(1).to_broadcast([128, 4, NE]),
                     op=ALU.mult), r=["eq4", "pb5"], w=["eq4"])
                P.op("dve", lambda e: e.tensor_reduce(out=s14, in_=eq4, axis=AX.X, op=ALU.add), r=["eq4"], w=["s14"])
                P.op("dve", lambda e: e.tensor_scalar(out=lt4, in0=s14, scalar1=float(CAP), scalar2=None, op0=ALU.is_le), r=["s14"], w=["lt4"])
                P.op("dve", lambda e, idx4=idx4: e.scalar_tensor_tensor(out=d4, in0=idx4, scalar=float(CAP), in1=s14, op0=ALU.mult, op1=ALU.add),
                     r=[("idxf", i), "s14"], w=["d4"])
                P.op("dve", lambda e: e.scalar_tensor_tensor(out=dh4, in0=hsk4, scalar=float(CH), in1=s14, op0=ALU.mult, op1=ALU.add),
                     r=["hsk4", "s14"], w=["dh4"])
                P.op("dve", lambda e: e.tensor_scalar(out=ok4, in0=hsk4, scalar1=float(NH) - 0.5, scalar2=None, op0=ALU.is_lt), r=["hsk4"], w=["ok4"])
                P.op("dve", lambda e: e.tensor_scalar(out=ov4, in0=s14, scalar1=float(CAP + CH), scalar2=None, op0=ALU.is_le), r=["s14"], w=["ov4"])
                P.op("dve", lambda e: e.tensor_tensor(out=ok4, in0=ok4, in1=ov4, op=ALU.mult), r=["ok4", "ov4"], w=["ok4"])
                P.op("dve", lambda e: e.tensor_scalar(out=ov4, in0=lt4, scalar1=-1.0, scalar2=1.0, op0=ALU.mult, op1=ALU.add), r=["lt4"], w=["ov4"])
                P.op("dve", lambda e: e.tensor_tensor(out=ok4, in0=ok4, in1=ov4, op=ALU.mult), r=["ok4", "ov4"], w=["ok4"])
                P.op("dve", lambda e: e.tensor_scalar(out=d4, in0=d4, scalar1=-1.0, scalar2=pcol[:, 0:1], op0=ALU.add, op1=ALU.subtract),
                     r=["d4", "pcol"], w=["d4"])
                P.op("dve", lambda e: e.tensor_scalar(out=dh4, in0=dh4, scalar1=float(NE * CAP - CAP - 1), scalar2=pcol[:, 0:1],
                     op0=ALU.add, op1=ALU.subtract), r=["dh4", "pcol"], w=["dh4"])
                P.op("dve", lambda e: e.tensor_tensor(out=d4, in0=d4, in1=lt4, op=ALU.mult), r=["d4", "lt4"], w=["d4"])
                P.op("dve", lambda e: e.tensor_tensor(out=dh4, in0=dh4, in1=ok4, op=ALU.mult), r=["dh4", "ok4"], w=["dh4"])
                P.op("dve", lambda e: e.tensor_tensor(out=d4, in0=d4, in1=dh4, op=ALU.add), r=["d4", "dh4"], w=["d4"])
                P.op("dve", lambda e: e.tensor_scalar(out=d4, in0=d4, scalar1=pcol[:, 0:1], scalar2=None, op0=ALU.add), r=["d4", "pcol"], w=["d4"])
                P.op("dve", lambda e, i=i: e.tensor_copy(out=dest_all[:, i, :], in_=d4), r=["d4"], w=[("dest", i)])
            for i in range(NT):
                sg_ = stage_sb[i % 2]; sgk = ("stage", i % 2)
                P.op("act", lambda e, i=i, sg_=sg_: e.activation(out=sg_[:, :], in_=h2b_all[:, i, :], func=AF.Copy),
                     r=[("h2b", i)], w=[sgk])
                for k in range(4):
                    P.op("pool", lambda e, i=i, k=k, sg_=sg_: e.indirect_dma_start(out=xg_d[:, :],
                         out_offset=bass.IndirectOffsetOnAxis(ap=dest_sb[:, i, k:k + 1], axis=0), in_=sg_[:, :],
                         in_offset=None),
                         r=[("dest", i), sgk], w=["Xg"], dma=True)
            if "gates" in dbg_out:
                finals.append(dma("sp", dbg_out["gates"], gates_all, r=["gates"]))
                finals.append(dma("sp", dbg_out["idxf"], idxf_all, r=["idxf"]))
                P.op("dve", lambda e: e.tensor_copy(out=gates_all, in_=dest_all), r=["dest", "gates"], w=["gates"])
                finals.append(dma("sp", dbg_out["dest"], gates_all, r=["gates"]))
            barrier()

        def phaseI():
            NS = CAP // 128
            AR.off = off_base
            xe = AR.get(BF16, [NS, D]); actT = AR.get(BF16, [8, CAP])
            assert AR.off <= off_mix
            AR.off = G["off_keep"]
            AR.push()
            wup = [AR.get(BF16, [8, 2 * D]), AR.get(BF16, [8, 2 * D])]
            wdn = [AR.get(BF16, [8, D]), AR.get(BF16, [8, D])]
            xeT = AR.get(BF16, [8, CAP])
            ye = [AR.get(F32, [D]), AR.get(F32, [D])]
            bdn = [AR.get(F32, [D]), AR.get(F32, [D])]
            bupc = AR.get(F32, [NE, 16]); buph = AR.get(F32, [NH, 16])
            gm = [AR.get(F32, [512]), AR.get(F32, [512])]; sgm = [AR.get(F32, [512]), AR.get(F32, [512])]
            l1 = [AR.get(F32, [512]), AR.get(F32, [512])]
            dma("sp", bupc, bup_col.rearrange("p (a b) -> p a b", a=NE), w=["bupc"])
            dbanks = [(pb[4], "pb4"), (pb[5], "pb5"), (pbt[0][:, :].bitcast(F32), "pbt0"), (pbt[1][:, :].bitcast(F32), "pbt1")]
            wupf = w_up.rearrange("e k n -> (e k) n"); wdnf = w_down.rearrange("e k n -> (e k) n")

            def load_w(e_, wb):
                wu3 = w_up[e_].rearrange("(k p) n -> p k n", p=128)
                for hh in range(2):
                    dma("pool", wup[wb][:, :, hh * D:(hh + 1) * D], wu3[:, :, hh * D:(hh + 1) * D], w=[("wup", wb)])
                dma("pool", wdn[wb], w_down[e_].rearrange("(k p) n -> p k n", p=128), w=[("wdn", wb)])
                dma("sp", bdn[wb], b_down[e_:e_ + 1, :].partition_broadcast(128), w=[("bdn", wb)])

            def load_w_heavy(h_, wb):
                for k in range(8):
                    P.op("pool", lambda e, k=k: e.indirect_dma_start(out=wup[wb][:, k, :], out_offset=None, in_=wupf[:, :],
                         in_offset=bass.IndirectOffsetOnAxis(ap=idxw_sb[:, h_, k:k + 1], axis=0)), r=[("idxw", h_)], w=[("wup", wb)], dma=True)
                for k in range(8):
                    P.op("pool", lambda e, k=k: e.indirect_dma_start(out=wdn[wb][:, k, :], out_offset=None, in_=wdnf[:, :],
                         in_offset=bass.IndirectOffsetOnAxis(ap=idxw_sb[:, h_, k:k + 1], axis=0)), r=[("idxw", h_)], w=[("wdn", wb)], dma=True)
                P.op("pool", lambda e: e.indirect_dma_start(out=buph[:, h_, :], out_offset=None, in_=bq_d[:, :],
                     in_offset=bass.IndirectOffsetOnAxis(ap=idxb_sb[:, h_, 0:1], axis=0)), r=[("idxb", h_)], w=[("buph", h_)], dma=True)
                P.op("pool", lambda e: e.indirect_dma_start(out=bdn[wb], out_offset=None, in_=b_down[:, :],
                     in_offset=bass.IndirectOffsetOnAxis(ap=idxb_sb[:, h_, 1:2], axis=0)), r=[("idxb", h_)], w=[("bdn", wb)], dma=True)

            def transposes(row0, ns):
                dma("sp", xe[:, 0:ns, :], xg_d[row0:row0 + ns * 128, :].rearrange("(s p) d -> p s d", p=128), r=["Xg"], w=["xe"])
                for c in range(8):
                    tp = pbt[c % 2]; tk = "pbt%d" % (c % 2)
                    for s_ in range(ns):
                        P.op("pe", lambda e, c=c, s_=s_, tp=tp: e.transpose(tp[:, s_ * 128:(s_ + 1) * 128],
                             xe[:, s_, c * 128:(c + 1) * 128], ident_bf), r=["xe", "ident_bf"], w=[tk])
                    if c % 2 == 0:
                        P.op("act", lambda e, c=c, tp=tp: e.activation(out=xeT[:, c, 0:ns * 128], in_=tp[:, 0:ns * 128], func=AF.Copy),
                             r=[tk], w=[("xeT", c)])
                    else:
                        P.op("dve", lambda e, c=c, tp=tp: e.tensor_copy(out=xeT[:, c, 0:ns * 128], in_=tp[:, 0:ns * 128]), r=[tk], w=[("xeT", c)])

            def up(wb, nn, bcol, bkey):
                n0 = 0
                for f in range(8):
                    bb = f % 2
                    gps = pb[bb * 2]; gk = "pb%d" % (bb * 2); lps = pb[bb * 2 + 1]; lk = "pb%d" % (bb * 2 + 1)
                    for k in range(8):
                        P.op("pe", lambda e, k=k, f=f, gps=gps: e.matmul(gps[:, 0:nn],
                             lhsT=wup[wb][:, k, f * 128:(f + 1) * 128], rhs=xeT[:, k, n0:n0 + nn], start=(k == 0), stop=(k == 7)),
                             r=[("wup", wb), "xeT"], w=[gk])
                    for k in range(8):
                        P.op("pe", lambda e, k=k, f=f, lps=lps: e.matmul(lps[:, 0:nn],
                             lhsT=wup[wb][:, k, D + f * 128:D + (f + 1) * 128], rhs=xeT[:, k, n0:n0 + nn], start=(k == 0),
                             stop=(k == 7)), r=[("wup", wb), "xeT"], w=[lk])
                    g_, s__, l_ = gm[bb], sgm[bb], l1[bb]
                    P.op("dve", lambda e, f=f, gps=gps, g_=g_: e.tensor_scalar(out=g_[:, 0:nn], in0=gps[:, 0:nn],
                         scalar1=bcol(f), scalar2=7.0, op0=ALU.add, op1=ALU.min), r=[gk, bkey], w=[("gm", bb)])
                    P.op("act", lambda e, g_=g_, s__=s__: e.activation(out=s__[:, 0:nn], in_=g_[:, 0:nn], func=AF.Sigmoid,
                         scale=1.702), r=[("gm", bb)], w=[("sgm", bb)])
                    P.op("dve", lambda e, f=f, lps=lps, l_=l_: e.tensor_scalar(out=l_[:, 0:nn], in0=lps[:, 0:nn],
                         scalar1=bcol(8 + f), scalar2=7.0, op0=ALU.add, op1=ALU.min), r=[lk, bkey], w=[("l1", bb)])
                    P.op("dve", lambda e, l_=l_: e.tensor_scalar(out=l_[:, 0:nn], in0=l_[:, 0:nn], scalar1=-7.0, scalar2=1.0,
                         op0=ALU.max, op1=ALU.add), r=[("l1", bb)], w=[("l1", bb)])
                    P.op("dve", lambda e, g_=g_, s__=s__: e.tensor_tensor(out=g_[:, 0:nn], in0=g_[:, 0:nn], in1=s__[:, 0:nn],
                         op=ALU.mult), r=[("gm", bb), ("sgm", bb)], w=[("gm", bb)])
                    P.op("dve", lambda e, f=f, g_=g_, l_=l_: e.tensor_tensor(out=actT[:, f, n0:n0 + nn],
                         in0=g_[:, 0:nn], in1=l_[:, 0:nn], op=ALU.mult), r=[("gm", bb), ("l1", bb)], w=[("actT", f)])

            def down(wb, row0, ns):
                for s_ in range(ns):
                    yb = ye[s_ % 2]; yk = ("ye", s_ % 2)
                    for nb in range(2):
                        ps, pk = dbanks[(s_ * 2 + nb) % 4]
                        for f in range(8):
                            P.op("pe", lambda e, f=f, s_=s_, nb=nb, ps=ps: e.matmul(ps[:, :], lhsT=actT[:, f, s_ * 128:(s_ + 1) * 128],
                                 rhs=wdn[wb][:, f, nb * 512:(nb + 1) * 512], start=(f == 0), stop=(f == 7)),
                                 r=["actT", ("wdn", wb)], w=[pk])
                        P.op("dve", lambda e, nb=nb, ps=ps, yb=yb: e.tensor_tensor(out=yb[:, nb * 512:(nb + 1) * 512], in0=ps[:, :],
                             in1=bdn[wb][:, nb * 512:(nb + 1) * 512], op=ALU.add), r=[pk, ("bdn", wb)], w=[yk])
                    r0 = row0 + s_ * 128
                    dma("sp", yall_d[r0:r0 + 128, :], yb, r=[yk], w=["Yall"])

            NU = NE + NH
            def unit(u):
                if u < NE:
                    return dict(row0=u * CAP, ns=NS, nn=CAP, bcol=(lambda f, u=u: bupc[:, u, f:f + 1]), bkey="bupc")
                h_ = u - NE
                return dict(row0=NE * CAP + h_ * CH, ns=CH // 128, nn=CH, bcol=(lambda f, h_=h_: buph[:, h_, f:f + 1]), bkey=("buph", h_))

            def load_unit(u, wb):
                if u < NE:
                    load_w(u, wb)
                else:
                    load_w_heavy(u - NE, wb)

            hpos = {6: 0, 13: 1, 20: 2, 27: 3}
            order = []
            for e_ in range(NE):
                order.append(e_)
                if e_ in hpos:
                    order.append(NE + hpos[e_])
            load_unit(order[0], 0)
            transposes(unit(order[0])["row0"], unit(order[0])["ns"])
            for pos_, u in enumerate(order):
                U = unit(u)
                wb = pos_ % 2
                if pos_ + 1 < len(order):
                    load_unit(order[pos_ + 1], (pos_ + 1) % 2)
                up(wb, U["nn"], U["bcol"], U["bkey"])
                if pos_ + 1 < len(order):
                    U2 = unit(order[pos_ + 1])
                    transposes(U2["row0"], U2["ns"])
                down(wb, U["row0"], U["ns"])
            barrier()
            AR.pop()

        def phaseJ():
            AR.off = G["off_keep"]
            AR.push()
            gates_all = G["gates_all"]; g2b = G["g2b"]
            yg = [[AR.get(F32, [D]) for _ in range(4)] for _ in range(2)]
            x1t = [AR.get(F32, [D]), AR.get(F32, [D])]; acc = [AR.get(F32, [D]), AR.get(F32, [D])]
            for i in range(NT):
                b = i % 2
                dma("sp", x1t[b], x1_d[i * 128:(i + 1) * 128, :], r=["x1d"], w=[("x1t", b)])
                for k in range(4):
                    P.op("pool", lambda e, i=i, k=k, b=b: e.indirect_dma_start(out=yg[b][k], out_offset=None, in_=yall_d[:, :],
                         in_offset=bass.IndirectOffsetOnAxis(ap=dest_sb[:, i, k:k + 1], axis=0)),
                         r=["Yall", ("dest", i)], w=[("yg", b, k)], dma=True)
                a_ = acc[b]; ak = ("acc", b)
                P.op("dve", lambda e, i=i, b=b, a_=a_: e.tensor_scalar(out=a_, in0=yg[b][0], scalar1=gates_all[:, i, 0:1], scalar2=None,
                     op0=ALU.mult), r=[("yg", b, 0), "gates"], w=[ak])
                for k in (1, 2, 3):
                    P.op("dve", lambda e, i=i, b=b, k=k, a_=a_: e.scalar_tensor_tensor(out=a_, in0=yg[b][k],
                         scalar=gates_all[:, i, k:k + 1], in1=a_, op0=ALU.mult, op1=ALU.add), r=[("yg", b, k), "gates", ak], w=[ak])
                P.op("dve", lambda e, a_=a_: e.tensor_tensor(out=a_, in0=a_, in1=g2b, op=ALU.mult), r=[ak, "modb"], w=[ak])
                P.op("dve", lambda e, b=b, a_=a_: e.tensor_tensor(out=a_, in0=a_, in1=x1t[b], op=ALU.add), r=[ak, ("x1t", b)], w=[ak])
                finals.append(dma("act", out_d[i * 128:(i + 1) * 128, :], a_, r=[ak]))
            AR.pop()

        phases = [phaseB, phaseC, phaseD, phaseE, phaseF, phaseG, phaseI, phaseJ]
        for i, ph in enumerate(phases):
            if stage < i + 2:
                break
            ph()
        def dump_bf(name, src, n):
            if name not in dbg_out:
                return
            AR.push()
            stg = [AR.get(F32, [n]), AR.get(F32, [n])]
            for k in range(8):
                P.op("dve", lambda e, k=k: e.tensor_copy(out=stg[k % 2], in_=src[:, k, :]), r=[name], w=[("stg", k % 2)])
                finals.append(dma("sp", dbg_out[name][:, k, :], stg[k % 2], r=[("stg", k % 2)]))
            AR.pop()
        if stage < 7:
            dump_bf("hT", hT, LT); dump_bf("yT", yT, T); dump_bf("mixT", mixT, T)
        if "x1" in dbg_out:
            AR.push()
            stg = AR.get(F32, [D])
            for i in range(NT):
                dma("sp", stg, x1_d[i * 128:(i + 1) * 128, :], r=["x1d"], w=["stg"])
                finals.append(dma("sp", dbg_out["x1"][i * 128:(i + 1) * 128, :], stg, r=["stg"]))
            AR.pop()
        P.emit(finals)
    return nc


def _consts():
    bf = ml_dtypes.bfloat16
    c = {}
    c["ident_bf"] = np.eye(128, dtype=np.float32).astype(bf)
    c["ident_f"] = np.eye(128, dtype=np.float32)
    k = np.arange(128)
    c["triu_bf"] = (k[:, None] <= k[None, :]).astype(np.float32).astype(bf)
    tb = np.zeros((128, 256), np.float32)
    tb[:, :128] = np.where(k[:, None] <= k[None, :], 0.0, NEG)
    c["tribias"] = tb.astype(bf)
    rm = np.zeros((128, 32), np.float32)
    for m in range(16):
        rm[m + 16, m] = 1.0
        rm[m, m + 16] = 1.0
    c["rm_f"] = rm
    half = 16
    freqs = (np.float32(500000.0) ** (-np.arange(half, dtype=np.float32) / np.float32(half))).astype(np.float32)
    fc = np.zeros((32, 2), np.float32)
    fc[:, 0] = np.concatenate([freqs, freqs])
    fc[:16, 1] = -1.0
    fc[16:, 1] = 1.0
    c["freq_col"] = fc
    c["iota32"] = np.tile(np.arange(NE, dtype=np.float32)[None, :], (128, 1))
    c["pcol"] = np.stack([NTOT + np.arange(128, dtype=np.float32), np.arange(128, dtype=np.float32)], axis=1).copy()
    c["k128"] = np.tile((128.0 * np.arange(8, dtype=np.float32))[None, :], (128, 1))
    return c


def _col(v, nchunk):
    return np.ascontiguousarray(np.asarray(v, np.float32).reshape(nchunk, 128).T)


def prep_inputs(inp):
    f = lambda a: np.ascontiguousarray(np.asarray(a, np.float32))
    x = f(inp["x"]); c = f(inp["c"]); positions = np.asarray(inp["positions"]).astype(np.int32)
    shared = {
        "ada_w": f(inp["ada_w"][0]),
        "ada_b_col": _col(inp["ada_b"][0], 48),
        "ada_b_row": f(inp["ada_b"][0][None, 2 * D:]),
        "n1g_col": _col(inp["norm1_g"][0], 8),
        "n2g_row": f(inp["norm2_g"][0][None, :]),
        "w_in": f(inp["w_in"][0]),
        "convw_col": np.ascontiguousarray(f(inp["conv_w"][0]).T.reshape(8, 128, 4).transpose(1, 0, 2).reshape(128, 32)),
        "convb_col": _col(inp["conv_b"][0], 8),
        "w_rg_a": f(inp["w_rg_a"][0]), "brga_col": _col(inp["b_rg_a"][0], 8),
        "w_rg_x": f(inp["w_rg_x"][0]), "brgx_col": _col(inp["b_rg_x"][0], 8),
        "lam_col": _col(inp["lru_lambda"][0], 8),
        "qg_col": f(inp["q_norm_g"][0][:, None]), "kg_col": f(inp["k_norm_g"][0][:, None]),
        "bgate_col": np.ascontiguousarray(f(inp["b_gate"][0]).reshape(2, 8, 128).transpose(2, 0, 1).reshape(128, 16)),
        "w_branch": f(inp["w_branch"][0]), "w_out": f(inp["w_out"][0]),
        "w_router": f(inp["w_router"][0]), "b_router_row": f(inp["b_router"][0][None, :]),
        "w_up": f(inp["w_up"][0]),
        "bup_col": np.ascontiguousarray(f(inp["b_up"][0]).reshape(NE, 16, 128).transpose(2, 0, 1).reshape(128, NE * 16)),
        "w_down": f(inp["w_down"][0]), "b_down": f(inp["b_down"][0]),
        "bq": np.ascontiguousarray(f(inp["b_up"][0]).reshape(NE, 16, 128).transpose(0, 2, 1).reshape(NE * 128, 16)),
    }
    shared.update(_consts())
    maps = []
    for core in range(8):
        b, h = core // 2, core % 2
        m = dict(shared)
        m["x_own"] = np.ascontiguousarray(x[b, h * T:(h + 1) * T])
        m["x_pre"] = np.ascontiguousarray(x[b, 0:T])
        m["pos"] = np.ascontiguousarray(np.concatenate([positions[b, 0:T], positions[b, h * T:(h + 1) * T]])[None, :])
        m["c_col"] = _col(c[b], 8)
        m["hflag"] = np.full((128, 1), float(h), np.float32)
        vb = np.full((8, 16), NEG, np.float32)
        for j in range(8):
            for n in range(16):
                if n < 8 + j and (n >= 8 or h == 1):
                    vb[j, n] = 0.0
        m["vbias"] = np.ascontiguousarray(np.tile(vb.reshape(1, 128), (128, 1)))
        maps.append(m)
    return maps


_NC_CACHE = {}


def kernel(**inputs):
    maps = prep_inputs(inputs)
    if "nc" not in _NC_CACHE:
        _NC_CACHE["nc"] = build_program()
    res = run_bass_kernel_spmd(_NC_CACHE["nc"], maps, core_ids=list(range(8)))
    out = np.zeros((4, 2 * T, D), np.float32)
    for core in range(8):
        b, h = core // 2, core % 2
        out[b, h * T:(h + 1) * T] = res.results[core]["out"]
    return out
```

```python
import numpy as np
import ml_dtypes
from contextlib import ExitStack
import concourse.bass as bass
import concourse.mybir as mybir
from concourse.bass_utils import run_bass_kernel_spmd

F32 = mybir.dt.float32
BF16 = mybir.dt.bfloat16
I32 = mybir.dt.int32
U32 = mybir.dt.uint32
AF = mybir.ActivationFunctionType
ALU = mybir.AluOpType
AX = mybir.AxisListType

D = 1024
T = 2048
NT = 16
LT = 4096
NE = 32
CAP = 512
NH = 4
CH = 256
NTOT = NE * CAP + NH * CH
EPS = 1e-6
NEG = -30000.0
FLAGS = {}


class _Op:
    __slots__ = ("eng", "fn", "deps", "signal", "sidx", "dma", "sem", "semval", "n")


class Prog:
    CE = ("pe", "act", "dve", "pool", "sp")
    EPOCH = 12000

    def __init__(self, nc, stack):
        self.nc = nc
        self.stack = stack
        self.ops = {e: [] for e in ("pe", "act", "dve", "pool", "sp")}
        self.state = {}
        self.subs = {}
        self.ndma = {"sp": 0, "act": 0, "pool": 0}
        self.dma_last = {}
        self.NDS = 8
        self.n = 0

    def sb(self, name, shape, dt):
        return self.stack.enter_context(self.nc.sbuf_tensor(name, list(shape), dt))

    def ps(self, name, shape, dt):
        return self.stack.enter_context(self.nc.psum_tensor(name, list(shape), dt))

    def _keys(self, k):
        if isinstance(k, tuple):
            name = k[0]
            self.subs.setdefault(name, set()).add(k)
            return [k, name]
        else:
            return [k] + list(self.subs.get(k, ()))

    def op(self, eng, fn, r=(), w=(), dma=False):
        o = _Op()
        o.eng, o.fn, o.deps, o.signal, o.dma = eng, fn, set(), False, dma
        o.sem = o.semval = o.sidx = None
        o.n = self.n
        self.n += 1
        for k in r:
            for kk in self._keys(k):
                st = self.state.get(kk)
                if st and st[0] is not None:
                    o.deps.add(st[0])
        for k in w:
            for kk in self._keys(k):
                st = self.state.get(kk)
                if st:
                    if st[0] is not None:
                        o.deps.add(st[0])
                    for x in st[1]:
                        o.deps.add(x)
        for k in r:
            st = self.state.setdefault(k, [None, []])
            st[1].append(o)
        for k in w:
            self.state[k] = [o, []]
            if not isinstance(k, tuple):
                for kk in self.subs.get(k, ()):
                    self.state[kk] = [o, []]
        o.deps.discard(o)
        if dma:
            j = self.ndma[eng] % self.NDS
            self.ndma[eng] += 1
            key = (eng, j)
            prev = self.dma_last.get(key)
            if prev is not None:
                o.deps.add(prev)
                o.semval = prev.semval + 16
            else:
                o.semval = 16
            o.sem = key
            self.dma_last[key] = o
        for x in o.deps:
            if not x.dma and not (x.eng == "pe" and eng == "pe" and not dma):
                x.signal = True
        self.ops[eng].append(o)
        return o

    def emit(self, final_ops):
        nc = self.nc
        st = self.stack
        nsig = {}
        for e in self.CE:
            c = 0
            for o in self.ops[e]:
                if o.signal and not o.dma:
                    o.sidx = c
                    c += 1
            nsig[e] = c
        esem = {}
        for e in self.CE:
            for ep in range(nsig[e] // self.EPOCH + 1):
                esem[(e, ep)] = st.enter_context(nc.semaphore("s_%s_%d" % (e, ep)))
        dsem = {}
        for q in ("sp", "act", "pool"):
            for j in range(min(self.NDS, self.ndma[q])):
                dsem[(q, j)] = st.enter_context(nc.semaphore("d_%s_%d" % (q, j)))
        fin = st.enter_context(nc.semaphore("fin"))
        semname = {id(v): k for k, v in list(esem.items()) + list(dsem.items())}
        block = st.enter_context(nc.Block())
        EP = self.EPOCH

        def run(ename, eng, extra=None):
            waited = {}
            for o in self.ops[ename]:
                need = {}
                for x in o.deps:
                    if x.dma:
                        s, v = dsem[x.sem], x.semval
                    else:
                        if x.eng == "pe" and ename == "pe" and not o.dma:
                            continue
                        s, v = esem[(x.eng, x.sidx // EP)], x.sidx % EP + 1
                    if waited.get(s, 0) >= v:
                        continue
                    if need.get(s, 0) < v:
                        need[s] = v
                for s, v in need.items():
                    eng.wait_ge(s, v)
                    waited[s] = v
                if FLAGS.get("trace"):
                    print(ename, o.n, "waits", [(semname[id(s)], v) for s, v in need.items()],
                          "sig", (o.sidx if o.signal and not o.dma else None), "dma", (o.sem, o.semval) if o.dma else None)
                ins = o.fn(eng)
                if o.dma:
                    ins.then_inc(dsem[o.sem], 16)
                elif o.signal:
                    ins.then_inc(esem[(ename, o.sidx // EP)], 1)
            if extra:
                extra(eng, waited)

        def fin_sp(eng, waited):
            for x in final_ops:
                s, v = dsem[x.sem], x.semval
                if waited.get(s, 0) < v:
                    eng.wait_ge(s, v)
                    waited[s] = v

        @block.sync
        def _(e):
            run("sp", e, fin_sp)

        @block.scalar
        def _(e):
            run("act", e)

        @block.vector
        def _(e):
            run("dve", e)

        @block.gpsimd
        def _(e):
            run("pool", e)

        @block.tensor
        def _(e):
            run("pe", e)


ARENA_F32 = 51600


class Arena:
    def __init__(self, big):
        self.big = big
        self.off = 0
        self.marks = []

    def push(self):
        self.marks.append(self.off)

    def pop(self):
        self.off = self.marks.pop()

    def get(self, dt, shape, parts=128):
        n = 1
        for s in shape:
            n *= s
        esz = 4 if dt in (F32, I32, U32) else 2
        words = (n * esz + 3) // 4
        words = (words + 7) // 8 * 8
        a = self.big[0:parts, self.off:self.off + words]
        self.off += words
        assert self.off <= ARENA_F32, ("arena overflow", self.off)
        if dt != F32:
            a = a.bitcast(dt)
        a = a[:, 0:n]
        if len(shape) == 2:
            a = a.rearrange("p (a b) -> p a b", a=shape[0])
        elif len(shape) == 3:
            a = a.rearrange("p (a b c) -> p a b c", a=shape[0], b=shape[1])
        return a


def build_program(stage=99, dbg=None):
    nc = bass.Bass("TRN2", target_bir_lowering=False)
    dbg = dbg or {}

    def din(name, shape, dt=F32):
        return nc.dram_tensor(name, list(shape), dt, kind="ExternalInput").ap()

    x_own = din("x_own", [T, D]); x_pre = din("x_pre", [T, D])
    pos = din("pos", [1, LT], I32)
    c_col = din("c_col", [128, 8]); hflag_d = din("hflag", [128, 1]); vbias_d = din("vbias", [128, 8 * 16])
    ada_w = din("ada_w", [D, 6 * D]); ada_b_col = din("ada_b_col", [128, 48]); ada_b_row = din("ada_b_row", [1, 4 * D])
    n1g_col = din("n1g_col", [128, 8]); n2g_row = din("n2g_row", [1, D])
    w_in = din("w_in", [D, 7 * D])
    convw_col = din("convw_col", [128, 32]); convb_col = din("convb_col", [128, 8])
    w_rg_a = din("w_rg_a", [8, 128, 128]); brga_col = din("brga_col", [128, 8])
    w_rg_x = din("w_rg_x", [8, 128, 128]); brgx_col = din("brgx_col", [128, 8])
    lam_col = din("lam_col", [128, 8]); qg_col = din("qg_col", [128, 1]); kg_col = din("kg_col", [128, 1])
    bgate_col = din("bgate_col", [128, 16])
    w_branch = din("w_branch", [2, D, D]); w_out = din("w_out", [D, D])
    w_router = din("w_router", [D, NE]); b_router_row = din("b_router_row", [1, NE])
    w_up = din("w_up", [NE, D, 2 * D]); bup_col = din("bup_col", [128, NE * 16])
    w_down = din("w_down", [NE, D, D]); b_down = din("b_down", [NE, D])
    ident_bf_d = din("ident_bf", [128, 128], BF16); ident_f_d = din("ident_f", [128, 128])
    triu_bf_d = din("triu_bf", [128, 128], BF16); tribias_d = din("tribias", [128, 256], BF16)
    rm_f_d = din("rm_f", [128, 32]); freq_d = din("freq_col", [32, 2]); iota32_d = din("iota32", [128, NE]); pcol_d = din("pcol", [128, 2]); k128_d = din("k128", [128, 8]); bq_d = din("bq", [NE * 128, 16])
    out_d = nc.dram_tensor("out", [T, D], F32, kind="ExternalOutput").ap()
    xg_d = nc.dram_tensor("xg_scr", [NTOT + 128, D], BF16, kind="Internal").ap()
    yall_d = nc.dram_tensor("yall_scr", [NTOT + 128, D], F32, kind="Internal").ap()
    x1_d = nc.dram_tensor("x1_scr", [T, D], F32, kind="Internal").ap()
    modb_d = nc.dram_tensor("modb_scr", [128, 4 * D], F32, kind="Internal").ap()
    dbg_out = {}
    for k, shp in dbg.items():
        dbg_out[k] = nc.dram_tensor("dbg_" + k, list(shp), F32, kind="ExternalOutput").ap()

    st = ExitStack()
    with st:
        P = Prog(nc, st)
        big = P.sb("arena", [128, ARENA_F32], F32)
        AR = Arena(big)
        dest_sb = P.sb("dest_sb", [128, 16, 4], I32)
        idxw_sb = P.sb("idxw_sb", [128, NH, 8], I32)
        idxb_sb = P.sb("idxb_sb", [128, NH, 2], I32)
        stage_sb = [P.sb("stage_sb%d" % i, [128, D], BF16) for i in range(2)]
        pb = [P.ps("pb%d" % i, [128, 512], F32) for i in range(6)]
        pbt = [P.ps("pbt%d" % i, [128, 1024], BF16) for i in range(2)]
        finals = []
        cnt = [0]

        def uid(s):
            cnt[0] += 1
            return "%s_%d" % (s, cnt[0])

        def dma(q, out, in_, r=(), w=()):
            return P.op(q, lambda e: e.dma_start(out=out, in_=in_), r=r, w=w, dma=True)

        def barrier():
            lasts = []
            for e in ("pe", "act", "dve", "pool", "sp"):
                if P.ops[e]:
                    lasts.append(P.ops[e][-1])
            lasts += list(P.dma_last.values())
            for e in ("pe", "act", "dve", "pool", "sp"):
                o = P.op(e, lambda en: en.nop())
                for x in lasts:
                    if x is not o:
                        o.deps.add(x)
                        if not x.dma:
                            x.signal = True

        def dump(name, ap, key):
            if name not in dbg_out:
                return
            finals.append(dma("sp", dbg_out[name], ap, r=[key]))

        ident_bf = AR.get(BF16, [128]); ident_f = AR.get(F32, [128])
        ones_bf = AR.get(BF16, [128]); triu_bf = AR.get(BF16, [128]); tribias = AR.get(BF16, [256])
        hflag = AR.get(F32, [1]); vbias = AR.get(F32, [8, 16])
        sh1c = AR.get(F32, [8]); gs1c = AR.get(F32, [8])
        sccol = AR.get(F32, [8])
        dma("sp", ident_bf, ident_bf_d, w=["ident_bf"]); dma("sp", ident_f, ident_f_d, w=["ident_f"])
        dma("sp", triu_bf, triu_bf_d, w=["triu_bf"]); dma("sp", tribias, tribias_d, w=["tribias"])
        dma("sp", hflag, hflag_d, w=["hflag"])
        dma("sp", vbias, vbias_d.rearrange("p (a b) -> p a b", a=8), w=["vbias"])
        P.op("dve", lambda e: e.memset(ones_bf, 1.0), w=["ones_bf"])
        zt = AR.get(BF16, [2, D])
        P.op("dve", lambda e: e.memset(zt, 0.0), w=["zt"])

        AR.push()
        ccol = AR.get(F32, [8])
        abcol = AR.get(F32, [48]); n1g = AR.get(F32, [8])
        wA = [AR.get(F32, [8, 512]), AR.get(F32, [8, 512])]
        dma("sp", ccol, c_col, w=["ccol"]); dma("sp", abcol, ada_b_col, w=["abcol"]); dma("sp", n1g, n1g_col, w=["n1g"])
        P.op("act", lambda e: e.activation(out=sccol, in_=ccol, func=AF.Silu), r=["ccol"], w=["sccol"])
        adw = ada_w.rearrange("(k p) n -> p k n", p=128)
        for g in range(4):
            wb = wA[g % 2]; wk = ("wA", g % 2)
            dma("sp" if g % 2 == 0 else "act", wb, adw[:, :, g * 512:(g + 1) * 512], w=[wk])
            for j in range(4):
                for k in range(8):
                    P.op("pe", lambda e, j=j, k=k, g=g, wb=wb: e.matmul(pb[0][:, g * 4 + j:g * 4 + j + 1],
                         lhsT=wb[:, k, j * 128:(j + 1) * 128], rhs=sccol[:, k:k + 1], start=(k == 0), stop=(k == 7)),
                         r=[wk, "sccol"], w=[("pb0", g * 4 + j)])
        P.op("dve", lambda e: e.tensor_tensor(out=sh1c, in0=pb[0][:, 0:8], in1=abcol[:, 0:8], op=ALU.add),
             r=["pb0", "abcol"], w=["sh1c"])
        P.op("dve", lambda e: e.scalar_tensor_tensor(out=gs1c, in0=pb[0][:, 8:16], scalar=1.0, in1=abcol[:, 8:16],
             op0=ALU.add, op1=ALU.add), r=["pb0", "abcol"], w=["gs1c"])
        P.op("dve", lambda e: e.tensor_tensor(out=gs1c, in0=gs1c, in1=n1g, op=ALU.mult), r=["gs1c", "n1g"], w=["gs1c"])
        dump("sh1c", sh1c, "sh1c"); dump("gs1c", gs1c, "gs1c")
        barrier()
        AR.pop()
        off_base = AR.off
        mixT = AR.get(BF16, [8, T])
        off_mix = AR.off
        hT = AR.get(BF16, [8, LT])
        yT = AR.get(BF16, [8, T])
        win3 = w_in.rearrange("(k p) n -> p k n", p=128)

        def phaseB():
            AR.push()
            xt = [AR.get(F32, [D]), AR.get(F32, [D])]
            junk = AR.get(BF16, [D])
            xnb = [AR.get(BF16, [D]), AR.get(BF16, [D])]
            ss = AR.get(F32, [32]); lnv = AR.get(F32, [32]); rstd = AR.get(F32, [32])
            def tile_steps(i):
                steps = []
                S = lambda *a_, **k_: steps.append((a_, k_))
                b = i % 2
                src = x_pre if i < 16 else x_own
                r0 = (i % 16) * 128
                S("sp", (lambda e, b=b, r0=r0, src=src: e.dma_start(out=xt[b], in_=src[r0:r0 + 128, :])), w=[("xt", b)], dma=True)
                S("act", lambda e, b=b, i=i: e.activation(out=junk, in_=xt[b], func=AF.Square, accum_out=ss[:, i:i + 1]),
                     r=[("xt", b)], w=["junk", ("ss", i)])
                S("act", lambda e, i=i: e.activation(out=lnv[:, i:i + 1], in_=ss[:, i:i + 1], func=AF.Ln, scale=1.0 / D, bias=EPS),
                     r=[("ss", i)], w=[("lnv", i)])
                S("act", lambda e, i=i: e.activation(out=rstd[:, i:i + 1], in_=lnv[:, i:i + 1], func=AF.Exp, scale=-0.5),
                     r=[("lnv", i)], w=[("rstd", i)])
                S("dve", lambda e, b=b, i=i: e.tensor_scalar(out=xnb[b], in0=xt[b], scalar1=rstd[:, i:i + 1], scalar2=None,
                     op0=ALU.mult), r=[("xt", b), ("rstd", i)], w=[("xnb", b)])
                pbT = pbt[b]
                pk = "pbt%d" % b
                for c in range(8):
                    S("pe", lambda e, b=b, c=c, pbT=pbT: e.transpose(pbT[:, c * 128:(c + 1) * 128],
                         xnb[b][:, c * 128:(c + 1) * 128], ident_bf), r=[("xnb", b), "ident_bf"], w=[(pk, c)])
                for c in range(8):
                    if FLAGS.get("B_noevac") or c >= FLAGS.get("B_nevac", 8):
                        continue
                    dst = hT[:, c, i * 128:(i + 1) * 128]
                    if FLAGS.get("B_dst2"):
                        dst = junk[:, c * 128:(c + 1) * 128]
                    if FLAGS.get("B_evaccopy"):
                        srcp = pb[3][:, 0:128] if FLAGS.get("B_src2") else pbT[:, c * 128:(c + 1) * 128]
                        S("dve", lambda e, c=c, dst=dst, srcp=srcp: e.tensor_copy(out=dst, in_=srcp),
                             r=[pk if FLAGS.get("B_waitall") else (pk, c)], w=[("hT", i)])
                        continue
                    if c % 2 == 0 and not FLAGS.get("B_dveonly"):
                        S("act", lambda e, c=c, dst=dst, pbT=pbT: e.activation(out=dst, in_=pbT[:, c * 128:(c + 1) * 128],
                             func=AF.Identity, scale=gs1c[:, c:c + 1], bias=sh1c[:, c:c + 1]),
                             r=[pk, "gs1c", "sh1c"], w=[("hT", i)])
                    else:
                        S("dve", lambda e, c=c, dst=dst, pbT=pbT: e.tensor_scalar(out=dst, in0=pbT[:, c * 128:(c + 1) * 128],
                             scalar1=gs1c[:, c:c + 1], scalar2=sh1c[:, c:c + 1], op0=ALU.mult, op1=ALU.add),
                             r=[pk, "gs1c", "sh1c"], w=[("hT", i)])
                return steps

            modb = AR.get(F32, [4 * D]); scb = AR.get(F32, [8, 128]); n2gb = AR.get(F32, [D])
            wA = [AR.get(F32, [8, 256]), AR.get(F32, [8, 256])]
            adw = ada_w.rearrange("(k p) n -> p k n", p=128)
            dma("pool", modb, ada_b_row.partition_broadcast(128), w=["modb"])
            dma("pool", n2gb, n2g_row.partition_broadcast(128), w=["n2gb"])
            for k in range(8):
                P.op("dve", lambda e, k=k: e.tensor_copy(out=scb[:, k, :], in_=sccol[:, k:k + 1].to_broadcast([128, 128])),
                     r=["sccol"], w=[("scb", k)])

            def wload(gg):
                return [(("pool", (lambda e, gg=gg: e.dma_start(out=wA[gg % 2], in_=adw[:, :, 2 * D + gg * 256:2 * D + (gg + 1) * 256]))),
                         dict(w=[("wA", gg % 2)], dma=True))]

            def wmm(gg):
                st_ = []
                wb = wA[gg % 2]; wk = ("wA", gg % 2)
                pp = pb[4 + gg % 2]; pk = "pb%d" % (4 + gg % 2)
                for k in range(8):
                    st_.append((("pe", (lambda e, k=k, wb=wb, pp=pp: e.matmul(pp[:, 0:256], lhsT=scb[:, k, :], rhs=wb[:, k, :],
                               start=(k == 0), stop=(k == 7)))), dict(r=[wk, "scb"], w=[pk])))
                st_.append((("dve", (lambda e, gg=gg, pp=pp: e.tensor_tensor(out=modb[:, gg * 256:(gg + 1) * 256],
                           in0=pp[:, 0:256], in1=modb[:, gg * 256:(gg + 1) * 256], op=ALU.add))), dict(r=[pk, "modb"], w=["modb"])))
                return st_

            import itertools
            ch0 = []; ch1 = [None] * 11; ch2 = wload(0) + wload(1) + [None] * 6
            for i2 in range(0, 32, 2):
                ch0 += tile_steps(i2); ch1 += tile_steps(i2 + 1)
                gg = i2 // 2
                grp = wmm(gg) + (wload(gg + 2) if gg + 2 < 16 else [])
                ch2 += grp + [None] * max(0, 22 - len(grp))
            for xs in itertools.zip_longest(ch0, ch1, ch2):
                for x in xs:
                    if x is not None:
                        P.op(*x[0], **x[1])
            gs2b_ = modb[:, 2 * D:3 * D]
            P.op("dve", lambda e: e.scalar_tensor_tensor(out=gs2b_, in0=gs2b_, scalar=1.0, in1=n2gb, op0=ALU.add, op1=ALU.mult),
                 r=["modb", "n2gb"], w=["modb"])
            dma("sp", modb_d, modb, r=["modb"], w=["modb_scr"])
            barrier()
            AR.pop()

        def phaseC():
            AR.push()
            cw = AR.get(F32, [8, 4]); cb = AR.get(F32, [8]); nba = AR.get(F32, [8]); nbx = AR.get(F32, [8])
            lam = AR.get(F32, [8]); s1 = AR.get(F32, [8]); s2 = AR.get(F32, [8])
            wga = AR.get(BF16, [8, 128]); wgx = AR.get(BF16, [8, 128])
            wxr = AR.get(BF16, [8, 512]); wgr = AR.get(BF16, [8, 512])
            BUF = []

            def mkset():
                d_ = dict(
                    xb=AR.get(F32, [515]), hist=AR.get(F32, [3]), xc=AR.get(F32, [512]), xcb=AR.get(BF16, [512]),
                    er=AR.get(F32, [512]), eg=AR.get(F32, [512]), aa=AR.get(F32, [512]), a2=AR.get(F32, [512]),
                    hb=AR.get(F32, [512]), carry=AR.get(F32, [1]), fx=AR.get(F32, [1]))
                d_["uu"] = d_["er"]; d_["gz"] = d_["er"]
                return d_
            save_ = AR.off
            AR.off = off_base
            BUF.append(mkset()); BUF.append(mkset())
            assert AR.off <= off_mix
            AR.off = save_
            BUF.append(mkset()); BUF.append(mkset())
            PB = [(pb[0], "pb0"), (pb[1], "pb1"), (pb[2], "pb2"), (pb[3], "pb3"), (pb[4], "pb4"), (pb[5], "pb5"),
                  (pbt[0][:, :].bitcast(F32), "pbt0"), (pbt[1][:, :].bitcast(F32), "pbt1")]
            dma("sp", cw, convw_col.rearrange("p (a b) -> p a b", a=8), w=["cw"]); dma("sp", cb, convb_col, w=["cb"])
            dma("sp", nba, brga_col, w=["nba"]); dma("sp", nbx, brgx_col, w=["nbx"]); dma("sp", lam, lam_col, w=["lam"])
            dma("pool", wga, w_rg_a.rearrange("n w v -> w n v"), w=["wga"])
            dma("pool", wgx, w_rg_x.rearrange("n w v -> w n v"), w=["wgx"])
            P.op("dve", lambda e: e.tensor_scalar(out=nba, in0=nba, scalar1=-1.0, scalar2=None, op0=ALU.mult), r=["nba"], w=["nba"])
            P.op("dve", lambda e: e.tensor_scalar(out=nbx, in0=nbx, scalar1=-1.0, scalar2=None, op0=ALU.mult), r=["nbx"], w=["nbx"])
            P.op("act", lambda e: e.activation(out=s1, in_=lam, func=AF.Exp, scale=-1.0), r=["lam"], w=["s1"])
            P.op("act", lambda e: e.activation(out=s1, in_=s1, func=AF.Ln, bias=1.0), r=["s1"], w=["s1"])
            P.op("dve", lambda e: e.tensor_scalar(out=s2, in0=s1, scalar1=-16.0, scalar2=None, op0=ALU.mult), r=["s1"], w=["s2"])
            P.op("dve", lambda e: e.tensor_scalar(out=s1, in0=s1, scalar1=-8.0, scalar2=None, op0=ALU.mult), r=["s1", "s2"], w=["s1"])
            GC = 1.5957691216057308

            def piece(c, j, par):
                B = BUF[par]
                steps = []
                S = lambda *a_, **k_: steps.append((a_, k_))
                cc = c % 4
                K = lambda n: (n, par)
                xc, xcb, er, eg, aa, a2, uu, gz, carry, fx = (B[n] for n in ("xc", "xcb", "er", "eg", "aa", "a2", "uu", "gz", "carry", "fx"))
                mm, ig = a2, eg
                xps, xk = PB[2 * par]
                for k in range(8):
                    S("pe", lambda e, k=k, j=j, cc=cc, xps=xps: e.matmul(xps[:, :],
                         lhsT=wxr[:, k, cc * 128:(cc + 1) * 128], rhs=hT[:, k, j * 512:(j + 1) * 512],
                         start=(k == 0), stop=(k == 7)), r=["wxr", "hT"], w=[xk])
                xb = B["xb"]; xbk = ("xb", par); hist = B["hist"]; hk_ = ("hist", par)
                if j == 0:
                    S("dve", lambda e, xb=xb: e.memset(xb[:, 0:3], 0.0), w=[xbk])
                elif j == 4:
                    S("dve", lambda e, xb=xb, hist=hist: e.tensor_scalar(out=xb[:, 0:3], in0=hist,
                         scalar1=hflag[:, 0:1], scalar2=None, op0=ALU.mult), r=[hk_, "hflag"], w=[xbk])
                else:
                    S("dve", lambda e, xb=xb, hist=hist: e.tensor_copy(out=xb[:, 0:3], in_=hist), r=[hk_], w=[xbk])
                S("dve", lambda e, xb=xb, xps=xps: e.tensor_copy(out=xb[:, 3:515], in_=xps[:, :]),
                     r=[xk, xbk], w=[xbk])
                S("dve", lambda e, xb=xb, c=c: e.tensor_scalar(out=xc, in0=xb[:, 3:515], scalar1=cw[:, c, 0:1],
                     scalar2=cb[:, c:c + 1], op0=ALU.mult, op1=ALU.add), r=[xbk, "cw", "cb"], w=[K("xc")])
                for i in (1, 2, 3):
                    S("dve", lambda e, xb=xb, c=c, i=i: e.scalar_tensor_tensor(out=xc, in0=xb[:, 3 - i:515 - i],
                         scalar=cw[:, c, i:i + 1], in1=xc, op0=ALU.mult, op1=ALU.add), r=[xbk, "cw", K("xc")], w=[K("xc")])
                S("dve", lambda e, xb=xb, hist=hist: e.tensor_copy(out=hist, in_=xb[:, 512:515]), r=[xbk], w=[hk_])
                S("dve", lambda e: e.tensor_copy(out=xcb, in_=xc), r=[K("xc")], w=[K("xcb")])
                rps, rk = PB[2 * par + 1]; gps, gk = PB[2 * par]
                S("pe", lambda e, c=c, rps=rps: e.matmul(rps[:, :], lhsT=wga[:, c, :], rhs=xcb, start=True, stop=True),
                     r=["wga", K("xcb")], w=[rk])
                S("pe", lambda e, c=c, gps=gps: e.matmul(gps[:, :], lhsT=wgx[:, c, :], rhs=xcb, start=True, stop=True),
                     r=["wgx", K("xcb")], w=[gk])
                S("act", lambda e, c=c, rps=rps: e.activation(out=er, in_=rps[:, :], func=AF.Exp, scale=-1.0,
                     bias=nba[:, c:c + 1]), r=[rk, "nba"], w=[K("er")])
                S("act", lambda e: e.activation(out=er, in_=er, func=AF.Ln, bias=1.0), r=[K("er")], w=[K("er")])
                S("act", lambda e: e.activation(out=er, in_=er, func=AF.Exp, scale=-1.0), r=[K("er")], w=[K("er")])
                S("act", lambda e, c=c, gps=gps: e.activation(out=eg, in_=gps[:, :], func=AF.Exp, scale=-1.0,
                     bias=nbx[:, c:c + 1]), r=[gk, "nbx"], w=[K("eg")])
                S("act", lambda e: e.activation(out=eg, in_=eg, func=AF.Ln, bias=1.0), r=[K("eg")], w=[K("eg")])
                S("act", lambda e: e.activation(out=eg, in_=eg, func=AF.Exp, scale=-1.0), r=[K("eg")], w=[K("eg")])
                S("act", lambda e, c=c: e.activation(out=aa, in_=er, func=AF.Exp, scale=s1[:, c:c + 1]), r=[K("er"), "s1"], w=[K("aa")])
                S("act", lambda e, c=c: e.activation(out=a2, in_=er, func=AF.Exp, scale=s2[:, c:c + 1]), r=[K("er"), "s2"], w=[K("a2")])
                S("pool", lambda e: e.tensor_scalar(out=mm, in0=a2, scalar1=-1.0, scalar2=1.0, op0=ALU.mult, op1=ALU.add),
                     r=[K("a2")], w=[K("a2")])
                S("act", lambda e: e.activation(out=mm, in_=mm, func=AF.Ln), r=[K("a2")], w=[K("a2")])
                S("act", lambda e: e.activation(out=mm, in_=mm, func=AF.Exp, scale=0.5), r=[K("a2")], w=[K("a2")])
                if j == 0:
                    S("dve", lambda e: e.memset(mm[:, 0:1], 1.0), r=[K("a2")], w=[K("a2")])
                elif j == 4:
                    S("dve", lambda e: e.tensor_scalar(out=fx, in0=mm[:, 0:1], scalar1=-1.0, scalar2=hflag[:, 0:1],
                         op0=ALU.add, op1=ALU.mult), r=[K("a2"), "hflag"], w=[K("fx")])
                    S("dve", lambda e: e.tensor_scalar(out=mm[:, 0:1], in0=fx, scalar1=1.0, scalar2=None, op0=ALU.add),
                         r=[K("fx"), K("a2")], w=[K("a2")])
                S("dve", lambda e: e.tensor_tensor(out=uu, in0=mm, in1=ig, op=ALU.mult), r=[K("a2"), K("eg")], w=[K("er")])
                S("pool", lambda e: e.tensor_tensor(out=uu, in0=uu, in1=xc, op=ALU.mult), r=[K("er"), K("xc")], w=[K("er")])
                hdst = B["hb"]; hk = ("hb", par)
                if j == 0:
                    S("dve", lambda e, hdst=hdst: e.tensor_tensor_scan(out=hdst, data0=aa, data1=uu, initial=0.0,
                         op0=ALU.mult, op1=ALU.add), r=[K("aa"), K("er")], w=[hk])
                else:
                    S("dve", lambda e, hdst=hdst: e.tensor_tensor_scan(out=hdst, data0=aa, data1=uu, initial=carry,
                         op0=ALU.mult, op1=ALU.add), r=[K("aa"), K("er"), K("carry")], w=[hk])
                if j == 3:
                    S("dve", lambda e, hdst=hdst: e.tensor_scalar(out=carry, in0=hdst[:, 511:512], scalar1=hflag[:, 0:1],
                         scalar2=None, op0=ALU.mult), r=[hk, "hflag"], w=[K("carry")])
                elif j < 7:
                    S("dve", lambda e, hdst=hdst: e.tensor_copy(out=carry, in_=hdst[:, 511:512]), r=[hk], w=[K("carry")])
                if j >= 4:
                    jj = j - 4
                    gps2, gk2 = PB[2 * par]
                    for k in range(8):
                        S("pe", lambda e, k=k, jj=jj, cc=cc, gps2=gps2: e.matmul(gps2[:, :],
                             lhsT=wgr[:, k, cc * 128:(cc + 1) * 128], rhs=hT[:, k, T + jj * 512:T + (jj + 1) * 512],
                             start=(k == 0), stop=(k == 7)), r=["wgr", "hT"], w=[gk2])
                    S("act", lambda e, gps2=gps2: e.activation(out=gz, in_=gps2[:, :], func=AF.Square), r=[gk2], w=[K("er")])
                    S("pool", lambda e: e.tensor_scalar(out=gz, in0=gz, scalar1=0.044715, scalar2=1.0, op0=ALU.mult, op1=ALU.add),
                         r=[K("er")], w=[K("er")])
                    S("dve", lambda e, gps2=gps2: e.tensor_tensor(out=gz, in0=gz, in1=gps2[:, :], op=ALU.mult), r=[K("er"), gk2], w=[K("er")])
                    S("act", lambda e: e.activation(out=gz, in_=gz, func=AF.Exp, scale=-GC), r=[K("er")], w=[K("er")])
                    S("act", lambda e: e.activation(out=gz, in_=gz, func=AF.Ln, bias=1.0), r=[K("er")], w=[K("er")])
                    S("act", lambda e: e.activation(out=gz, in_=gz, func=AF.Exp, scale=-1.0), r=[K("er")], w=[K("er")])
                    S("dve", lambda e, gps2=gps2: e.tensor_tensor(out=gz, in0=gz, in1=gps2[:, :], op=ALU.mult), r=[K("er"), gk2], w=[K("er")])
                    S("dve", lambda e, jj=jj, c=c, hdst=hdst: e.tensor_tensor(out=yT[:, c, jj * 512:(jj + 1) * 512],
                         in0=hdst, in1=gz, op=ALU.mult), r=[hk, K("er")], w=[("yT", c)])
                return steps

            import itertools
            xg2 = xg_d[0:NTOT, :].rearrange("(e p r) d -> e p r d", p=128, r=2)
            for e_ in range(NTOT // 256):
                dma("sp", xg2[e_], zt, r=["zt"], w=["Xg"])
            for cg in range(2):
                dma("pool", wxr, win3[:, :, cg * 512:(cg + 1) * 512], w=["wxr"])
                dma("pool", wgr, win3[:, :, D + cg * 512:D + (cg + 1) * 512], w=["wgr"])
                c0 = cg * 4
                sts = []
                for q in range(4):
                    lst = [None] * (q * 12)
                    for j in range(8):
                        lst += piece(c0 + q, j, q)
                    sts.append(lst)
                for xs in itertools.zip_longest(*sts):
                    for x in xs:
                        if x is not None:
                            P.op(*x[0], **x[1])
            barrier()
            AR.pop()

        def merge(g, first):
            AR.push()
            wbr = [AR.get(BF16, [8, 512]), AR.get(BF16, [8, 512])]
            wgl = [AR.get(BF16, [8, 512]), AR.get(BF16, [8, 512])]
            bgc = AR.get(F32, [16]); sg = AR.get(F32, [512]); tmp = AR.get(F32, [512])
            dma("sp", bgc, bgate_col, w=["bgc"])
            wb3 = w_branch[g].rearrange("(k p) n -> p k n", p=128)
            for grp in range(2):
                dma("pool", wbr[grp], wb3[:, :, grp * 512:(grp + 1) * 512], w=[("wbr", grp)])
                c0 = 5 * D + g * D + grp * 512
                dma("pool", wgl[grp], win3[:, :, c0:c0 + 512], w=[("wgl", grp)])
            for m in range(8):
                grp, mm = m // 4, m % 4
                for pc in range(4):
                    zps = pb[pc % 2]; zk = "pb%d" % (pc % 2); gps = pb[2 + pc % 2]; gk = "pb%d" % (2 + pc % 2)
                    for k in range(8):
                        P.op("pe", lambda e, k=k, pc=pc, grp=grp, mm=mm, zps=zps: e.matmul(zps[:, :],
                             lhsT=wbr[grp][:, k, mm * 128:(mm + 1) * 128], rhs=yT[:, k, pc * 512:(pc + 1) * 512],
                             start=(k == 0), stop=(k == 7)), r=[("wbr", grp), "yT"], w=[zk])
                    for k in range(8):
                        P.op("pe", lambda e, k=k, pc=pc, grp=grp, mm=mm, gps=gps: e.matmul(gps[:, :],
                             lhsT=wgl[grp][:, k, mm * 128:(mm + 1) * 128], rhs=hT[:, k, T + pc * 512:T + (pc + 1) * 512],
                             start=(k == 0), stop=(k == 7)), r=[("wgl", grp), "hT"], w=[gk])
                    P.op("act", lambda e, gps=gps, m=m: e.activation(out=sg, in_=gps[:, :], func=AF.Sigmoid,
                         bias=bgc[:, g * 8 + m:g * 8 + m + 1]), r=[gk, "bgc"], w=["sg"])
                    dst = mixT[:, m, pc * 512:(pc + 1) * 512]
                    if first:
                        P.op("dve", lambda e, zps=zps, dst=dst: e.tensor_tensor(out=dst, in0=zps[:, :], in1=sg, op=ALU.mult),
                             r=[zk, "sg"], w=[("mixT", m)])
                    else:
                        P.op("dve", lambda e, zps=zps: e.tensor_tensor(out=tmp, in0=zps[:, :], in1=sg, op=ALU.mult),
                             r=[zk, "sg"], w=["tmp"])
                        P.op("dve", lambda e, dst=dst: e.tensor_tensor(out=dst, in0=dst, in1=tmp, op=ALU.add),
                             r=["tmp", ("mixT", m)], w=[("mixT", m)])
            barrier()
            AR.pop()

        def phaseD():
            merge(0, True)

        def phaseF():
            merge(1, False)

        def phaseE():
            AR.push()
            SCALE = 128.0 ** -0.5
            C32 = AR.get(BF16, [LT], parts=32); S32 = AR.get(BF16, [LT], parts=32)
            qgc = AR.get(F32, [1]); kgc = AR.get(F32, [1]); rmf = AR.get(F32, [32]); fq = AR.get(F32, [2], parts=32)
            dma("sp", qgc, qg_col, w=["qgc"]); dma("sp", kgc, kg_col, w=["kgc"])
            dma("sp", rmf, rm_f_d, w=["rmf"]); dma("sp", fq, freq_d, w=["fq"])
            AR.push()
            posi = AR.get(I32, [1024], parts=32); ang = AR.get(F32, [1024], parts=32); tq = AR.get(F32, [1024], parts=32)
            ki = AR.get(I32, [1024], parts=32); kf = AR.get(F32, [1024], parts=32); rr_ = AR.get(F32, [1024], parts=32)
            ones32 = AR.get(F32, [1], parts=32)
            P.op("dve", lambda e: e.memset(ones32, 1.0), w=["ones32"])
            TWO_PI = 6.283185307179586
            C1 = 6.28125
            C2 = TWO_PI - C1
            for pc in range(4):
                sl = slice(pc * 1024, (pc + 1) * 1024)
                dma("sp", posi, pos[:, sl].partition_broadcast(32), w=["posi"])
                P.op("dve", lambda e: e.tensor_copy(out=ang, in_=posi), r=["posi"], w=["ang"])
                P.op("dve", lambda e: e.tensor_scalar(out=ang, in0=ang, scalar1=fq[:, 0:1], scalar2=None, op0=ALU.mult),
                     r=["ang", "fq"], w=["ang"])
                for which in (0, 1):
                    off = 0.0 if which == 0 else np.pi / 2
                    P.op("dve", lambda e, off=off: e.tensor_scalar(out=tq, in0=ang, scalar1=float(off), scalar2=1.0 / TWO_PI,
                         op0=ALU.add, op1=ALU.mult), r=["ang"], w=["tq"])
                    P.op("dve", lambda e: e.tensor_copy(out=ki, in_=tq), r=["tq"], w=["ki"])
                    P.op("dve", lambda e: e.tensor_copy(out=kf, in_=ki), r=["ki"], w=["kf"])
                    P.op("dve", lambda e, off=off: e.scalar_tensor_tensor(out=rr_, in0=kf, scalar=-C1, in1=ang,
                         op0=ALU.mult, op1=ALU.add), r=["kf", "ang"], w=["rr"])
                    P.op("dve", lambda e, off=off: e.scalar_tensor_tensor(out=rr_, in0=kf, scalar=-C2, in1=rr_,
                         op0=ALU.mult, op1=ALU.add), r=["kf", "rr"], w=["rr"])
                    P.op("dve", lambda e, off=off: e.tensor_scalar(out=rr_, in0=rr_, scalar1=float(off), scalar2=3.14159,
                         op0=ALU.add, op1=ALU.min), r=["rr"], w=["rr"])
                    P.op("dve", lambda e: e.tensor_scalar(out=rr_, in0=rr_, scalar1=-3.14159, scalar2=None, op0=ALU.max),
                         r=["rr"], w=["rr"])
                    if which == 0:
                        P.op("act", lambda e, sl=sl: e.activation(out=S32[:, sl], in_=rr_, func=AF.Sin, scale=fq[:, 1:2]),
                             r=["rr", "fq"], w=[("S32", pc)])
                    else:
                        P.op("act", lambda e, sl=sl: e.activation(out=C32[:, sl], in_=rr_, func=AF.Sin), r=["rr"], w=[("C32", pc)])
            barrier()
            AR.pop()
            kT = AR.get(BF16, [LT]); vb_ = AR.get(BF16, [32, 128]); qT = AR.get(BF16, [T])
            mbT = AR.get(BF16, [T], parts=16); mball = AR.get(BF16, [16, 16])
            wq = AR.get(BF16, [8, 128]); wk_ = AR.get(BF16, [8, 128]); wv = AR.get(BF16, [8, 128])
            kraw2 = [AR.get(F32, [512]), AR.get(F32, [512])]; sq2 = [AR.get(BF16, [512]), AR.get(BF16, [512])]
            lnr = AR.get(F32, [512]); rstd = AR.get(F32, [512])
            t1 = AR.get(F32, [512], parts=32); t2 = AR.get(F32, [512], parts=32)
            kmT = AR.get(F32, [16])
            gb = AR.get(F32, [16]); mx = AR.get(F32, [8]); sel = AR.get(F32, [16])
            pT = [AR.get(BF16, [512]), AR.get(BF16, [512]), AR.get(BF16, [512])]
            rl = AR.get(F32, [512])

            pbtF = [pbt[0][:, :].bitcast(F32), pbt[1][:, :].bitcast(F32)]

            def piece_info(hd, j):
                if j < 8:
                    return wk_, kgc, "kgc", j * 512, kT[:, j * 512:(j + 1) * 512]
                jj = j - 8
                return wq, qgc, "qgc", T + jj * 512, qT[:, jj * 512:(jj + 1) * 512]

            def s1_proj(hd, j):
                wt, gcol, gkey, tok0, dstT = piece_info(hd, j)
                ps = pb[j % 2]; pk = "pb%d" % (j % 2)
                for k in range(8):
                    P.op("pe", lambda e, k=k, ps=ps, wt=wt, tok0=tok0: e.matmul(ps[:, :], lhsT=wt[:, k, :], rhs=hT[:, k, tok0:tok0 + 512],
                         start=(k == 0), stop=(k == 7)), r=["wqkv", "hT"], w=[pk])

            def s1_evac(hd, j):
                ps = pb[j % 2]; pk = "pb%d" % (j % 2)
                kr = kraw2[j % 2]; krk = ("kraw", j % 2); sq_ = sq2[j % 2]; sqk = ("sq", j % 2)
                P.op("act", lambda e, ps=ps, kr=kr: e.activation(out=kr, in_=ps[:, :], func=AF.Copy), r=[pk], w=[krk])
                P.op("act", lambda e, ps=ps, sq_=sq_: e.activation(out=sq_, in_=ps[:, :], func=AF.Square), r=[pk], w=[sqk])

            def s2a(hd, j):
                wt, gcol, gkey, tok0, dstT = piece_info(hd, j)
                kr = kraw2[j % 2]; krk = ("kraw", j % 2); sq_ = sq2[j % 2]; sqk = ("sq", j % 2)
                sp_ = pb[2 + j % 2]; sk = "pb%d" % (2 + j % 2)
                P.op("pe", lambda e, sp_=sp_, sq_=sq_: e.matmul(sp_[:, :], lhsT=ones_bf, rhs=sq_, start=True, stop=True),
                     r=["ones_bf", sqk], w=[sk])
                P.op("act", lambda e, sp_=sp_: e.activation(out=lnr, in_=sp_[:, :], func=AF.Ln, scale=1.0 / 128, bias=EPS),
                     r=[sk], w=["lnr"])
                P.op("act", lambda e: e.activation(out=rstd, in_=lnr, func=AF.Exp, scale=-0.5), r=["lnr"], w=["rstd"])
                P.op("dve", lambda e, kr=kr, gcol=gcol: e.scalar_tensor_tensor(out=kr, in0=kr, scalar=gcol[:, 0:1], in1=rstd,
                     op0=ALU.mult, op1=ALU.mult), r=[krk, gkey, "rstd"], w=[krk])

            def s2b(hd, j):
                wt, gcol, gkey, tok0, dstT = piece_info(hd, j)
                kr = kraw2[j % 2]; krk = ("kraw", j % 2)
                rp = pb[4 + j % 2]; rk = "pb%d" % (4 + j % 2)
                P.op("pe", lambda e, rp=rp, kr=kr: e.matmul(rp[0:32, :], lhsT=rmf, rhs=kr, start=True, stop=True),
                     r=["rmf", krk], w=[rk])
                P.op("dve", lambda e, kr=kr, tok0=tok0: e.tensor_tensor(out=t1, in0=kr[0:32, :], in1=C32[:, tok0:tok0 + 512],
                     op=ALU.mult), r=[krk, "C32"], w=["t1"])
                P.op("dve", lambda e, rp=rp, tok0=tok0: e.tensor_tensor(out=t2, in0=rp[0:32, :], in1=S32[:, tok0:tok0 + 512], op=ALU.mult),
                     r=[rk, "S32"], w=["t2"])
                P.op("dve", lambda e, kr=kr: e.tensor_tensor(out=kr[0:32, :], in0=t1, in1=t2, op=ALU.add), r=["t1", "t2", krk], w=[krk])
                P.op("act", lambda e, kr=kr, dstT=dstT: e.activation(out=dstT, in_=kr, func=AF.Copy), r=[krk], w=["qkT"])
                if j < 8:
                    P.op("dve", lambda e, j=j, kr=kr: e.tensor_reduce(out=kmT[:, 2 * j:2 * j + 2],
                         in_=kr.rearrange("p (b t) -> p b t", b=2), axis=AX.X, op=ALU.add), r=[krk], w=["kmT"])
                else:
                    jj = j - 8
                    gp = pb[4 + j % 2]; gk = rk
                    for qt in range(4):
                        P.op("pe", lambda e, qt=qt, gp=gp, kr=kr: e.matmul(gp[:, 64 + qt * 16:64 + (qt + 1) * 16],
                             lhsT=kr[:, qt * 128:(qt + 1) * 128], rhs=kmT, start=True, stop=True),
                             r=[krk, "kmT"], w=[gk])
                    for qt in range(4):
                        i = jj * 4 + qt
                        P.op("dve", lambda e, qt=qt, i=i, gp=gp: e.tensor_tensor(out=gb, in0=gp[:, 64 + qt * 16:64 + (qt + 1) * 16],
                             in1=vbias[:, i // 2, :], op=ALU.add), r=[gk, "vbias"], w=["gb"])
                        P.op("dve", lambda e: e.max(out=mx, in_=gb), r=["gb"], w=["mx"])
                        P.op("dve", lambda e: e.tensor_scalar(out=sel, in0=gb, scalar1=mx[:, 2:3], scalar2=-1.0,
                             op0=ALU.is_ge, op1=ALU.add), r=["gb", "mx"], w=["sel"])
                        P.op("dve", lambda e, i=i: e.scalar_tensor_tensor(out=mball[:, i, :], in0=sel, scalar=-NEG,
                             in1=vbias[:, i // 2, :], op0=ALU.mult, op1=ALU.min), r=["sel", "vbias"], w=["mball"])

            def v_group(hd, tg):
                vps = pbtF[tg % 2]; vk = "pbt%d" % (tg % 2)
                for tt in range(4):
                    tile_i = tg * 4 + tt
                    for k in range(8):
                        P.op("pe", lambda e, k=k, tt=tt, tile_i=tile_i, vps=vps: e.matmul(vps[:, tt * 128:(tt + 1) * 128],
                             lhsT=hT[:, k, tile_i * 128:(tile_i + 1) * 128], rhs=wv[:, k, :], start=(k == 0), stop=(k == 7)),
                             r=["wqkv", "hT"], w=[vk])
                P.op("act", lambda e, tg=tg, vps=vps: e.activation(out=vb_[:, tg * 4:(tg + 1) * 4, :],
                     in_=vps.rearrange("p (a b) -> p a b", a=4), func=AF.Copy), r=[vk], w=["vb"])

            for hd in range(8):
                dma("pool", wq, win3[:, :, 2 * D + hd * 128:2 * D + (hd + 1) * 128], w=["wqkv"])
                dma("pool", wk_, win3[:, :, 3 * D + hd * 128:3 * D + (hd + 1) * 128], w=["wqkv"])
                dma("pool", wv, win3[:, :, 4 * D + hd * 128:4 * D + (hd + 1) * 128], w=["wqkv"])
                NP_ = 12
                s1_proj(hd, 0)
                s1_evac(hd, 0)
                for j in range(NP_):
                    if j + 1 < NP_:
                        s1_proj(hd, j + 1)
                    if j < 8:
                        v_group(hd, j)
                    if j >= 1:
                        s2b(hd, j - 1)
                    if j + 1 < NP_:
                        s1_evac(hd, j + 1)
                    s2a(hd, j)
                s2b(hd, NP_ - 1)
                for half in range(2):
                    tp = pbt[half]; tk = "pbt%d" % half
                    for ii in range(8):
                        i = half * 8 + ii
                        P.op("pe", lambda e, i=i, ii=ii, tp=tp: e.transpose(tp[0:16, ii * 128:(ii + 1) * 128], mball[:, i, :], ident_bf),
                             r=["mball", "ident_bf"], w=[tk])
                    P.op("dve", lambda e, half=half, tp=tp: e.tensor_copy(out=mbT[:, half * 1024:(half + 1) * 1024], in_=tp[0:16, :]),
                         r=[tk], w=["mbT"])
                for g4 in range(4):
                    qbA = 8 + 2 * g4
                    q0 = g4 * 512
                    ops_ = pb[2 + g4 % 2]; ok = "pb%d" % (2 + g4 % 2)
                    lps = pb[4 + g4 % 2]; lk = "pb%d" % (4 + g4 % 2)
                    chunks = [("A0", 2 * qbA, 0, 512), ("A1", 2 * qbA + 1, 128, 384),
                              ("B0", 2 * qbA + 2, 256, 256), ("B1", 2 * qbA + 3, 384, 128)]
                    chunks += [("P", c, 0, 512) for c in range(2 * qbA)]

                    def scores_pe(ci):
                        kind, kc, co, nq = chunks[ci]
                        sps = pb[ci % 2]; sk = "pb%d" % (ci % 2)
                        qs = q0 + co
                        P.op("pe", lambda e, kc=kc, qs=qs, nq=nq, sps=sps: e.matmul(sps[:, 0:nq], lhsT=kT[:, kc * 128:(kc + 1) * 128],
                             rhs=qT[:, qs:qs + nq], start=True, stop=False), r=["qkT"], w=[sk])
                        if kind == "P":
                            n = kc // 2
                            P.op("pe", lambda e, n=n, qs=qs, nq=nq, sps=sps: e.matmul(sps[:, 0:nq],
                                 lhsT=ident_bf[0:16, n:n + 1].to_broadcast([16, 128]), rhs=mbT[:, qs:qs + nq],
                                 start=False, stop=True), r=["mbT", "ident_bf"], w=[sk])
                        elif kind in ("A0", "A1"):
                            ntri = 256 if kind == "A0" else 128
                            P.op("pe", lambda e, ntri=ntri, sps=sps: e.matmul(sps[:, 0:ntri], lhsT=ident_bf, rhs=tribias[:, 0:ntri],
                                 start=False, stop=False), r=["tribias", "ident_bf"], w=[sk])
                            P.op("pe", lambda e, ntri=ntri, nq=nq, sps=sps, qbA=qbA, q0=q0: e.matmul(sps[:, ntri:nq],
                                 lhsT=ident_bf[0:16, qbA:qbA + 1].to_broadcast([16, 128]), rhs=mbT[:, q0 + 256:q0 + 512],
                                 start=False, stop=True), r=["mbT", "ident_bf"], w=[sk])
                        else:
                            P.op("pe", lambda e, nq=nq, sps=sps: e.matmul(sps[:, 0:nq], lhsT=ident_bf, rhs=tribias[:, 0:nq],
                                 start=False, stop=True), r=["tribias", "ident_bf"], w=[sk])

                    nch = len(chunks)
                    scores_pe(0)
                    for ci in range(nch):
                        kind, kc, co, nq = chunks[ci]
                        sps = pb[ci % 2]; sk = "pb%d" % (ci % 2)
                        ptile = pT[ci % 3]; ptk = ("pT", ci % 3)
                        P.op("act", lambda e, nq=nq, sps=sps, ptile=ptile: e.activation(out=ptile[:, 0:nq], in_=sps[:, 0:nq],
                             func=AF.Exp, scale=SCALE), r=[sk], w=[ptk])
                        if ci + 1 < nch:
                            scores_pe(ci + 1)
                        first, last = (ci == 0), (ci == nch - 1)
                        P.op("pe", lambda e, kc=kc, nq=nq, co=co, ptile=ptile, ops_=ops_, first=first, last=last: e.matmul(
                             ops_[:, co:co + nq], lhsT=vb_[:, kc, :], rhs=ptile[:, 0:nq], start=first, stop=last),
                             r=["vb", ptk], w=[ok])
                        P.op("pe", lambda e, nq=nq, co=co, ptile=ptile, lps=lps, first=first, last=last: e.matmul(
                             lps[:, co:co + nq], lhsT=ones_bf, rhs=ptile[:, 0:nq], start=first, stop=last),
                             r=["ones_bf", ptk], w=[lk])
                    P.op("dve", lambda e, lps=lps: e.reciprocal(out=rl, in_=lps[:, :]), r=[lk], w=["rl"])
                    P.op("dve", lambda e, hd=hd, q0=q0, ops_=ops_: e.tensor_tensor(out=yT[:, hd, q0:q0 + 512], in0=ops_[:, :],
                         in1=rl, op=ALU.mult), r=[ok, "rl"], w=[("yT", hd)])
            barrier()
            AR.pop()

        G = {}

        def phaseG():
            AR.off = off_mix
            AR.push()
            modb = AR.get(F32, [4 * D])
            g1b, sh2b, gs2b, g2b = modb[:, 0:D], modb[:, D:2 * D], modb[:, 2 * D:3 * D], modb[:, 3 * D:4 * D]
            G["g2b"] = g2b
            gates_all = AR.get(F32, [16, 4])
            G["off_keep"] = AR.off
            dma("sp", modb, modb_d, r=["modb_scr"], w=["modb"])
            wout = AR.get(BF16, [8, D])
            xt = [AR.get(F32, [D]), AR.get(F32, [D])]; x1t = [AR.get(F32, [D]), AR.get(F32, [D])]
            h2f = AR.get(F32, [D]); junk = AR.get(BF16, [D])
            h2b_all = AR.get(BF16, [16, D]); h2T = AR.get(F32, [8, 128]); wrf = AR.get(F32, [8, NE])
            brb = AR.get(F32, [NE]); lg = AR.get(F32, [NE]); top8 = AR.get(F32, [8]); idx8 = AR.get(U32, [8])
            ss = AR.get(F32, [16]); lnv = AR.get(F32, [16]); rs2 = AR.get(F32, [16])
            negv = AR.get(F32, [1]); e4 = AR.get(F32, [4]); se = AR.get(F32, [1]); rse = AR.get(F32, [1])
            idxf_all = AR.get(F32, [16, 4]); A_all = AR.get(BF16, [16, NE])
            dest_all = dest_sb[:, :, :]; io32 = AR.get(F32, [NE])
            eq = AR.get(F32, [NE]); s1_ = AR.get(F32, [1]); d1 = AR.get(F32, [1]); ov = AR.get(F32, [1])
            zf = AR.get(F32, [D]); pcol = AR.get(F32, [2]); tt_ = AR.get(F32, [1])
            cnt_b = AR.get(F32, [NE]); heavy_b = AR.get(F32, [NE]); hs_b = AR.get(F32, [NE]); one32 = AR.get(F32, [NE])
            hsk = AR.get(F32, [1]); lt_ = AR.get(F32, [1]); okh = AR.get(F32, [1]); dh_ = AR.get(F32, [1]); ehf = AR.get(F32, [1]); k128 = AR.get(F32, [8]); tmp8 = AR.get(F32, [8]); eq2 = AR.get(F32, [NE])
            eq4 = AR.get(F32, [4, NE]); eq4b = AR.get(F32, [4, NE]); hsk4 = AR.get(F32, [4]); s14 = AR.get(F32, [4]); lt4 = AR.get(F32, [4])
            d4 = AR.get(F32, [4]); dh4 = AR.get(F32, [4]); ok4 = AR.get(F32, [4]); ov4 = AR.get(F32, [4])
            G.update(gates_all=gates_all, dest_all=dest_all, h2b_all=h2b_all)
            dma("pool", wout, w_out.rearrange("(k p) n -> p k n", p=128), w=["wout"])
            dma("sp", wrf, w_router.rearrange("(k p) n -> p k n", p=128), w=["wrf"])
            dma("sp", brb, b_router_row.partition_broadcast(128), w=["brb"])
            dma("sp", io32, iota32_d, w=["io32"]); dma("sp", pcol, pcol_d, w=["pcol"]); dma("sp", k128, k128_d, w=["k128"])
            P.op("dve", lambda e: e.memset(zf, 0.0), w=["zf"])
            dma("sp", yall_d[NTOT:NTOT + 128, :], zf, r=["zf"], w=["Yall"])
            pbtF = [pbt[0][:, :].bitcast(F32), pbt[1][:, :].bitcast(F32)]
            h2f2 = [h2f, AR.get(F32, [D])]; h2T2 = [h2T, AR.get(F32, [8, 128])]
            lg2 = [lg, AR.get(F32, [NE])]; top82 = [top8, AR.get(F32, [8])]; idx82 = [idx8, AR.get(U32, [8])]
            negv2 = [negv, AR.get(F32, [1])]; e42 = [e4, AR.get(F32, [4])]; se2 = [se, AR.get(F32, [1])]; rse2 = [rse, AR.get(F32, [1])]

            def tile_steps(i):
                steps = []
                S = lambda *a_, **k_: steps.append((a_, k_))
                b = i % 2
                K = lambda n: (n, b)
                h2f_, h2T_, lg_, top8_, idx8_, negv_, e4_, se_, rse_ = h2f2[b], h2T2[b], lg2[b], top82[b], idx82[b], negv2[b], e42[b], se2[b], rse2[b]
                S("sp", (lambda e, b=b, i=i: e.dma_start(out=xt[b], in_=x_own[i * 128:(i + 1) * 128, :])), w=[("xt", b)], dma=True)
                ps = pb[b]; pk = "pb%d" % b
                for nb in range(2):
                    for k in range(8):
                        S("pe", lambda e, k=k, nb=nb, i=i, ps=ps: e.matmul(ps[:, :], lhsT=mixT[:, k, i * 128:(i + 1) * 128],
                             rhs=wout[:, k, nb * 512:(nb + 1) * 512], start=(k == 0), stop=(k == 7)), r=["mixT", "wout"], w=[pk])
                    S("dve", lambda e, nb=nb, b=b, ps=ps: e.tensor_tensor(out=x1t[b][:, nb * 512:(nb + 1) * 512], in0=ps[:, :],
                         in1=g1b[:, nb * 512:(nb + 1) * 512], op=ALU.mult), r=[pk, "modb"], w=[("x1t", b)])
                S("dve", lambda e, b=b: e.tensor_tensor(out=x1t[b], in0=x1t[b], in1=xt[b], op=ALU.add),
                     r=[("x1t", b), ("xt", b)], w=[("x1t", b)])
                S("sp", (lambda e, b=b, i=i: e.dma_start(out=x1_d[i * 128:(i + 1) * 128, :], in_=x1t[b])), r=[("x1t", b)], w=["x1d"], dma=True)
                S("act", lambda e, b=b, i=i: e.activation(out=junk, in_=x1t[b], func=AF.Square, accum_out=ss[:, i:i + 1]),
                     r=[("x1t", b)], w=["junk", ("ss", i)])
                S("act", lambda e, i=i: e.activation(out=lnv[:, i:i + 1], in_=ss[:, i:i + 1], func=AF.Ln, scale=1.0 / D, bias=EPS),
                     r=[("ss", i)], w=[("lnv", i)])
                S("act", lambda e, i=i: e.activation(out=rs2[:, i:i + 1], in_=lnv[:, i:i + 1], func=AF.Exp, scale=-0.5),
                     r=[("lnv", i)], w=[("rs2", i)])
                S("dve", lambda e, b=b, i=i: e.scalar_tensor_tensor(out=h2f_, in0=x1t[b], scalar=rs2[:, i:i + 1], in1=gs2b,
                     op0=ALU.mult, op1=ALU.mult), r=[("x1t", b), ("rs2", i), "modb"], w=[K("h2f")])
                S("dve", lambda e: e.tensor_tensor(out=h2f_, in0=h2f_, in1=sh2b, op=ALU.add), r=[K("h2f"), "modb"], w=[K("h2f")])
                S("act", lambda e, i=i: e.activation(out=h2b_all[:, i, :], in_=h2f_, func=AF.Copy), r=[K("h2f")], w=[("h2b", i)])
                tpb = [pb[2 + 2 * b], pb[3 + 2 * b]]; tkk = ["pb%d" % (2 + 2 * b), "pb%d" % (3 + 2 * b)]
                for c in range(8):
                    tp = tpb[c // 4]; tk = tkk[c // 4]
                    S("pe", lambda e, c=c, tp=tp: e.transpose(tp[:, (c % 4) * 128:(c % 4 + 1) * 128],
                         h2f_[:, c * 128:(c + 1) * 128], ident_f), r=[K("h2f"), "ident_f"], w=[tk])
                S("act", lambda e: e.activation(out=h2T_[:, 0:4, :], in_=tpb[0][:, :].rearrange("p (a b) -> p a b", a=4),
                     func=AF.Copy), r=[tkk[0]], w=[("h2T", b, 0)])
                S("dve", lambda e: e.tensor_copy(out=h2T_[:, 4:8, :], in_=tpb[1][:, :].rearrange("p (a b) -> p a b", a=4)),
                     r=[tkk[1]], w=[("h2T", b, 1)])
                lp = pbtF[b]; lpk = "pbt%d" % b
                for k in range(8):
                    S("pe", lambda e, k=k: e.matmul(lp[:, 0:NE], lhsT=h2T_[:, k, :], rhs=wrf[:, k, :],
                         start=(k == 0), stop=(k == 7)), r=[("h2T", b, 0), ("h2T", b, 1), "wrf"], w=[lpk])
                S("dve", lambda e: e.tensor_tensor(out=lg_, in0=lp[:, 0:NE], in1=brb, op=ALU.add), r=[lpk, "brb"], w=[K("lg")])
                S("dve", lambda e: e.max(out=top8_, in_=lg_), r=[K("lg")], w=[K("top8")])
                S("dve", lambda e: e.max_index(out=idx8_, in_max=top8_, in_values=lg_), r=[K("lg"), K("top8")], w=[K("idx8")])
                S("dve", lambda e: e.tensor_scalar(out=negv_, in0=top8_[:, 0:1], scalar1=-1.0, scalar2=None, op0=ALU.mult),
                     r=[K("top8")], w=[K("negv")])
                S("act", lambda e: e.activation(out=e4_, in_=top8_[:, 0:4], func=AF.Exp, bias=negv_[:, 0:1], accum_out=se_),
                     r=[K("top8"), K("negv")], w=[K("e4"), K("se")])
                S("dve", lambda e: e.reciprocal(out=rse_, in_=se_), r=[K("se")], w=[K("rse")])
                S("dve", lambda e, i=i: e.tensor_scalar(out=gates_all[:, i, :], in0=e4_, scalar1=rse_[:, 0:1], scalar2=None,
                     op0=ALU.mult), r=[K("e4"), K("rse")], w=[("gates", i)])
                S("dve", lambda e, i=i: e.tensor_scalar(out=A_all[:, i, :], in0=lg_, scalar1=top8_[:, 3:4], scalar2=None,
                     op0=ALU.is_ge), r=[K("lg"), K("top8")], w=[("A", i)])
                S("dve", lambda e, i=i: e.tensor_copy(out=idxf_all[:, i, :], in_=idx8_[:, 0:4]), r=[K("idx8")], w=[("idxf", i)])
                return steps

            import itertools
            ch0 = []; ch1 = [None] * 24
            for i2 in range(0, NT, 2):
                ch0 += tile_steps(i2); ch1 += tile_steps(i2 + 1)
            for xs in itertools.zip_longest(ch0, ch1):
                for x in xs:
                    if x is not None:
                        P.op(*x[0], **x[1])
            for i in range(NT):
                P.op("pe", lambda e, i=i: e.matmul(pb[0][:, 0:NE], lhsT=ones_bf, rhs=A_all[:, i, :], start=(i == 0), stop=(i == NT - 1)),
                     r=["ones_bf", "A"], w=["pb0"])
            P.op("dve", lambda e: e.tensor_copy(out=cnt_b, in_=pb[0][:, 0:NE]), r=["pb0"], w=["cnt_b"])
            P.op("dve", lambda e: e.tensor_scalar(out=heavy_b, in0=cnt_b, scalar1=float(CAP), scalar2=None, op0=ALU.is_gt),
                 r=["cnt_b"], w=["heavy_b"])
            P.op("dve", lambda e: e.memset(one32, 1.0), w=["one32"])
            P.op("dve", lambda e: e.tensor_tensor_scan(out=hs_b, data0=one32, data1=heavy_b, initial=0.0, op0=ALU.mult, op1=ALU.add),
                 r=["one32", "heavy_b"], w=["hs_b"])
            P.op("dve", lambda e: e.tensor_tensor(out=hs_b, in0=hs_b, in1=heavy_b, op=ALU.subtract), r=["hs_b", "heavy_b"], w=["hs_b"])
            for h_ in range(NH):
                P.op("dve", lambda e, h_=h_: e.tensor_scalar(out=eq2, in0=hs_b, scalar1=float(h_), scalar2=None, op0=ALU.is_equal),
                     r=["hs_b"], w=["eq2"])
                P.op("dve", lambda e: e.tensor_tensor(out=eq2, in0=eq2, in1=heavy_b, op=ALU.mult), r=["eq2", "heavy_b"], w=["eq2"])
                P.op("dve", lambda e: e.tensor_tensor(out=eq2, in0=eq2, in1=io32, op=ALU.mult), r=["eq2", "io32"], w=["eq2"])
                P.op("dve", lambda e: e.tensor_reduce(out=ehf, in_=eq2, axis=AX.X, op=ALU.add), r=["eq2"], w=["ehf"])
                P.op("dve", lambda e: e.scalar_tensor_tensor(out=dh_, in0=ehf, scalar=1024.0, in1=pcol[:, 1:2], op0=ALU.mult, op1=ALU.add),
                     r=["ehf", "pcol"], w=["dh_"])
                P.op("dve", lambda e: e.tensor_scalar(out=tmp8, in0=k128, scalar1=dh_[:, 0:1], scalar2=None, op0=ALU.add),
                     r=["k128", "dh_"], w=["tmp8"])
                P.op("dve", lambda e, h_=h_: e.tensor_copy(out=idxw_sb[:, h_, :], in_=tmp8), r=["tmp8"], w=[("idxw", h_)])
                P.op("dve", lambda e: e.scalar_tensor_tensor(out=dh_, in0=ehf, scalar=128.0, in1=pcol[:, 1:2], op0=ALU.mult, op1=ALU.add),
                     r=["ehf", "pcol"], w=["dh_"])
                P.op("dve", lambda e, h_=h_: e.tensor_copy(out=idxb_sb[:, h_, 0:1], in_=dh_), r=["dh_"], w=[("idxb", h_)])
                P.op("dve", lambda e, h_=h_: e.tensor_copy(out=idxb_sb[:, h_, 1:2], in_=ehf), r=["ehf"], w=[("idxb", h_)])
            for i in range(NT):
                P.op("pe", lambda e, i=i: e.matmul(pb[5][:, 0:NE], lhsT=triu_bf, rhs=A_all[:, i, :], start=True, stop=(i == 0)),
                     r=["triu_bf", "A"], w=["pb5"])
                for j in range(i):
                    P.op("pe", lambda e, j=j, i=i: e.matmul(pb[5][:, 0:NE], lhsT=ones_bf, rhs=A_all[:, j, :], start=False,
                         stop=(j == i - 1)), r=["ones_bf", "A"], w=["pb5"])
                idx4 = idxf_all[:, i, :]
                P.op("dve", lambda e, idx4=idx4: e.tensor_tensor(out=eq4, in0=io32.unsqueeze(1).to_broadcast([128, 4, NE]),
                     in1=idx4.unsqueeze(2).to_broadcast([128, 4, NE]), op=ALU.is_equal), r=["io32", ("idxf", i)], w=["eq4"])
                P.op("dve", lambda e: e.tensor_tensor(out=eq4b, in0=eq4, in1=hs_b.unsqueeze(1).to_broadcast([128, 4, NE]), op=ALU.mult),
                     r=["eq4", "hs_b"], w=["eq4b"])
                P.op("dve", lambda e: e.tensor_reduce(out=hsk4, in_=eq4b, axis=AX.X, op=ALU.add), r=["eq4b"], w=["hsk4"])
                P.op("dve", lambda e: e.tensor_tensor(out=eq4, in0=eq4, in1=pb[5][:, 0:NE].unsqueeze(1).to_broadcast([128, 4, NE]),
                     op=ALU.mult), r=["eq4", "pb5"], w=["eq4"])
                P.op("dve", lambda e: e.tensor_reduce(out=s14, in_=eq4, axis=AX.X, op=ALU.add), r=["eq4"], w=["s14"])
                P.op("dve", lambda e: e.tensor_scalar(out=lt4, in0=s14, scalar1=float(CAP), scalar2=None, op0=ALU.is_le), r=["s14"], w=["lt4"])
                P.op("dve", lambda e, idx4=idx4: e.scalar_tensor_tensor(out=d4, in0=idx4, scalar=float(CAP), in1=s14, op0=ALU.mult, op1=ALU.add),
                     r=[("idxf", i), "s14"], w=["d4"])
                P.op("dve", lambda e: e.scalar_tensor_tensor(out=dh4, in0=hsk4, scalar=float(CH), in1=s14, op0=ALU.mult, op1=ALU.add),
                     r=["hsk4", "s14"], w=["dh4"])
                P.op("dve", lambda e: e.tensor_scalar(out=ok4, in0=hsk4, scalar1=float(NH) - 0.5, scalar2=None, op0=ALU.is_lt), r=["hsk4"], w=["ok4"])
                P.op("dve", lambda e: e.tensor_scalar(out=ov4, in0=s14, scalar1=float(CAP + CH), scalar2=None, op0=ALU.is_le), r=["s14"], w=["ov4"])
                P.op("dve", lambda e: e.tensor_tensor(out=ok4, in0=ok4, in1=ov4, op=ALU.mult), r=["ok4", "ov4"], w=["ok4"])
                P.op("dve", lambda e: e.tensor_scalar(out=ov4, in0=lt4, scalar1=-1.0, scalar2=1.0, op0=ALU.mult, op1=ALU.add), r=["lt4"], w=["ov4"])
                P.op("dve", lambda e: e.tensor_tensor(out=ok4, in0=ok4, in1=ov4, op=ALU.mult), r=["ok4", "ov4"], w=["ok4"])
                P.op("dve", lambda e: e.tensor_scalar(out=d4, in0=d4, scalar1=-1.0, scalar2=pcol[:, 0:1], op0=ALU.add, op1=ALU.subtract),
                     r=["d4", "pcol"], w=["d4"])
                P.op("dve", lambda e: e.tensor_scalar(out=dh4, in0=dh4, scalar1=float(NE * CAP - CAP - 1), scalar2=pcol[:, 0:1],
                     op0=ALU.add, op1=ALU.subtract), r=["dh4", "pcol"], w=["dh4"])
                P.op("dve", lambda e: e.tensor_tensor(out=d4, in0=d4, in1=lt4, op=ALU.mult), r=["d4", "lt4"], w=["d4"])
                P.op("dve", lambda e: e.tensor_tensor(out=dh4, in0=dh4, in1=ok4, op=ALU.mult), r=["dh4", "ok4"], w=["dh4"])
                P.op("dve", lambda e: e.tensor_tensor(out=d4, in0=d4, in1=dh4, op=ALU.add), r=["d4", "dh4"], w=["d4"])
                P.op("dve", lambda e: e.tensor_scalar(out=d4, in0=d4, scalar1=pcol[:, 0:1], scalar2=None, op0=ALU.add), r=["d4", "pcol"], w=["d4"])
                P.op("dve", lambda e, i=i: e.tensor_copy(out=dest_all[:, i, :], in_=d4), r=["d4"], w=[("dest", i)])
            for i in range(NT):
                sg_ = stage_sb[i % 2]; sgk = ("stage", i % 2)
                P.op("act", lambda e, i=i, sg_=sg_: e.activation(out=sg_[:, :], in_=h2b_all[:, i, :], func=AF.Copy),
                     r=[("h2b", i)], w=[sgk])
                for k in range(4):
                    P.op("pool", lambda e, i=i, k=k, sg_=sg_: e.indirect_dma_start(out=xg_d[:, :],
                         out_offset=bass.IndirectOffsetOnAxis(ap=dest_sb[:, i, k:k + 1], axis=0), in_=sg_[:, :],
                         in_offset=None),
                         r=[("dest", i), sgk], w=["Xg"], dma=True)
            if "gates" in dbg_out:
                finals.append(dma("sp", dbg_out["gates"], gates_all, r=["gates"]))
                finals.append(dma("sp", dbg_out["idxf"], idxf_all, r=["idxf"]))
                P.op("dve", lambda e: e.tensor_copy(out=gates_all, in_=dest_all), r=["dest", "gates"], w=["gates"])
                finals.append(dma("sp", dbg_out["dest"], gates_all, r=["gates"]))
            barrier()

        def phaseI():
            NS = CAP // 128
            AR.off = off_base
            xe = AR.get(BF16, [NS, D]); actT = AR.get(BF16, [8, CAP])
            assert AR.off <= off_mix
            AR.off = G["off_keep"]
            AR.push()
            wup = [AR.get(BF16, [8, 2 * D]), AR.get(BF16, [8, 2 * D])]
            wdn = [AR.get(BF16, [8, D]), AR.get(BF16, [8, D])]
            xeT = AR.get(BF16, [8, CAP])
            ye = [AR.get(F32, [D]), AR.get(F32, [D])]
            bdn = [AR.get(F32, [D]), AR.get(F32, [D])]
            bupc = AR.get(F32, [NE, 16]); buph = AR.get(F32, [NH, 16])
            gm = [AR.get(F32, [512]), AR.get(F32, [512])]; sgm = [AR.get(F32, [512]), AR.get(F32, [512])]
            l1 = [AR.get(F32, [512]), AR.get(F32, [512])]
            dma("sp", bupc, bup_col.rearrange("p (a b) -> p a b", a=NE), w=["bupc"])
            dbanks = [(pb[4], "pb4"), (pb[5], "pb5"), (pbt[0][:, :].bitcast(F32), "pbt0"), (pbt[1][:, :].bitcast(F32), "pbt1")]
            wupf = w_up.rearrange("e k n -> (e k) n"); wdnf = w_down.rearrange("e k n -> (e k) n")

            def load_w(e_, wb):
                wu3 = w_up[e_].rearrange("(k p) n -> p k n", p=128)
                for hh in range(2):
                    dma("pool", wup[wb][:, :, hh * D:(hh + 1) * D], wu3[:, :, hh * D:(hh + 1) * D], w=[("wup", wb)])
                dma("pool", wdn[wb], w_down[e_].rearrange("(k p) n -> p k n", p=128), w=[("wdn", wb)])
                dma("sp", bdn[wb], b_down[e_:e_ + 1, :].partition_broadcast(128), w=[("bdn", wb)])

            def load_w_heavy(h_, wb):
                for k in range(8):
                    P.op("pool", lambda e, k=k: e.indirect_dma_start(out=wup[wb][:, k, :], out_offset=None, in_=wupf[:, :],
                         in_offset=bass.IndirectOffsetOnAxis(ap=idxw_sb[:, h_, k:k + 1], axis=0)), r=[("idxw", h_)], w=[("wup", wb)], dma=True)
                for k in range(8):
                    P.op("pool", lambda e, k=k: e.indirect_dma_start(out=wdn[wb][:, k, :], out_offset=None, in_=wdnf[:, :],
                         in_offset=bass.IndirectOffsetOnAxis(ap=idxw_sb[:, h_, k:k + 1], axis=0)), r=[("idxw", h_)], w=[("wdn", wb)], dma=True)
                P.op("pool", lambda e: e.indirect_dma_start(out=buph[:, h_, :], out_offset=None, in_=bq_d[:, :],
                     in_offset=bass.IndirectOffsetOnAxis(ap=idxb_sb[:, h_, 0:1], axis=0)), r=[("idxb", h_)], w=[("buph", h_)], dma=True)
                P.op("pool", lambda e: e.indirect_dma_start(out=bdn[wb], out_offset=None, in_=b_down[:, :],
                     in_offset=bass.IndirectOffsetOnAxis(ap=idxb_sb[:, h_, 1:2], axis=0)), r=[("idxb", h_)], w=[("bdn", wb)], dma=True)

            def transposes(row0, ns):
                dma("sp", xe[:, 0:ns, :], xg_d[row0:row0 + ns * 128, :].rearrange("(s p) d -> p s d", p=128), r=["Xg"], w=["xe"])
                for c in range(8):
                    tp = pbt[c % 2]; tk = "pbt%d" % (c % 2)
                    for s_ in range(ns):
                        P.op("pe", lambda e, c=c, s_=s_, tp=tp: e.transpose(tp[:, s_ * 128:(s_ + 1) * 128],
                             xe[:, s_, c * 128:(c + 1) * 128], ident_bf), r=["xe", "ident_bf"], w=[tk])
                    if c % 2 == 0:
                        P.op("act", lambda e, c=c, tp=tp: e.activation(out=xeT[:, c, 0:ns * 128], in_=tp[:, 0:ns * 128], func=AF.Copy),
                             r=[tk], w=[("xeT", c)])
                    else:
                        P.op("dve", lambda e, c=c, tp=tp: e.tensor_copy(out=xeT[:, c, 0:ns * 128], in_=tp[:, 0:ns * 128]), r=[tk], w=[("xeT", c)])

            def up(wb, nn, bcol, bkey):
                n0 = 0
                for f in range(8):
                    bb = f % 2
                    gps = pb[bb * 2]; gk = "pb%d" % (bb * 2); lps = pb[bb * 2 + 1]; lk = "pb%d" % (bb * 2 + 1)
                    for k in range(8):
                        P.op("pe", lambda e, k=k, f=f, gps=gps: e.matmul(gps[:, 0:nn],
                             lhsT=wup[wb][:, k, f * 128:(f + 1) * 128], rhs=xeT[:, k, n0:n0 + nn], start=(k == 0), stop=(k == 7)),
                             r=[("wup", wb), "xeT"], w=[gk])
                    for k in range(8):
                        P.op("pe", lambda e, k=k, f=f, lps=lps: e.matmul(lps[:, 0:nn],
                             lhsT=wup[wb][:, k, D + f * 128:D + (f + 1) * 128], rhs=xeT[:, k, n0:n0 + nn], start=(k == 0),
                             stop=(k == 7)), r=[("wup", wb), "xeT"], w=[lk])
                    g_, s__, l_ = gm[bb], sgm[bb], l1[bb]
                    P.op("dve", lambda e, f=f, gps=gps, g_=g_: e.tensor_scalar(out=g_[:, 0:nn], in0=gps[:, 0:nn],
                         scalar1=bcol(f), scalar2=7.0, op0=ALU.add, op1=ALU.min), r=[gk, bkey], w=[("gm", bb)])
                    P.op("act", lambda e, g_=g_, s__=s__: e.activation(out=s__[:, 0:nn], in_=g_[:, 0:nn], func=AF.Sigmoid,
                         scale=1.702), r=[("gm", bb)], w=[("sgm", bb)])
                    P.op("dve", lambda e, f=f, lps=lps, l_=l_: e.tensor_scalar(out=l_[:, 0:nn], in0=lps[:, 0:nn],
                         scalar1=bcol(8 + f), scalar2=7.0, op0=ALU.add, op1=ALU.min), r=[lk, bkey], w=[("l1", bb)])
                    P.op("dve", lambda e, l_=l_: e.tensor_scalar(out=l_[:, 0:nn], in0=l_[:, 0:nn], scalar1=-7.0, scalar2=1.0,
                         op0=ALU.max, op1=ALU.add), r=[("l1", bb)], w=[("l1", bb)])
                    P.op("dve", lambda e, g_=g_, s__=s__: e.tensor_tensor(out=g_[:, 0:nn], in0=g_[:, 0:nn], in1=s__[:, 0:nn],
                         op=ALU.mult), r=[("gm", bb), ("sgm", bb)], w=[("gm", bb)])
                    P.op("dve", lambda e, f=f, g_=g_, l_=l_: e.tensor_tensor(out=actT[:, f, n0:n0 + nn],
                         in0=g_[:, 0:nn], in1=l_[:, 0:nn], op=ALU.mult), r=[("gm", bb), ("l1", bb)], w=[("actT", f)])

            def down(wb, row0, ns):
                for s_ in range(ns):
                    yb = ye[s_ % 2]; yk = ("ye", s_ % 2)
                    for nb in range(2):
                        ps, pk = dbanks[(s_ * 2 + nb) % 4]
                        for f in range(8):
                            P.op("pe", lambda e, f=f, s_=s_, nb=nb, ps=ps: e.matmul(ps[:, :], lhsT=actT[:, f, s_ * 128:(s_ + 1) * 128],
                                 rhs=wdn[wb][:, f, nb * 512:(nb + 1) * 512], start=(f == 0), stop=(f == 7)),
                                 r=["actT", ("wdn", wb)], w=[pk])
                        P.op("dve", lambda e, nb=nb, ps=ps, yb=yb: e.tensor_tensor(out=yb[:, nb * 512:(nb + 1) * 512], in0=ps[:, :],
                             in1=bdn[wb][:, nb * 512:(nb + 1) * 512], op=ALU.add), r=[pk, ("bdn", wb)], w=[yk])
                    r0 = row0 + s_ * 128
                    dma("sp", yall_d[r0:r0 + 128, :], yb, r=[yk], w=["Yall"])

            NU = NE + NH
            def unit(u):
                if u < NE:
                    return dict(row0=u * CAP, ns=NS, nn=CAP, bcol=(lambda f, u=u: bupc[:, u, f:f + 1]), bkey="bupc")
                h_ = u - NE
                return dict(row0=NE * CAP + h_ * CH, ns=CH // 128, nn=CH, bcol=(lambda f, h_=h_: buph[:, h_, f:f + 1]), bkey=("buph", h_))

            def load_unit(u, wb):
                if u < NE:
                    load_w(u, wb)
                else:
                    load_w_heavy(u - NE, wb)

            hpos = {6: 0, 13: 1, 20: 2, 27: 3}
            order = []
            for e_ in range(NE):
                order.append(e_)
                if e_ in hpos:
                    order.append(NE + hpos[e_])
            load_unit(order[0], 0)
            transposes(unit(order[0])["row0"], unit(order[0])["ns"])
            for pos_, u in enumerate(order):
                U = unit(u)
                wb = pos_ % 2
                if pos_ + 1 < len(order):
                    load_unit(order[pos_ + 1], (pos_ + 1) % 2)
                up(wb, U["nn"], U["bcol"], U["bkey"])
                if pos_ + 1 < len(order):
                    U2 = unit(order[pos_ + 1])
                    transposes(U2["row0"], U2["ns"])
                down(wb, U["row0"], U["ns"])
            barrier()
            AR.pop()

        def phaseJ():
            AR.off = G["off_keep"]
            AR.push()
            gates_all = G["gates_all"]; g2b = G["g2b"]
            yg = [[AR.get(F32, [D]) for _ in range(4)] for _ in range(2)]
            x1t = [AR.get(F32, [D]), AR.get(F32, [D])]; acc = [AR.get(F32, [D]), AR.get(F32, [D])]
            for i in range(NT):
                b = i % 2
                dma("sp", x1t[b], x1_d[i * 128:(i + 1) * 128, :], r=["x1d"], w=[("x1t", b)])
                for k in range(4):
                    P.op("pool", lambda e, i=i, k=k, b=b: e.indirect_dma_start(out=yg[b][k], out_offset=None, in_=yall_d[:, :],
                         in_offset=bass.IndirectOffsetOnAxis(ap=dest_sb[:, i, k:k + 1], axis=0)),
                         r=["Yall", ("dest", i)], w=[("yg", b, k)], dma=True)
                a_ = acc[b]; ak = ("acc", b)
                P.op("dve", lambda e, i=i, b=b, a_=a_: e.tensor_scalar(out=a_, in0=yg[b][0], scalar1=gates_all[:, i, 0:1], scalar2=None,
                     op0=ALU.mult), r=[("yg", b, 0), "gates"], w=[ak])
                for k in (1, 2, 3):
                    P.op("dve", lambda e, i=i, b=b, k=k, a_=a_: e.scalar_tensor_tensor(out=a_, in0=yg[b][k],
                         scalar=gates_all[:, i, k:k + 1], in1=a_, op0=ALU.mult, op1=ALU.add), r=[("yg", b, k), "gates", ak], w=[ak])
                P.op("dve", lambda e, a_=a_: e.tensor_tensor(out=a_, in0=a_, in1=g2b, op=ALU.mult), r=[ak, "modb"], w=[ak])
                P.op("dve", lambda e, b=b, a_=a_: e.tensor_tensor(out=a_, in0=a_, in1=x1t[b], op=ALU.add), r=[ak, ("x1t", b)], w=[ak])
                finals.append(dma("act", out_d[i * 128:(i + 1) * 128, :], a_, r=[ak]))
            AR.pop()

        phases = [phaseB, phaseC, phaseD, phaseE, phaseF, phaseG, phaseI, phaseJ]
        for i, ph in enumerate(phases):
            if stage < i + 2:
                break
            ph()
        def dump_bf(name, src, n):
            if name not in dbg_out:
                return
            AR.push()
            stg = [AR.get(F32, [n]), AR.get(F32, [n])]
            for k in range(8):
                P.op("dve", lambda e, k=k: e.tensor_copy(out=stg[k % 2], in_=src[:, k, :]), r=[name], w=[("stg", k % 2)])
                finals.append(dma("sp", dbg_out[name][:, k, :], stg[k % 2], r=[("stg", k % 2)]))
            AR.pop()
        if stage < 7:
            dump_bf("hT", hT, LT); dump_bf("yT", yT, T); dump_bf("mixT", mixT, T)
        if "x1" in dbg_out:
            AR.push()
            stg = AR.get(F32, [D])
            for i in range(NT):
                dma("sp", stg, x1_d[i * 128:(i + 1) * 128, :], r=["x1d"], w=["stg"])
                finals.append(dma("sp", dbg_out["x1"][i * 128:(i + 1) * 128, :], stg, r=["stg"]))
            AR.pop()
        P.emit(finals)
    return nc


def _consts():
    bf = ml_dtypes.bfloat16
    c = {}
    c["ident_bf"] = np.eye(128, dtype=np.float32).astype(bf)
    c["ident_f"] = np.eye(128, dtype=np.float32)
    k = np.arange(128)
    c["triu_bf"] = (k[:, None] <= k[None, :]).astype(np.float32).astype(bf)
    tb = np.zeros((128, 256), np.float32)
    tb[:, :128] = np.where(k[:, None] <= k[None, :], 0.0, NEG)
    c["tribias"] = tb.astype(bf)
    rm = np.zeros((128, 32), np.float32)
    for m in range(16):
        rm[m + 16, m] = 1.0
        rm[m, m + 16] = 1.0
    c["rm_f"] = rm
    half = 16
    freqs = (np.float32(500000.0) ** (-np.arange(half, dtype=np.float32) / np.float32(half))).astype(np.float32)
    fc = np.zeros((32, 2), np.float32)
    fc[:, 0] = np.concatenate([freqs, freqs])
    fc[:16, 1] = -1.0
    fc[16:, 1] = 1.0
    c["freq_col"] = fc
    c["iota32"] = np.tile(np.arange(NE, dtype=np.float32)[None, :], (128, 1))
    c["pcol"] = np.stack([NTOT + np.arange(128, dtype=np.float32), np.arange(128, dtype=np.float32)], axis=1).copy()
    c["k128"] = np.tile((128.0 * np.arange(8, dtype=np.float32))[None, :], (128, 1))
    return c


def _col(v, nchunk):
    return np.ascontiguousarray(np.asarray(v, np.float32).reshape(nchunk, 128).T)


def prep_inputs(inp):
    f = lambda a: np.ascontiguousarray(np.asarray(a, np.float32))
    x = f(inp["x"]); c = f(inp["c"]); positions = np.asarray(inp["positions"]).astype(np.int32)
    shared = {
        "ada_w": f(inp["ada_w"][0]),
        "ada_b_col": _col(inp["ada_b"][0], 48),
        "ada_b_row": f(inp["ada_b"][0][None, 2 * D:]),
        "n1g_col": _col(inp["norm1_g"][0], 8),
        "n2g_row": f(inp["norm2_g"][0][None, :]),
        "w_in": f(inp["w_in"][0]),
        "convw_col": np.ascontiguousarray(f(inp["conv_w"][0]).T.reshape(8, 128, 4).transpose(1, 0, 2).reshape(128, 32)),
        "convb_col": _col(inp["conv_b"][0], 8),
        "w_rg_a": f(inp["w_rg_a"][0]), "brga_col": _col(inp["b_rg_a"][0], 8),
        "w_rg_x": f(inp["w_rg_x"][0]), "brgx_col": _col(inp["b_rg_x"][0], 8),
        "lam_col": _col(inp["lru_lambda"][0], 8),
        "qg_col": f(inp["q_norm_g"][0][:, None]), "kg_col": f(inp["k_norm_g"][0][:, None]),
        "bgate_col": np.ascontiguousarray(f(inp["b_gate"][0]).reshape(2, 8, 128).transpose(2, 0, 1).reshape(128, 16)),
        "w_branch": f(inp["w_branch"][0]), "w_out": f(inp["w_out"][0]),
        "w_router": f(inp["w_router"][0]), "b_router_row": f(inp["b_router"][0][None, :]),
        "w_up": f(inp["w_up"][0]),
        "bup_col": np.ascontiguousarray(f(inp["b_up"][0]).reshape(NE, 16, 128).transpose(2, 0, 1).reshape(128, NE * 16)),
        "w_down": f(inp["w_down"][0]), "b_down": f(inp["b_down"][0]),
        "bq": np.ascontiguousarray(f(inp["b_up"][0]).reshape(NE, 16, 128).transpose(0, 2, 1).reshape(NE * 128, 16)),
    }
    shared.update(_consts())
    maps = []
    for core in range(8):
        b, h = core // 2, core % 2
        m = dict(shared)
        m["x_own"] = np.ascontiguousarray(x[b, h * T:(h + 1) * T])
        m["x_pre"] = np.ascontiguousarray(x[b, 0:T])
        m["pos"] = np.ascontiguousarray(np.concatenate([positions[b, 0:T], positions[b, h * T:(h + 1) * T]])[None, :])
        m["c_col"] = _col(c[b], 8)
        m["hflag"] = np.full((128, 1), float(h), np.float32)
        vb = np.full((8, 16), NEG, np.float32)
        for j in range(8):
            for n in range(16):
                if n < 8 + j and (n >= 8 or h == 1):
                    vb[j, n] = 0.0
        m["vbias"] = np.ascontiguousarray(np.tile(vb.reshape(1, 128), (128, 1)))
        maps.append(m)
    return maps


_NC_CACHE = {}


def kernel(**inputs):
    maps = prep_inputs(inputs)
    if "nc" not in _NC_CACHE:
        _NC_CACHE["nc"] = build_program()
    res = run_bass_kernel_spmd(_NC_CACHE["nc"], maps, core_ids=list(range(8)))
    out = np.zeros((4, 2 * T, D), np.float32)
    for core in range(8):
        b, h = core // 2, core % 2
        out[b, h * T:(h + 1) * T] = res.results[core]["out"]
    return out
```
